# Optimizing a Trainium2 kernel written in Bass

```python
import jax
import jax.numpy as jnp
from jax import lax
import numpy as np


D_MODEL = 1024
BATCH = 8
SEQ = 4096
DEPTH = 1

GRID_W = 64
CTX_LEN = 256
D_MIX = D_MODEL
D_POOL = D_MIX // 2
POOL_WINDOWS = (2, 4, 8, 16)
POOL_GW = D_POOL // len(POOL_WINDOWS)
D_ATTN = D_MIX - D_POOL
NA_HEADS = 8
NA_HEAD_DIM = D_ATTN // NA_HEADS
NA_KH_MAX = 8
NA_KW = 16
D_IN = D_POOL + 3 * D_ATTN
PEER_HEADS = 8
PEER_NKEYS = 128
PEER_N_EXPERTS = PEER_NKEYS * PEER_NKEYS
PEER_TOPK = 16
PEER_DKEY = 256
PEER_TOKEN_BLOCK = 128
N_MOD = 6
EPS = 1e-6

kernel_name = 'hybrid_pool_natten_peer_dit_layer'


def rmsnorm(x, g):
    xf = x.astype(jnp.float32)
    y = xf * lax.rsqrt(jnp.mean(xf * xf, axis=-1, keepdims=True) + EPS)
    return (y * g.astype(jnp.float32)).astype(x.dtype)


def modulate(h, shift, scale):
    return h * (1 + scale) + shift


def multiscale_pool(p, w_grp, scale):
    B, L, _ = p.shape
    pf = p.astype(jnp.float32)
    cs = jnp.concatenate([jnp.zeros((B, 1, D_POOL), jnp.float32), jnp.cumsum(pf, axis=1)], axis=1)
    t = jnp.arange(L)
    outs = []
    for g, w in enumerate(POOL_WINDOWS):
        sl = slice(g * POOL_GW, (g + 1) * POOL_GW)
        lo = jnp.clip(t - w // 2, 0, L)
        hi = jnp.clip(t + w // 2, 0, L)
        csg = cs[..., sl]
        cnt = (hi - lo).astype(jnp.float32)[None, :, None]
        outs.append((jnp.take(csg, hi, axis=1) - jnp.take(csg, lo, axis=1)) / cnt - pf[..., sl])
    pooled = jnp.stack(outs, axis=2).astype(p.dtype)
    y = jnp.einsum('blgc,gcd->blgd', pooled, w_grp).reshape(B, L, D_POOL)
    return y * scale


def neighbourhood_attention(q, k, v, k_ctx, v_ctx, rpb):
    B, S, H, Dh = q.shape
    rows = S // GRID_W
    kh = min(NA_KH_MAX, rows)
    n_loc = kh * NA_KW
    qg = q.reshape(B, rows, GRID_W, H, Dh)
    kg = k.reshape(B, rows, GRID_W, H, Dh)
    vg = v.reshape(B, rows, GRID_W, H, Dh)
    col = jnp.arange(GRID_W)
    c0 = jnp.clip(col - NA_KW // 2, 0, GRID_W - NA_KW)
    col_idx = c0[:, None] + jnp.arange(NA_KW)[None, :]
    col_off = col_idx - col[:, None] + (NA_KW - 1)
    scale = Dh ** -0.5

    def row_block(r):
        r0 = jnp.clip(r - kh // 2, 0, rows - kh)
        k_rows = lax.dynamic_slice_in_dim(kg, r0, kh, axis=1)
        v_rows = lax.dynamic_slice_in_dim(vg, r0, kh, axis=1)
        k_win = k_rows[:, :, col_idx]
        v_win = v_rows[:, :, col_idx]
        q_r = lax.dynamic_index_in_dim(qg, r, axis=1, keepdims=False)
        row_off = r0 + jnp.arange(kh) - r + (NA_KH_MAX - 1)
        bias = rpb[:, row_off[None, :, None], col_off[:, None, :]].astype(jnp.float32)
        s_loc = jnp.einsum('bwhd,biwjhd->bhwij', q_r, k_win).astype(jnp.float32) * scale + bias
        s_ctx = jnp.einsum('bwhd,bchd->bhwc', q_r, k_ctx).astype(jnp.float32) * scale
        s_all = jnp.concatenate([s_loc.reshape(B, H, GRID_W, n_loc), s_ctx], axis=-1)
        prob = jax.nn.softmax(s_all, axis=-1)
        p_loc = prob[..., :n_loc].reshape(B, H, GRID_W, kh, NA_KW).astype(v.dtype)
        p_ctx = prob[..., n_loc:].astype(v.dtype)
        return (jnp.einsum('bhwij,biwjhd->bwhd', p_loc, v_win)
                + jnp.einsum('bhwc,bchd->bwhd', p_ctx, v_ctx))

    out = lax.map(row_block, jnp.arange(rows))
    return jnp.moveaxis(out, 0, 1).reshape(B, S, H * Dh)


def context_attention(q, k, v):
    B, C, H, Dh = q.shape
    s = jnp.einsum('bqhd,bkhd->bhqk', q, k).astype(jnp.float32) * (Dh ** -0.5)
    prob = jax.nn.softmax(s, axis=-1).astype(v.dtype)
    return jnp.einsum('bhqk,bkhd->bqhd', prob, v).reshape(B, C, H * Dh)


def peer(h, w_q, sub_keys, u, v):
    B, L, D = h.shape
    tok = h.reshape((B * L) // PEER_TOKEN_BLOCK, PEER_TOKEN_BLOCK, D)
    k = PEER_TOPK

    def block(xb):
        tb = xb.shape[0]
        q = (xb @ w_q).reshape(tb, PEER_HEADS, 2, PEER_DKEY // 2)
        s = jnp.einsum('thpd,hpnd->thpn', q, sub_keys).astype(jnp.float32)
        sv, si = lax.top_k(s, k)
        cand = (sv[:, :, 0, :, None] + sv[:, :, 1, None, :]).reshape(tb, PEER_HEADS, k * k)
        cand_idx = (si[:, :, 0, :, None] * PEER_NKEYS + si[:, :, 1, None, :]).reshape(tb, PEER_HEADS, k * k)
        top_v, top_i = lax.top_k(cand, k)
        experts = jnp.take_along_axis(cand_idx, top_i, axis=-1)
        g = jax.nn.softmax(top_v, axis=-1)
        u_e = u[experts]
        v_e = v[experts]
        a = jax.nn.gelu(jnp.einsum('td,thkd->thk', xb, u_e).astype(jnp.float32), approximate=False)
        return jnp.einsum('thk,thkd->td', (g * a).astype(xb.dtype), v_e)

    return lax.map(block, tok).reshape(B, L, D)


def split_heads(t):
    B, L, _ = t.shape
    return t.reshape(B, L, NA_HEADS, NA_HEAD_DIM)


def setup_inputs(seed: int = 0) -> dict:
    key = jax.random.key(seed)
    ks = jax.random.split(key, 18)
    f32 = jnp.float32
    D = D_MODEL

    def nrm(k, shape, s):
        return jax.random.normal(k, shape, f32) * s

    return {
        'x': nrm(ks[0], (BATCH, SEQ, D), 1.0),
        'c': nrm(ks[1], (BATCH, D), 1.0),
        'ctx': nrm(ks[2], (BATCH, CTX_LEN, D), 1.0),
        'c_ctx': nrm(ks[3], (D,), 1.0),
        'ada_w': nrm(ks[4], (DEPTH, D, N_MOD * D), 0.5 * D ** -0.5),
        'ada_b': nrm(ks[5], (DEPTH, N_MOD * D), 0.01),
        'norm1_g': 1.0 + nrm(ks[6], (DEPTH, D), 0.05),
        'w_in': nrm(ks[7], (DEPTH, D, D_IN), D ** -0.5),
        'pool_w': nrm(ks[8], (DEPTH, len(POOL_WINDOWS), POOL_GW, POOL_GW), POOL_GW ** -0.5),
        'pool_scale': 1.0 + nrm(ks[9], (DEPTH, D_POOL), 0.1),
        'na_rpb': nrm(ks[10], (DEPTH, NA_HEADS, 2 * NA_KH_MAX - 1, 2 * NA_KW - 1), 0.2),
        'w_out': nrm(ks[11], (DEPTH, D_MIX, D), D_MIX ** -0.5),
        'norm2_g': 1.0 + nrm(ks[12], (DEPTH, D), 0.05),
        'peer_wq': nrm(ks[13], (DEPTH, D, PEER_HEADS * PEER_DKEY), D ** -0.5),
        'peer_keys': nrm(ks[14], (DEPTH, PEER_HEADS, 2, PEER_NKEYS, PEER_DKEY // 2), (PEER_DKEY // 2) ** -0.5),
        'peer_u': nrm(ks[15], (DEPTH, PEER_N_EXPERTS, D), D ** -0.5),
        'peer_v': nrm(ks[16], (DEPTH, PEER_N_EXPERTS, D), 0.5),
        'final_g': 1.0 + nrm(ks[17], (D,), 0.05),
    }


def reference(x, c, ctx, c_ctx, ada_w, ada_b, norm1_g, w_in, pool_w, pool_scale, na_rpb, w_out,
              norm2_g, peer_wq, peer_keys, peer_u, peer_v, final_g):
    kv_cols = slice(D_POOL + D_ATTN, D_IN)
    for layer in range(DEPTH):
        last = layer == DEPTH - 1
        mod_x = (jax.nn.silu(c) @ ada_w[layer] + ada_b[layer])[:, None, :]
        mod_c = jax.nn.silu(c_ctx) @ ada_w[layer] + ada_b[layer]
        sh1, sc1, g1, sh2, sc2, g2 = jnp.split(mod_x, N_MOD, axis=-1)
        csh1, csc1, cg1, csh2, csc2, cg2 = jnp.split(mod_c, N_MOD, axis=-1)

        hx = modulate(rmsnorm(x, norm1_g[layer]), sh1, sc1)
        hc = modulate(rmsnorm(ctx, norm1_g[layer]), csh1, csc1)
        zx = hx @ w_in[layer]
        px = zx[..., :D_POOL]
        qx = split_heads(zx[..., D_POOL:D_POOL + D_ATTN])
        kx = split_heads(zx[..., D_POOL + D_ATTN:D_POOL + 2 * D_ATTN])
        vx = split_heads(zx[..., D_POOL + 2 * D_ATTN:])
        zc_kv = hc @ w_in[layer][:, kv_cols]
        kc = split_heads(zc_kv[..., :D_ATTN])
        vc = split_heads(zc_kv[..., D_ATTN:])

        pool_x = multiscale_pool(px, pool_w[layer], pool_scale[layer])
        attn_x = neighbourhood_attention(qx, kx, vx, kc, vc, na_rpb[layer])
        x = x + g1 * (jnp.concatenate([pool_x, attn_x], axis=-1) @ w_out[layer])

        if not last:
            zc_pq = hc @ w_in[layer][:, :D_POOL + D_ATTN]
            pool_c = multiscale_pool(zc_pq[..., :D_POOL], pool_w[layer], pool_scale[layer])
            attn_c = context_attention(split_heads(zc_pq[..., D_POOL:]), kc, vc)
            ctx = ctx + cg1 * (jnp.concatenate([pool_c, attn_c], axis=-1) @ w_out[layer])

        h2 = modulate(rmsnorm(x, norm2_g[layer]), sh2, sc2)
        x = x + g2 * peer(h2, peer_wq[layer], peer_keys[layer], peer_u[layer], peer_v[layer])
        if not last:
            h2c = modulate(rmsnorm(ctx, norm2_g[layer]), csh2, csc2)
            ctx = ctx + cg2 * peer(h2c, peer_wq[layer], peer_keys[layer], peer_u[layer], peer_v[layer])

    return rmsnorm(x, final_g)
```

```python
import numpy as np
from contextlib import ExitStack
import concourse.bass as bass
import concourse.mybir as mybir
from concourse.bass_utils import run_bass_kernel_spmd

F32 = mybir.dt.float32
BF16 = mybir.dt.bfloat16
U32 = mybir.dt.uint32
I32 = mybir.dt.int32
AF = mybir.ActivationFunctionType
ALU = mybir.AluOpType
AX = mybir.AxisListType

D = 1024
SEQ = 4096
NT = SEQ // 128
NEG = -30000.0
EPS = 1e-6


class Buf:
    __slots__ = ("w", "r", "name")

    def __init__(self, name=""):
        self.w = None
        self.r = []
        self.name = name


class Sched:
    ENG = ("pe", "act", "dve", "pool", "sp")

    def __init__(self, nc, es, n_dma_sems=40):
        self.nc = nc
        self.streams = {e: [] for e in self.ENG}
        self.sem = {e: es.enter_context(nc.semaphore("s_" + e)) for e in self.ENG}
        self.cnt = {e: 0 for e in self.ENG}
        self.waited = {e: {} for e in self.ENG}
        self.dq = {"sp": list(range(0, 26)), "pool": list(range(26, 40)), "conv": list(range(40, 52))}
        n_dma_sems = 52
        self.dsem = [es.enter_context(nc.semaphore("s_dma%d" % i)) for i in range(n_dma_sems)]
        self.duse = [0] * n_dma_sems
        self.drr = {"sp": 0, "pool": 0, "conv": 0}
        self.out_toks = []

    def _waits(self, eng, reads, writes):
        waits = {}

        def need(tok):
            if tok is None:
                return
            sem, val, st = tok
            if st == "pe" and eng == "pe":
                return
            if self.waited[eng].get(id(sem), (None, 0))[1] >= val:
                return
            if id(sem) not in waits or waits[id(sem)][1] < val:
                waits[id(sem)] = (sem, val)

        for b in reads:
            need(b.w)
        for b in writes:
            need(b.w)
            for t in b.r:
                need(t)
        for k, v in waits.items():
            self.waited[eng][k] = v
        return list(waits.values())

    def _commit(self, tok, reads, writes):
        for b in reads:
            b.r.append(tok)
        for b in writes:
            b.w = tok
            b.r = []

    def add(self, eng, fn, reads=(), writes=(), inc=True):
        waits = self._waits(eng, reads, writes)
        sem = self.sem[eng]
        if inc:
            self.cnt[eng] += 1
            n = self.cnt[eng]
            tok = (sem, n, eng)
        else:
            assert eng == "pe"
            tok = (sem, self.cnt[eng] + 1, eng)
        assert self.cnt[eng] < 60000

        def th(e, waits=waits, fn=fn, inc=inc, sem=sem):
            for s, v in waits:
                e.wait_ge(s, v)
            ins = getattr(e, fn[0])(*fn[1], **fn[2])
            if inc:
                ins.then_inc(sem, 1)

        self.streams[eng].append(th)
        self._commit(tok, reads, writes)

    def dma(self, eng, out, in_, reads=(), writes=(), is_output=False, slow=False, qname=None):
        waits = self._waits(eng, reads, writes)
        qname = qname or eng
        q = self.dq[qname]
        k = q[self.drr[qname] % len(q)]
        self.drr[qname] += 1
        sem = self.dsem[k]
        prev = 16 * self.duse[k]
        if prev > 0 and self.waited[eng].get(id(sem), (None, 0))[1] < prev:
            self.waited[eng][id(sem)] = (sem, prev)
            waits = [w for w in waits if w[0] is not sem] + [(sem, prev)]
        self.duse[k] += 1
        tok = (sem, 16 * self.duse[k], "dma")

        def th(e, waits=waits, sem=sem, out=out, in_=in_, slow=slow):
            for s, v in waits:
                e.wait_ge(s, v)
            if slow:
                e.dma_start(out=out, in_=in_, allow_slow_non_contiguous=True).then_inc(sem, 16)
            else:
                e.dma_start(out=out, in_=in_).then_inc(sem, 16)

        self.streams[eng].append(th)
        self._commit(tok, reads, writes)
        if is_output:
            self.out_toks.append(tok)

    def barrier(self):
        toks = [(self.sem[e], self.cnt[e]) for e in self.ENG if self.cnt[e] > 0]
        toks += [(self.dsem[k], 16 * self.duse[k]) for k in range(len(self.dsem)) if self.duse[k] > 0 and k not in self.dq["conv"]]
        for eng in self.ENG:
            waits = []
            for s, v in toks:
                if self.waited[eng].get(id(s), (None, 0))[1] >= v:
                    continue
                self.waited[eng][id(s)] = (s, v)
                waits.append((s, v))

            def th(e, waits=waits):
                for s, v in waits:
                    e.wait_ge(s, v)

            self.streams[eng].append(th)

    def finish(self):
        final = {}
        for sem, val, _ in self.out_toks:
            if id(sem) not in final or final[id(sem)][1] < val:
                final[id(sem)] = (sem, val)
        fl = list(final.values())

        def th(e):
            for s, v in fl:
                e.wait_ge(s, v)

        self.streams["sp"].append(th)

    def replay(self, blk):
        st = self.streams

        @blk.sync
        def _(e):
            for th in st["sp"]:
                th(e)

        @blk.tensor
        def _(e):
            for th in st["pe"]:
                th(e)

        @blk.scalar
        def _(e):
            for th in st["act"]:
                th(e)

        @blk.vector
        def _(e):
            for th in st["dve"]:
                th(e)

        @blk.gpsimd
        def _(e):
            for th in st["pool"]:
                th(e)


def I(_opname, *a, **k):
    return (_opname, a, k)


def V(ap, off, dims):
    return bass.AP(ap.tensor, ap.offset + off, [list(ap.ap[0])] + [list(d) for d in dims])


def _consts():
    c = {}
    p = np.arange(128)
    qc = p % 64
    rho = p // 64
    kc = np.arange(64)
    c0 = np.clip(qc - 8, 0, 48)
    colmask = np.where((kc[None, :] >= c0[:, None]) & (kc[None, :] < c0[:, None] + 16), 0.0, NEG)
    c["colmask"] = colmask.astype(np.float32)
    rm = np.zeros((3, 128, 9, 64), np.float32)
    rm[0, :, 8, :] = NEG
    rm[1, :64, 8, :] = NEG
    rm[1, 64:, 0, :] = NEG
    rm[2, :, 0, :] = NEG
    c["rowmask"] = rm.reshape(3, 128, 576)
    c["ident"] = np.eye(128, dtype=np.float32)
    L = SEQ
    MT = np.zeros((5, 4, 128, 128), np.float32)

    def Mfull(w, i, rel):
        t = i * 128 + np.arange(128)
        s = (i + rel) * 128 + np.arange(128)
        lo = np.clip(t - w // 2, 0, L)
        hi = np.clip(t + w // 2, 0, L)
        cnt = (hi - lo).astype(np.float64)
        m = ((s[None, :] >= lo[:, None]) & (s[None, :] < hi[:, None])) / cnt[:, None] - (s[None, :] == t[:, None])
        return m.T

    for g, w in enumerate((2, 4, 8, 16)):
        MT[0, g] = Mfull(w, 5, 0)
        MT[1, g] = Mfull(w, 5, -1)
        MT[2, g] = Mfull(w, 5, 1)
        MT[3, g] = Mfull(w, 0, 0)
        MT[4, g] = Mfull(w, NT - 1, 0)
    c["MT"] = np.ascontiguousarray(MT.transpose(2, 0, 1, 3)).reshape(128, 20 * 128).astype(np.float32)
    c["iota16"] = np.tile(np.arange(16, dtype=np.float32)[None, :], (128, 1))
    c["iota128"] = np.tile(np.arange(128, dtype=np.float32)[None, :], (128, 1))
    return c


def _tile_geom(i):
    if i == 0:
        return 0, 0
    if i == 1:
        return 0, -2
    if i <= 29:
        return 1, -4
    if i == 30:
        return 2, -5
    return 2, -7


def build_nc(dbg=(), stop_after=None):
    nc = bass.Bass("TRN2", target_bir_lowering=False)

    def din(name, shape, dt=F32):
        return nc.dram_tensor(name, list(shape), dt, kind="ExternalInput").ap()

    def dscr(name, shape, dt):
        kind = "ExternalOutput" if name in dbg else "Internal"
        return nc.dram_tensor(name, list(shape), dt, kind=kind).ap()

    x_d = din("x", [SEQ, D])
    ctx_d = din("ctx", [256, D])
    cc_d = din("cc", [128, 16])
    adaw_d = din("ada_w", [D, 6 * D])
    adab_d = din("ada_b", [128, 48])
    adabrow_d = din("ada_brow", [1, 6 * D])
    g1n_d = din("g1n", [128, 8])
    g2n_d = din("g2n", [128, 8])
    fg_d = din("fg", [1, D])
    win_d = din("w_in", [D, 2048])
    poolw_d = din("pool_w", [4, 128, 128])
    pscale_d = din("pscale", [128, 4])
    tt_d = din("tt", [128, 8 * 17 * 64])
    wout_d = din("w_out", [D, D])
    wq_d = din("peer_wq", [D, 2048])
    keysT_d = din("keysT", [128, 16 * 128])
    uT_d = din("uT", [128 * 128, 1024])
    vv_d = din("vv", [128 * 128, 1024])
    colmask_d = din("colmask", [128, 64])
    rowmask_d = din("rowmask", [3, 128, 576])
    ident_d = din("ident", [128, 128])
    MT_d = din("MT", [128, 20 * 128])
    iota16_d = din("iota16", [128, 16])
    iota128_d = din("iota128", [128, 128])

    out_d = nc.dram_tensor("out", [SEQ, D], F32, kind="ExternalOutput").ap()

    gvec_d = dscr("gvec", [2, D], F32)
    qT_d = dscr("qT_scr", [128, 4, SEQ], BF16)
    kT_d = dscr("kT_scr", [128, 4, SEQ], BF16)
    p_d = dscr("p_scr", [SEQ, 512], BF16)
    v_d = dscr("v_scr", [SEQ, 512], BF16)
    x1_d = dscr("x1_scr", [SEQ, D], F32)
    h2T_d = dscr("h2T_scr", [128, 8, SEQ], BF16)
    rt_d = dscr("rt_scr", [128, 3, SEQ], BF16)
    uTb_d = dscr("uTb_scr", [128 * 128, 1024], BF16)
    vb_d = dscr("vb_scr", [128 * 128, 1024], BF16)

    es = ExitStack()
    with es:
        S = Sched(nc, es)

        def sb(st, name, shape, dt):
            return st.enter_context(nc.sbuf_tensor("sb_" + name, list(shape), dt))

        def ps(st, name, shape, dt=F32):
            return st.enter_context(nc.psum_tensor("ps_" + name, list(shape), dt))

        ident = sb(es, "ident", [128, 128], BF16)
        identf = sb(es, "identf", [128, 128], F32)
        mod = sb(es, "mod", [128, 48, 2], F32)
        gsc1 = sb(es, "gsc1", [128, 8], F32)
        gsc1c = sb(es, "gsc1c", [128, 8], F32)
        gsc2 = sb(es, "gsc2", [128, 8], F32)
        g1n = sb(es, "g1n", [128, 8], F32)
        g2n = sb(es, "g2n", [128, 8], F32)
        g1row = sb(es, "g1row", [128, D], F32)
        g2row = sb(es, "g2row", [128, D], F32)
        fgrow = sb(es, "fgrow", [128, D], F32)
        B_const = Buf("const")
        B_mod = Buf("mod")

        def rmsnorm_tile(xt, Bx, junk, Bjunk, ss, sq, rstd, Bst, xn, Bxn):
            S.add("act", I("activation", out=junk[:], in_=xt, func=AF.Square, accum_out=ss[:]),
                  reads=[Bx], writes=[Bjunk, Bst])
            S.add("act", I("activation", out=sq[:], in_=ss[:], func=AF.Sqrt, scale=1.0 / D, bias=epsc[:]),
                  reads=[B_const], writes=[Bst])
            S.add("dve", I("reciprocal", out=rstd[:], in_=sq[:]), reads=[], writes=[Bst])
            S.add("act", I("activation", out=xn[:], in_=xt, func=AF.Copy, scale=rstd[:]),
                  reads=[Bx, Bst], writes=[Bxn])

        epsc = sb(es, "epsc", [128, 1], F32)

        with ExitStack() as p0:
            cc = sb(p0, "cc", [128, 8, 2], F32)
            scc = sb(p0, "scc", [128, 8, 2], F32)
            adab = sb(p0, "adab", [128, 48], F32)
            awr = [sb(p0, "awr%d" % i, [128, 8, 512], F32) for i in range(2)]
            Bawr = [Buf(), Buf()]
            row_ps = [ps(p0, "row_ps%d" % i, [128, 512], F32) for i in range(4)]
            Brow = [Buf() for _ in range(4)]
            mod_ps = ps(p0, "mod_ps", [128, 48, 2], F32)
            Bmodps = Buf()
            tmp8 = sb(p0, "tmp8", [128, 8], F32)
            identtmp = sb(p0, "identtmp", [128, 128], F32)
            Bcc = Buf()

            S.add("dve", I("memset", epsc[:], EPS), writes=[B_const])
            S.dma("sp", cc[:].rearrange("p k t -> p (k t)"), cc_d, writes=[Bcc])
            S.dma("sp", adab[:], adab_d, writes=[Bcc])
            S.dma("sp", g1n[:], g1n_d, writes=[Bcc])
            S.dma("sp", g2n[:], g2n_d, writes=[Bcc])
            S.dma("sp", identf[:], ident_d, writes=[B_const])
            S.dma("sp", fgrow[:], fg_d[0:1, :].partition_broadcast(128), writes=[B_const])
            S.add("dve", I("tensor_copy", out=ident[:], in_=identf[:]), reads=[], writes=[B_const])
            S.add("act", I("activation", out=scc[:], in_=cc[:], func=AF.Silu), reads=[Bcc], writes=[Bcc])
            modrow = sb(p0, "modrow", [2, 6 * D], F32)
            Bmr = Buf()
            for cb in range(12):
                q = cb % 2
                S.dma("sp", awr[q][:], adaw_d[:, cb * 512:(cb + 1) * 512].rearrange("(k p) n -> p k n", p=128), writes=[Bawr[q]])
                for k in range(8):
                    S.add("pe", I("matmul", row_ps[cb % 4][0:2, :], lhsT=scc[:, k, :], rhs=awr[q][:, k, :], start=(k == 0), stop=(k == 7)),
                          reads=[Bawr[q], Bcc], writes=[Brow[cb % 4]], inc=(k == 7))
                S.add("act", I("activation", out=modrow[:, cb * 512:(cb + 1) * 512], in_=row_ps[cb % 4][0:2, :], func=AF.Copy),
                      reads=[Brow[cb % 4]], writes=[Bmr])
            for m in range(48):
                S.add("pe", I("transpose", out=mod_ps[:, m, :], in_=modrow[:, m * 128:(m + 1) * 128], identity=identf[0:2, 0:2]),
                      reads=[Bmr, B_const], writes=[Bmodps], inc=(m == 47))
            S.add("dve", I("tensor_tensor", out=mod[:], in0=mod_ps[:], in1=V(adab[:], 0, [[1, 48], [0, 2]]), op=ALU.add),
                  reads=[Bmodps, Bcc], writes=[B_mod])

            def mk_gsc(dst, gn, lo, col):
                S.add("dve", I("tensor_scalar", out=tmp8[:], in0=mod[:, lo:lo + 8, col], scalar1=1.0, scalar2=None, op0=ALU.add),
                      reads=[B_mod], writes=[Bcc])
                S.add("dve", I("tensor_tensor", out=dst[:], in0=tmp8[:], in1=gn[:], op=ALU.mult),
                      reads=[Bcc], writes=[B_mod])

            mk_gsc(gsc1, g1n, 8, 0)
            mk_gsc(gsc1c, g1n, 8, 1)
            mk_gsc(gsc2, g2n, 32, 0)
            abrow = sb(p0, "abrow", [128, 2, D], F32)
            Bab = Buf()
            onesr = sb(p0, "onesr", [1, 128], F32)
            S.add("dve", I("memset", onesr[:], 1.0), writes=[Bab])
            S.dma("sp", abrow[:, 0, :], adabrow_d[0:1, 2048:3072].partition_broadcast(128), writes=[Bab])
            S.dma("sp", abrow[:, 1, :], adabrow_d[0:1, 5120:6144].partition_broadcast(128), writes=[Bab])
            for gi, (col0, dst) in enumerate(((2048, g1row), (5120, g2row))):
                for hf in range(2):
                    q = (gi * 2 + hf) % 4
                    c0_ = col0 + hf * 512
                    S.add("pe", I("matmul", row_ps[q][:], lhsT=onesr[:], rhs=modrow[0:1, c0_:c0_ + 512], start=True, stop=True),
                          reads=[Bmr, Bab], writes=[Brow[q]], inc=True)
                    S.add("dve", I("tensor_tensor", out=dst[:, hf * 512:(hf + 1) * 512], in0=row_ps[q][:], in1=abrow[:, gi, hf * 512:(hf + 1) * 512], op=ALU.add),
                          reads=[Brow[q], Bab], writes=[B_const])

        S.barrier()
        if stop_after == 0:
            S.dma("sp", out_d[0:128, 0:96].rearrange("p (a b) -> p a b", b=2), mod[:], reads=[B_mod], writes=[Buf()], is_output=True, slow=True)
            S.dma("sp", out_d[128:256, :], g1row[:], reads=[B_const], writes=[Buf()], is_output=True)
            S.dma("sp", out_d[256:384, 0:8], gsc1[:], reads=[B_mod], writes=[Buf()], is_output=True)
            S.finish()
            with nc.Block() as blk:
                S.replay(blk)
            return nc

        rr = {"act": 0}

        def evac(out, in_, reads, writes, scale=None):
            rr["act"] ^= 1
            if rr["act"]:
                if scale is None:
                    S.add("act", I("activation", out=out, in_=in_, func=AF.Copy), reads=reads, writes=writes)
                else:
                    S.add("act", I("activation", out=out, in_=in_, func=AF.Copy, scale=float(scale)), reads=reads, writes=writes)
            else:
                if scale is None:
                    S.add("dve", I("tensor_copy", out=out, in_=in_), reads=reads, writes=writes)
                else:
                    S.add("dve", I("tensor_scalar", out=out, in0=in_, scalar1=float(scale), scalar2=None, op0=ALU.mult), reads=reads, writes=writes)

        Bu = [Buf() for _ in range(32)]
        Bvb = [Buf() for _ in range(32)]
        def convert(jb0, jb1):
            for jb in range(jb0, jb1):
                S.dma("pool", uTb_d[jb * 512:(jb + 1) * 512, :].rearrange("(p j) n -> j p n", j=4),
                      uT_d[jb * 512:(jb + 1) * 512, :].rearrange("(j p) n -> j p n", p=128), writes=[Bu[jb]], qname="conv")
                S.dma("pool", vb_d[jb * 512:(jb + 1) * 512, :].rearrange("(p j) n -> j p n", j=4),
                      vv_d[jb * 512:(jb + 1) * 512, :].rearrange("(j p) n -> j p n", p=128), writes=[Bvb[jb]], qname="conv")

        pA = ExitStack()
        es.enter_context(pA)
        kcT = sb(pA, "kcT", [128, 4, 256], BF16)
        vc = sb(pA, "vc", [128, 2, 512], BF16)
        B_ckv = Buf()
        Bq = [Buf() for _ in range(8)]
        Bk = [Buf() for _ in range(8)]
        Bp = [Buf() for _ in range(8)]
        Bv = [Buf() for _ in range(8)]
        Bx1 = [Buf() for _ in range(NT)]

        with ExitStack() as p01:
            w_in = sb(p01, "w_in", [128, 8, 2048], BF16)
            B_win = Buf()
            for k in range(8):
                S.dma("pool", w_in[:, k, :], win_d[k * 128:(k + 1) * 128, :], writes=[B_win])
            convert(0, 8)
            xin = [sb(p01, "xin%d" % i, [128, D], F32) for i in range(2)]
            Bxin = [Buf(), Buf()]
            junk = sb(p01, "junk", [128, D], BF16)
            Bjunk = Buf()
            xn = [sb(p01, "xn%d" % i, [128, D], BF16) for i in range(2)]
            Bxn = [Buf(), Buf()]
            st_ss = [sb(p01, "ss%d" % i, [128, 1], F32) for i in range(2)]
            st_sq = [sb(p01, "sq%d" % i, [128, 1], F32) for i in range(2)]
            st_rs = [sb(p01, "rs%d" % i, [128, 1], F32) for i in range(2)]
            Bst = [Buf(), Buf()]
            psT = [ps(p01, "psT%d" % i, [128, 8, 128], BF16) for i in range(2)]
            BpsT = [Buf(), Buf()]
            z_ps = [ps(p01, "z_ps%d" % i, [128, 512], F32) for i in range(3)]
            Bz = [Buf() for _ in range(3)]
            zc = {"i": 0}

            def norm_T(src_ap, slot, dstT, col0, Bdst, gs, shcol):
                S.dma("sp", xin[slot][:], src_ap, writes=[Bxin[slot]])
                rmsnorm_tile(xin[slot][:], Bxin[slot], junk, Bjunk, st_ss[slot], st_sq[slot], st_rs[slot], Bst[slot], xn[slot], Bxn[slot])
                for k in range(8):
                    S.add("pe", I("transpose", out=psT[slot][:, k, :], in_=xn[slot][:, k * 128:(k + 1) * 128], identity=ident[:]),
                          reads=[Bxn[slot], B_const], writes=[BpsT[slot]], inc=(k == 7))
                for k in range(8):
                    S.add("dve", I("tensor_scalar", out=dstT[:, k, col0:col0 + 128], in0=psT[slot][:, k, :],
                                                                 scalar1=gs[:, k:k + 1], scalar2=mod[:, k + shcol[0], shcol[1]:shcol[1] + 1],
                                                                 op0=ALU.mult, op1=ALU.add),
                          reads=[BpsT[slot], B_mod], writes=[Bdst])

            def proj(out_ps_i, lhs_fn, rhs_fn, reads):
                zi = zc["i"] % 3
                zc["i"] += 1
                for k in range(8):
                    S.add("pe", I("matmul", z_ps[zi][:, 0:out_ps_i], lhsT=lhs_fn(k), rhs=rhs_fn(k), start=(k == 0), stop=(k == 7)),
                          reads=reads, writes=[Bz[zi]], inc=(k == 7))
                return zi

            with ExitStack() as p0b:
                hcT = sb(p0b, "hcT", [128, 8, 256], BF16)
                BhcT = Buf()
                for tl in range(2):
                    norm_T(ctx_d[tl * 128:(tl + 1) * 128, :], tl, hcT, tl * 128, BhcT, gsc1c, (0, 1))
                for j in range(4):
                    zi = proj(256, lambda k, j=j: w_in[:, k, 1024 + j * 128:1024 + (j + 1) * 128], lambda k: hcT[:, k, :], [B_win, BhcT])
                    evac(kcT[:, j, :], z_ps[zi][:, 0:256], [Bz[zi]], [B_ckv])
                for tl in range(2):
                    zi = proj(512, lambda k, tl=tl: hcT[:, k, tl * 128:(tl + 1) * 128], lambda k: w_in[:, k, 1536:2048], [B_win, BhcT])
                    evac(vc[:, tl, :], z_ps[zi][:, :], [Bz[zi]], [B_ckv])

            if stop_after != 1:
                S.barrier()
            if stop_after == 1:
                dbg_d = nc.dram_tensor("dbg0", [128, 4 * 256 + 2 * 512], BF16, kind="ExternalOutput").ap()
                S.dma("sp", dbg_d[:, 0:1024], kcT[:].rearrange("p a b -> p (a b)"), reads=[B_ckv], writes=[Buf()], is_output=True)
                S.dma("sp", dbg_d[:, 1024:2048], vc[:].rearrange("p a b -> p (a b)"), reads=[B_ckv], writes=[Buf()], is_output=True)
                dbg1_d = nc.dram_tensor("dbg1", [128, 4096], BF16, kind="ExternalOutput").ap()
                S.dma("sp", dbg1_d[:, 0:1024], w_in[:, 0, 0:1024], reads=[B_win], writes=[Buf()], is_output=True)
                S.dma("sp", dbg1_d[:, 1024:3072], hcT[:].rearrange("p a b -> p (a b)"), reads=[BhcT], writes=[Buf()], is_output=True)
                S.dma("sp", dbg1_d[:, 3072:4096], xn[1][:], reads=[Bxn[1]], writes=[Buf()], is_output=True)
                for qi, tl_ in enumerate((st_ss[1], st_sq[1], st_rs[1])):
                    dq = nc.dram_tensor("dbg2_%d" % qi, [128, 1], F32, kind="ExternalOutput").ap()
                    S.dma("sp", dq, tl_[:], reads=[Bst[1]], writes=[Buf()], is_output=True)
                S.finish()
                with nc.Block() as blk:
                    S.replay(blk)
                return nc

            hT = [sb(p01, "hT%d" % i, [128, 8, 512], BF16) for i in range(2)]
            BhT = [Buf(), Buf()]
            qTs = [sb(p01, "qTs%d" % i, [128, 4, 512], BF16) for i in range(2)]
            kTs = [sb(p01, "kTs%d" % i, [128, 4, 512], BF16) for i in range(2)]
            pss = [sb(p01, "pss%d" % i, [128, 4, 512], BF16) for i in range(2)]
            vss = [sb(p01, "vss%d" % i, [128, 4, 512], BF16) for i in range(2)]
            Bqs = [Buf(), Buf()]
            Bks = [Buf(), Buf()]
            Bpss = [Buf(), Buf()]
            Bvss = [Buf(), Buf()]
            pend = []
            for g in range(8):
                gs_ = g % 2
                for tl in range(4):
                    ti = g * 4 + tl
                    norm_T(x_d[ti * 128:(ti + 1) * 128, :], ti % 2, hT[gs_], tl * 128, BhT[gs_], gsc1, (0, 0))
                    if tl == 1:
                        for f in pend:
                            f()
                        pend = []
                for j in range(4):
                    zi = proj(512, lambda k, j=j: w_in[:, k, 512 + j * 128:512 + (j + 1) * 128], lambda k: hT[gs_][:, k, :], [B_win, BhT[gs_]])
                    evac(qTs[gs_][:, j, :], z_ps[zi][:, :], [Bz[zi]], [Bqs[gs_]], scale=0.125)
                for j in range(4):
                    zi = proj(512, lambda k, j=j: w_in[:, k, 1024 + j * 128:1024 + (j + 1) * 128], lambda k: hT[gs_][:, k, :], [B_win, BhT[gs_]])
                    evac(kTs[gs_][:, j, :], z_ps[zi][:, :], [Bz[zi]], [Bks[gs_]])
                for tl in range(4):
                    zi = proj(512, lambda k, tl=tl: hT[gs_][:, k, tl * 128:(tl + 1) * 128], lambda k: w_in[:, k, 0:512], [B_win, BhT[gs_]])
                    evac(pss[gs_][:, tl, :], z_ps[zi][:, :], [Bz[zi]], [Bpss[gs_]])
                    zi = proj(512, lambda k, tl=tl: hT[gs_][:, k, tl * 128:(tl + 1) * 128], lambda k: w_in[:, k, 1536:2048], [B_win, BhT[gs_]])
                    evac(vss[gs_][:, tl, :], z_ps[zi][:, :], [Bz[zi]], [Bvss[gs_]])
                def stores(g=g, gs_=gs_):
                    S.dma("sp", qT_d[:, :, g * 512:(g + 1) * 512], qTs[gs_][:], reads=[Bqs[gs_]], writes=[Bq[g]])
                    S.dma("sp", kT_d[:, :, g * 512:(g + 1) * 512], kTs[gs_][:], reads=[Bks[gs_]], writes=[Bk[g]])
                    S.dma("sp", p_d[g * 512:(g + 1) * 512, :].rearrange("(t p) n -> p t n", p=128), pss[gs_][:], reads=[Bpss[gs_]], writes=[Bp[g]])
                    S.dma("sp", v_d[g * 512:(g + 1) * 512, :].rearrange("(t p) n -> p t n", p=128), vss[gs_][:], reads=[Bvss[gs_]], writes=[Bv[g]])
                pend.append(stores)
            for f in pend:
                f()

        S.barrier()
        if stop_after == 2:
            S.dma("sp", out_d[0:128, 0:8], gsc1[:], reads=[Bq[7], Bk[7], Bp[7], Bv[7]] + Bq + Bk + Bp + Bv, writes=[Buf()], is_output=True)
            S.finish()
            with nc.Block() as blk:
                S.replay(blk)
            return nc

        with ExitStack() as p2:
            ttf = sb(p2, "ttf", [128, 8 * 17 * 64], F32)
            cmask = sb(p2, "cmask", [128, 64], F32)
            TTb = sb(p2, "TTb", [128, 8, 17, 64], BF16)
            rmask = sb(p2, "rmask", [128, 3, 576], BF16)
            MTs = sb(p2, "MTs", [128, 20, 128], BF16)
            poolw = sb(p2, "poolw", [128, 4, 128], BF16)
            pscale = sb(p2, "pscale", [128, 4], F32)
            w_out = sb(p2, "w_out", [128, 8, D], BF16)
            B_c2 = Buf()
            Btt = Buf()
            S.dma("sp", ttf[:], tt_d, writes=[Btt])
            S.dma("sp", cmask[:], colmask_d, writes=[Btt])
            S.dma("sp", pscale[:], pscale_d, writes=[B_c2])
            for a in range(3):
                S.dma("pool", rmask[:, a, :], rowmask_d[a], writes=[B_c2])
            S.dma("pool", MTs[:].rearrange("p a b -> p (a b)"), MT_d, writes=[B_c2])
            for g in range(4):
                S.dma("pool", poolw[:, g, :], poolw_d[g], writes=[B_c2])
            for k in range(8):
                S.dma("pool", w_out[:, k, :], wout_d[k * 128:(k + 1) * 128, :], writes=[B_c2])
            convert(8, 20)
            S.add("dve", I("tensor_tensor", out=TTb[:].rearrange("p h m c -> p (h m) c"),
                                                   in0=ttf[:].rearrange("p (a c) -> p a c", c=64),
                                                   in1=V(cmask[:], 0, [[0, 136], [1, 64]]), op=ALU.add),
                  reads=[Btt], writes=[B_c2])

            TTi = sb(p2, "TTi", [128, 8, 9, 64], BF16)
            S.add("dve", I("tensor_tensor", out=TTi[:].rearrange("p h m c -> p h (m c)"), in0=TTb[:, :, 4:13, :].rearrange("p h m c -> p h (m c)"),
                           in1=V(rmask[:], 576, [[0, 8], [1, 576]]), op=ALU.add),
                  reads=[B_c2], writes=[B_c2])
            QT = [sb(p2, "QT%d" % i, [128, 4, 128], BF16) for i in range(2)]
            KT = [sb(p2, "KT%d" % i, [128, 4, 576], BF16) for i in range(2)]
            Vt = [sb(p2, "Vt%d" % i, [128, 5, 512], BF16) for i in range(2)]
            Pt = [sb(p2, "Pt%d" % i, [128, 3, 512], BF16) for i in range(2)]
            xr = [sb(p2, "xr%d" % i, [128, D], F32) for i in range(2)]
            Bld = [Buf(), Buf()]
            Bxr = [Buf(), Buf()]
            S_ps = [ps(p2, "S_ps%d" % i, [128, 1024], F32) for i in range(2)]
            BS = [Buf(), Buf()]
            ET_ps = ps(p2, "ET_ps", [128, 7, 128], BF16)
            BETp = Buf()
            O_ps = ps(p2, "O_ps", [128, 512], F32)
            BO = Buf()
            pl_ps = ps(p2, "pl_ps", [128, 4, 128], F32)
            Bpl = Buf()
            out_ps = ps(p2, "out_ps", [128, 512], F32)
            Bout = Buf()
            E_sb = [sb(p2, "E_sb%d" % i, [128, 832], BF16) for i in range(2)]
            BE = [Buf(), Buf()]
            ET_sb = [sb(p2, "ET_sb%d" % i, [128, 7, 128], BF16) for i in range(2)]
            BET = [Buf(), Buf()]
            nmx = [sb(p2, "nmx%d" % i, [128, 1], F32) for i in range(2)]
            Bnmx = [Buf(), Buf()]
            rsum = [sb(p2, "rsum%d" % i, [128, 8], F32) for i in range(2)]
            rinv = [sb(p2, "rinv%d" % i, [128, 8], F32) for i in range(2)]
            Brs = [Buf(), Buf()]
            attn = sb(p2, "attn", [128, 512], BF16)
            Battn = Buf()
            pooledT = sb(p2, "pooledT", [128, 4, 128], BF16)
            Bpooled = Buf()
            mixT = [sb(p2, "mixT%d" % i, [128, 8, 128], BF16) for i in range(2)]
            Bmix = [Buf(), Buf()]
            t1 = sb(p2, "t1", [128, D], F32)
            Bt1 = Buf()
            x1s = [sb(p2, "x1s%d" % i, [128, D], F32) for i in range(2)]
            Bx1s = [Buf(), Buf()]

            def grp_range(lo_tok, hi_tok, arr):
                return [arr[g] for g in range(lo_tok // 512, (hi_tok - 1) // 512 + 1)]

            Bqt = [Buf(), Buf()]
            Bkt = [Buf(), Buf()]
            Bvta = [Buf(), Buf()]
            Bvtb = [Buf(), Buf()]
            Bpt = [Buf(), Buf()]

            def loads2(i):
                sl = i % 2
                mtype, off = _tile_geom(i)
                base = 2 * i + off
                kt0 = base * 64
                S.dma("sp", QT[sl][:], qT_d[:, :, i * 128:(i + 1) * 128], reads=[Bq[i // 4]], writes=[Bqt[sl]])
                S.dma("sp", KT[sl][:], kT_d[:, :, kt0:kt0 + 576], reads=grp_range(kt0, kt0 + 576, Bk), writes=[Bkt[sl]])
                S.dma("sp", Vt[sl][:, 0:4, :], v_d[kt0:kt0 + 512, :].rearrange("(c p) d -> p c d", p=128),
                      reads=grp_range(kt0, kt0 + 576, Bv), writes=[Bvta[sl]])
                S.dma("sp", Vt[sl][0:64, 4, :], v_d[kt0 + 512:kt0 + 576, :], reads=grp_range(kt0, kt0 + 576, Bv), writes=[Bvtb[sl]])
                plo = max(i - 1, 0)
                phi = min(i + 1, NT - 1)
                S.dma("sp", Pt[sl][:, plo - (i - 1):phi - (i - 1) + 1, :],
                      p_d[plo * 128:(phi + 1) * 128, :].rearrange("(c p) d -> p c d", p=128),
                      reads=grp_range(plo * 128, (phi + 1) * 128, Bp), writes=[Bpt[sl]])
                S.dma("sp", xr[sl][:], x_d[i * 128:(i + 1) * 128, :], writes=[Bxr[sl]])

            loads2(0)
            for i in range(NT):
                sl = i % 2
                mtype, off = _tile_geom(i)
                base = 2 * i + off
                m0 = off + 8
                kt0 = base * 64
                if i + 1 < NT:
                    loads2(i + 1)
                cols = [(0, 128), (128, 128), (256, 128), (384, 128), (576, 128), (704, 128), (512, 64)]

                def emit_qk(h):
                    j = h // 2
                    po = (h % 2) * 64
                    hs = h % 2
                    q_ap = QT[sl][po:po + 64, j, :]
                    rd = [Bqt[sl], Bkt[sl], B_c2, B_const, B_ckv]
                    Sp = S_ps[hs]
                    S.add("pe", I("matmul", Sp[:, 0:512], lhsT=q_ap, rhs=KT[sl][po:po + 64, j, 0:512], start=True, stop=False),
                          reads=rd, writes=[BS[hs]], inc=False)
                    if mtype == 1:
                        S.add("pe", I("matmul", Sp[:, 0:512], lhsT=ident[:], rhs=TTi[:, h, 0:8, :].rearrange("p a b -> p (a b)"), start=False, stop=True),
                              reads=rd, writes=[BS[hs]], inc=False)
                    else:
                        S.add("pe", I("matmul", Sp[:, 0:512], lhsT=ident[:], rhs=TTb[:, h, m0:m0 + 8, :].rearrange("p a b -> p (a b)"), start=False, stop=False),
                              reads=rd, writes=[BS[hs]], inc=False)
                        S.add("pe", I("matmul", Sp[:, 0:512], lhsT=ident[:], rhs=rmask[:, mtype, 0:512], start=False, stop=True),
                              reads=rd, writes=[BS[hs]], inc=False)
                    S.add("pe", I("matmul", Sp[:, 512:576], lhsT=q_ap, rhs=KT[sl][po:po + 64, j, 512:576], start=True, stop=False),
                          reads=rd, writes=[BS[hs]], inc=False)
                    if mtype == 1:
                        S.add("pe", I("matmul", Sp[:, 512:576], lhsT=ident[:], rhs=TTi[:, h, 8, :], start=False, stop=True),
                              reads=rd, writes=[BS[hs]], inc=False)
                    else:
                        S.add("pe", I("matmul", Sp[:, 512:576], lhsT=ident[:], rhs=TTb[:, h, m0 + 8, :], start=False, stop=False),
                              reads=rd, writes=[BS[hs]], inc=False)
                        S.add("pe", I("matmul", Sp[:, 512:576], lhsT=ident[:], rhs=rmask[:, mtype, 512:576], start=False, stop=True),
                              reads=rd, writes=[BS[hs]], inc=False)
                    S.add("pe", I("matmul", Sp[:, 576:832], lhsT=q_ap, rhs=kcT[po:po + 64, j, :], start=True, stop=True),
                          reads=rd, writes=[BS[hs]], inc=True)
                    S.add("dve", I("tensor_reduce", out=nmx[hs][:], in_=Sp[:, 0:832], axis=AX.X, op=ALU.max, negate=True),
                          reads=[BS[hs]], writes=[Bnmx[hs]])
                    S.add("act", I("activation", out=E_sb[hs][:], in_=Sp[:, 0:832], func=AF.Exp, bias=nmx[hs][:], scale=1.0,
                                   accum_out=rsum[sl][:, h:h + 1]),
                          reads=[BS[hs], Bnmx[hs]], writes=[BE[hs], Brs[sl]])

                def emit_T(h):
                    hs = h % 2
                    for c, (c0_, cw) in enumerate(cols):
                        S.add("pe", I("transpose", out=ET_ps[0:cw, c, :], in_=E_sb[hs][:, c0_:c0_ + cw], identity=ident[:]),
                              reads=[BE[hs], B_const], writes=[BETp], inc=(c == 6))
                    S.add("dve", I("tensor_copy", out=ET_sb[hs][:, 0:6, :], in_=ET_ps[:, 0:6, :]), reads=[BETp], writes=[BET[hs]])
                    S.add("act", I("activation", out=ET_sb[hs][0:64, 6, :], in_=ET_ps[0:64, 6, :], func=AF.Copy), reads=[BETp], writes=[BET[hs]])

                def emit_pv(h):
                    hs = h % 2
                    vsrc = [Vt[sl][:, 0, h * 64:(h + 1) * 64], Vt[sl][:, 1, h * 64:(h + 1) * 64], Vt[sl][:, 2, h * 64:(h + 1) * 64],
                            Vt[sl][:, 3, h * 64:(h + 1) * 64], vc[:, 0, h * 64:(h + 1) * 64], vc[:, 1, h * 64:(h + 1) * 64],
                            Vt[sl][0:64, 4, h * 64:(h + 1) * 64]]
                    for c in range(7):
                        lw = 64 if c == 6 else 128
                        S.add("pe", I("matmul", O_ps[:, h * 64:(h + 1) * 64], lhsT=ET_sb[hs][0:lw, c, :], rhs=vsrc[c],
                                      start=(c == 0), stop=(c == 6)),
                              reads=[BET[hs], Bvta[sl], Bvtb[sl], B_ckv], writes=[BO], inc=(c == 6))

                emit_qk(0)
                for h in range(8):
                    if h + 1 < 8:
                        emit_qk(h + 1)
                    emit_T(h)
                    if h >= 1:
                        emit_pv(h - 1)
                emit_pv(7)
                S.add("dve", I("reciprocal", out=rinv[sl][:], in_=rsum[sl][:]), reads=[Brs[sl]], writes=[Brs[sl]])
                S.add("dve", I("tensor_tensor", out=attn[:].rearrange("p (h d) -> p h d", d=64), in0=O_ps[:].rearrange("p (h d) -> p h d", d=64),
                                                       in1=V(rinv[sl][:], 0, [[1, 8], [0, 64]]), op=ALU.mult),
                      reads=[BO, Brs[sl]], writes=[Battn])
                for c in range(4):
                    S.add("pe", I("transpose", out=ET_ps[:, c, :], in_=attn[:, c * 128:(c + 1) * 128], identity=ident[:]),
                          reads=[Battn, B_const], writes=[BETp], inc=(c == 3))
                S.add("act", I("activation", out=mixT[sl][:, 4:8, :], in_=ET_ps[:, 0:4, :], func=AF.Copy), reads=[BETp], writes=[Bmix[sl]])
                rels = []
                if i > 0:
                    rels.append((0, 1))
                rels.append((1, 3 if i == 0 else (4 if i == NT - 1 else 0)))
                if i < NT - 1:
                    rels.append((2, 2))
                for g in range(4):
                    for ri, (slot, kind) in enumerate(rels):
                        S.add("pe", I("matmul", pl_ps[:, g, :], lhsT=Pt[sl][:, slot, g * 128:(g + 1) * 128], rhs=MTs[:, kind * 4 + g, :],
                                                                                   start=(ri == 0), stop=(ri == len(rels) - 1)),
                              reads=[Bpt[sl], B_c2], writes=[Bpl], inc=(g == 3 and ri == len(rels) - 1))
                S.add("act", I("activation", out=pooledT[:], in_=pl_ps[:], func=AF.Copy), reads=[Bpl], writes=[Bpooled])
                for g in range(4):
                    S.add("pe", I("matmul", pl_ps[:, g, :], lhsT=poolw[:, g, :], rhs=pooledT[:, g, :], start=True, stop=True),
                          reads=[Bpooled, B_c2], writes=[Bpl], inc=(g == 3))
                for g in range(4):
                    S.add("dve", I("tensor_scalar", out=mixT[sl][:, g, :], in0=pl_ps[:, g, :], scalar1=pscale[:, g:g + 1], scalar2=None, op0=ALU.mult),
                          reads=[Bpl, B_c2], writes=[Bmix[sl]])
                for hf in range(2):
                    for k in range(8):
                        S.add("pe", I("matmul", out_ps[:], lhsT=mixT[sl][:, k, :], rhs=w_out[:, k, hf * 512:(hf + 1) * 512],
                                      start=(k == 0), stop=(k == 7)),
                              reads=[Bmix[sl], B_c2], writes=[Bout], inc=(k == 7))
                    S.add("dve", I("tensor_tensor", out=t1[:, hf * 512:(hf + 1) * 512], in0=out_ps[:], in1=g1row[:, hf * 512:(hf + 1) * 512], op=ALU.mult),
                          reads=[Bout, B_const], writes=[Bt1])
                S.add("dve", I("tensor_tensor", out=x1s[sl][:], in0=t1[:], in1=xr[sl][:], op=ALU.add), reads=[Bt1, Bxr[sl]], writes=[Bx1s[sl]])
                S.dma("sp", x1_d[i * 128:(i + 1) * 128, :], x1s[sl][:], reads=[Bx1s[sl]], writes=[Bx1[i]])

        S.barrier()
        if stop_after == 3:
            S.dma("sp", out_d[0:128, 0:8], gsc1[:], reads=Bx1, writes=[Buf()], is_output=True)
            S.finish()
            with nc.Block() as blk:
                S.replay(blk)
            return nc

        Bh2 = [Buf() for _ in range(NT)]
        Brt = [Buf() for _ in range(NT)]
        with ExitStack() as p3:
            wq = sb(p3, "wq", [128, 8, 2048], BF16)
            keysT = sb(p3, "keysT", [128, 16, 128], BF16)
            iota16 = sb(p3, "iota16", [128, 16], F32)
            B_c3 = Buf()
            for k in range(8):
                S.dma("pool", wq[:, k, :], wq_d[k * 128:(k + 1) * 128, :], writes=[B_c3])
            S.dma("pool", keysT[:].rearrange("p a b -> p (a b)"), keysT_d, writes=[B_c3])
            convert(20, 32)
            S.dma("sp", iota16[:], iota16_d, writes=[B_c3])
            xin = [sb(p3, "xin3_%d" % i, [128, D], F32) for i in range(2)]
            Bxin = [Buf(), Buf()]
            junk = sb(p3, "junk3", [128, D], BF16)
            Bjunk = Buf()
            xn = [sb(p3, "xn3_%d" % i, [128, D], BF16) for i in range(2)]
            Bxn = [Buf(), Buf()]
            st_ss = [sb(p3, "ss3_%d" % i, [128, 1], F32) for i in range(2)]
            st_sq = [sb(p3, "sq3_%d" % i, [128, 1], F32) for i in range(2)]
            st_rs = [sb(p3, "rs3_%d" % i, [128, 1], F32) for i in range(2)]
            Bst = [Buf(), Buf()]
            psT = [ps(p3, "psT3_%d" % i, [128, 8, 128], BF16) for i in range(2)]
            BpsT = [Buf(), Buf()]
            h2T = [sb(p3, "h2T%d" % i, [128, 8, 128], BF16) for i in range(2)]
            Bh2s = [Buf(), Buf()]
            qp_ps = ps(p3, "qp_ps", [128, 16, 128], F32)
            Bqp = [Buf() for _ in range(4)]
            rt_ps = ps(p3, "rt_ps", [128, 3, 128], F32)
            Brtp = Buf()
            qpT = sb(p3, "qpT", [128, 16, 128], BF16)
            BqpT = [Buf() for _ in range(4)]
            s_sbs = [sb(p3, "s_sb%d" % i, [128, 16, 128], F32) for i in range(2)]
            Bss = [[Buf() for _ in range(4)] for _ in range(2)]
            s2 = sb(p3, "s2", [128, 16, 128], F32)
            Bs2 = [Buf() for _ in range(16)]
            Bsva = [Buf() for _ in range(16)]
            Bsvb = [Buf() for _ in range(16)]
            Bsia = [Buf() for _ in range(16)]
            Bsib = [Buf() for _ in range(16)]
            Btva = [Buf() for _ in range(8)]
            Btvb = [Buf() for _ in range(8)]
            Btia = [Buf() for _ in range(8)]
            Btib = [Buf() for _ in range(8)]
            Bc2 = [Buf() for _ in range(8)]
            sv = sb(p3, "sv", [128, 16, 16], F32)
            si_u = sb(p3, "si_u", [128, 16, 16], U32)
            si_f = sb(p3, "si_f", [128, 16, 16], F32)
            Bsv = Buf()
            cand = sb(p3, "cand", [128, 8, 256], F32)
            cand2 = sb(p3, "cand2", [128, 8, 256], F32)
            Bcand = Buf()
            tv = sb(p3, "tv", [128, 8, 16], F32)
            tvc = sb(p3, "tvc", [128, 8, 16], F32)
            ti_u = sb(p3, "ti_u", [128, 8, 16], U32)
            ta_u = sb(p3, "ta_u", [128, 8, 16], U32)
            tb_u = sb(p3, "tb_u", [128, 8, 16], U32)
            ta_f = sb(p3, "ta_f", [128, 8, 16], F32)
            tb_f = sb(p3, "tb_f", [128, 8, 16], F32)
            Btv = Buf()
            eqb = sb(p3, "eqb", [128, 8, 16, 16], F32)
            prod = sb(p3, "prod", [128, 8, 16, 16], F32)
            Beq = Buf()
            dlt = sb(p3, "dlt", [128, 8, 16], F32)
            ex = sb(p3, "ex", [128, 8, 16], F32)
            zz = sb(p3, "zz", [128, 8], F32)
            rz = sb(p3, "rz", [128, 8], F32)
            Bex = Buf()
            RTf = sb(p3, "RTf", [128, 3, 128], F32)
            BRTf = Buf()
            RTs = [sb(p3, "RTs%d" % i, [128, 3, 128], BF16) for i in range(2)]
            BRTs = [Buf(), Buf()]

            def part_A(i):
                sl = i % 2
                s_sb = s_sbs[sl]
                Bs = Bss[sl]
                S.dma("sp", xin[sl][:], x1_d[i * 128:(i + 1) * 128, :], reads=[Bx1[i]], writes=[Bxin[sl]])
                rmsnorm_tile(xin[sl][:], Bxin[sl], junk, Bjunk, st_ss[sl], st_sq[sl], st_rs[sl], Bst[sl], xn[sl], Bxn[sl])
                for k in range(8):
                    S.add("pe", I("transpose", out=psT[sl][:, k, :], in_=xn[sl][:, k * 128:(k + 1) * 128], identity=ident[:]),
                          reads=[Bxn[sl], B_const], writes=[BpsT[sl]], inc=(k == 7))
                for k in range(8):
                    S.add("act", I("activation", out=h2T[sl][:, k, :], in_=psT[sl][:, k, :], func=AF.Identity, scale=gsc2[:, k:k + 1],
                                   bias=mod[:, 24 + k, 0:1]),
                          reads=[BpsT[sl], B_mod], writes=[Bh2s[sl]])
                S.dma("sp", h2T_d[:, :, i * 128:(i + 1) * 128], h2T[sl][:], reads=[Bh2s[sl]], writes=[Bh2[i]])
                for jq in range(16):
                    for k in range(8):
                        S.add("pe", I("matmul", qp_ps[:, jq, :], lhsT=wq[:, k, jq * 128:(jq + 1) * 128], rhs=h2T[sl][:, k, :], start=(k == 0), stop=(k == 7)),
                              reads=[B_c3, Bh2s[sl]], writes=[Bqp[jq // 4]], inc=(k == 7 and jq % 4 == 3))
                for b4 in range(4):
                    evac(qpT[:, b4 * 4:(b4 + 1) * 4, :], qp_ps[:, b4 * 4:(b4 + 1) * 4, :], [Bqp[b4]], [BqpT[b4]])
                for jq in range(16):
                    S.add("pe", I("matmul", qp_ps[:, jq, :], lhsT=qpT[:, jq, :], rhs=keysT[:, jq, :], start=True, stop=True),
                          reads=[B_c3, BqpT[jq // 4]], writes=[Bqp[jq // 4]], inc=(jq % 4 == 3))
                for b4 in range(4):
                    S.add("act", I("activation", out=s_sb[:, b4 * 4:(b4 + 1) * 4, :], in_=qp_ps[:, b4 * 4:(b4 + 1) * 4, :], func=AF.Copy),
                          reads=[Bqp[b4]], writes=[Bs[b4]])

            def part_B(i):
                sl = i % 2
                s_sb = s_sbs[sl]
                Bs = Bss[sl]
                for jq in range(16):
                    S.add("dve", I("max", out=sv[:, jq, 0:8], in_=s_sb[:, jq, :]), reads=[Bs[jq // 4]], writes=[Bsva[jq]])
                for jq in range(16):
                    S.add("dve", I("max_index", out=si_u[:, jq, 0:8], in_max=sv[:, jq, 0:8], in_values=s_sb[:, jq, :]),
                          reads=[Bs[jq // 4], Bsva[jq]], writes=[Bsia[jq]])
                for jq in range(16):
                    S.add("dve", I("match_replace", out=s2[:, jq, :], in_to_replace=sv[:, jq, 0:8], in_values=s_sb[:, jq, :], imm_value=-1e30),
                          reads=[Bs[jq // 4], Bsva[jq]], writes=[Bs2[jq]])
                for jq in range(16):
                    S.add("dve", I("max", out=sv[:, jq, 8:16], in_=s2[:, jq, :]), reads=[Bs2[jq]], writes=[Bsvb[jq]])
                for jq in range(16):
                    S.add("dve", I("max_index", out=si_u[:, jq, 8:16], in_max=sv[:, jq, 8:16], in_values=s2[:, jq, :]),
                          reads=[Bs2[jq], Bsvb[jq]], writes=[Bsib[jq]])
                S.add("dve", I("tensor_copy", out=si_f[:], in_=si_u[:]), reads=Bsia + Bsib, writes=[Bsv])
                S.add("dve", I("tensor_tensor", out=cand[:].rearrange("p h (a b) -> p h a b", b=16),
                               in0=V(sv[:], 0, [[32, 8], [1, 16], [0, 16]]), in1=V(sv[:], 16, [[32, 8], [0, 16], [1, 16]]), op=ALU.add),
                      reads=Bsva + Bsvb, writes=[Bcand])
                for h in range(8):
                    S.add("dve", I("max", out=tv[:, h, 0:8], in_=cand[:, h, :]), reads=[Bcand], writes=[Btva[h]])
                for h in range(8):
                    S.add("dve", I("max_index", out=ti_u[:, h, 0:8], in_max=tv[:, h, 0:8], in_values=cand[:, h, :]), reads=[Bcand, Btva[h]], writes=[Btia[h]])
                for h in range(8):
                    S.add("dve", I("match_replace", out=cand2[:, h, :], in_to_replace=tv[:, h, 0:8], in_values=cand[:, h, :], imm_value=-1e30),
                          reads=[Bcand, Btva[h]], writes=[Bc2[h]])
                for h in range(8):
                    S.add("dve", I("max", out=tv[:, h, 8:16], in_=cand2[:, h, :]), reads=[Bc2[h]], writes=[Btvb[h]])
                for h in range(8):
                    S.add("dve", I("max_index", out=ti_u[:, h, 8:16], in_max=tv[:, h, 8:16], in_values=cand2[:, h, :]), reads=[Bc2[h], Btvb[h]], writes=[Btib[h]])
                S.add("dve", I("tensor_copy", out=tvc[:], in_=tv[:]), reads=Btva + Btvb, writes=[Btv])
                S.add("dve", I("tensor_scalar", out=ta_u[:], in0=ti_u[:], scalar1=4, scalar2=None, op0=ALU.logical_shift_right), reads=Btia + Btib, writes=[Btv])
                S.add("dve", I("tensor_scalar", out=tb_u[:], in0=ti_u[:], scalar1=15, scalar2=None, op0=ALU.bitwise_and), reads=Btia + Btib, writes=[Btv])
                S.add("dve", I("tensor_copy", out=ta_f[:], in_=ta_u[:]), reads=[], writes=[Btv])
                S.add("dve", I("tensor_copy", out=tb_f[:], in_=tb_u[:]), reads=[], writes=[Btv])
                for pp, tf in ((0, ta_f), (1, tb_f)):
                    S.add("dve", I("tensor_tensor", out=eqb[:], in0=V(tf[:], 0, [[16, 8], [1, 16], [0, 16]]),
                                   in1=V(iota16[:], 0, [[0, 8], [0, 16], [1, 16]]), op=ALU.is_equal),
                          reads=[Btv, B_c3], writes=[Beq])
                    S.add("dve", I("tensor_tensor", out=prod[:], in0=eqb[:], in1=V(si_f[:], pp * 16, [[32, 8], [0, 16], [1, 16]]), op=ALU.mult),
                          reads=[Bsv], writes=[Beq])
                    S.add("dve", I("tensor_reduce", out=RTf[:, pp, :].rearrange("p (h r) -> p h r", r=16), in_=prod[:], axis=AX.X, op=ALU.add),
                          reads=[Beq], writes=[BRTf])
                S.add("dve", I("tensor_tensor", out=dlt[:], in0=tvc[:], in1=V(tvc[:], 0, [[16, 8], [0, 16]]), op=ALU.subtract), reads=[Btv], writes=[Bex])
                S.add("act", I("activation", out=ex[:], in_=dlt[:], func=AF.Exp), reads=[], writes=[Bex])
                S.add("dve", I("tensor_reduce", out=zz[:], in_=ex[:], axis=AX.X, op=ALU.add), reads=[], writes=[Bex])
                S.add("dve", I("reciprocal", out=rz[:], in_=zz[:]), reads=[], writes=[Bex])
                S.add("dve", I("tensor_tensor", out=RTf[:, 2, :].rearrange("p (h r) -> p h r", r=16), in0=ex[:], in1=V(rz[:], 0, [[1, 8], [0, 16]]), op=ALU.mult),
                      reads=[Bex], writes=[BRTf])
                for c in range(3):
                    S.add("pe", I("transpose", out=rt_ps[:, c, :], in_=RTf[:, c, :], identity=identf[:]), reads=[BRTf, B_const], writes=[Brtp], inc=(c == 2))
                S.add("act", I("activation", out=RTs[sl][:], in_=rt_ps[:], func=AF.Copy), reads=[Brtp], writes=[BRTs[sl]])
                S.dma("sp", rt_d[:, :, i * 128:(i + 1) * 128], RTs[sl][:], reads=[BRTs[sl]], writes=[Brt[i]])

            part_A(0)
            for i in range(NT):
                if i + 1 < NT:
                    part_A(i + 1)
                part_B(i)

        S.barrier()
        if stop_after == 4:
            S.dma("sp", out_d[0:128, 0:8], gsc1[:], reads=Brt + Bh2, writes=[Buf()], is_output=True)
            S.finish()
            with nc.Block() as blk:
                S.replay(blk)
            return nc

        TG = 256
        NG = SEQ // TG
        JB = 4
        with ExitStack() as p4:
            iotab = sb(p4, "iotab", [128, 128], BF16)
            B_c4 = Buf()
            S.dma("pool", iotab[:], iota128_d, writes=[B_c4])
            h2g = [sb(p4, "h2g%d" % i, [128, 8, TG], BF16) for i in range(2)]
            rtg = [sb(p4, "rtg%d" % i, [128, 3, TG], BF16) for i in range(2)]
            Bg = [Buf(), Buf()]
            iota_rep = sb(p4, "iota_rep", [128, 128, 16], BF16)
            S.add("dve", I("tensor_copy", out=iota_rep[:], in_=V(iotab[:], 0, [[1, 128], [0, 16]])), reads=[B_c4], writes=[B_c4])
            P0 = [sb(p4, "P0_%d" % i, [128, 128, 16], BF16) for i in range(2)]
            P1 = [sb(p4, "P1_%d" % i, [128, 64, 16], BF16) for i in range(2)]
            P1w = [sb(p4, "P1w_%d" % i, [128, 64, 16], BF16) for i in range(2)]
            BP0 = [Buf(), Buf()]
            BP1 = [Buf(), Buf()]
            BP1w = [Buf(), Buf()]
            NCH = TG // 16
            G_half = [sb(p4, "G_half%d" % i, [128, 64, TG], BF16) for i in range(2)]
            BGh = [[Buf() for _ in range(NCH)] for _ in range(2)]
            G_ps = [ps(p4, "G_ps%d" % i, [128, 8, 64], F32) for i in range(2)]
            BGp = [Buf(), Buf()]
            A_ps = [ps(p4, "A_ps%d" % i, [128, 512], F32) for i in range(2)]
            BA = [Buf() for _ in range(4)]
            o_ps = [ps(p4, "o_ps%d" % i, [128, D], F32) for i in range(2)]
            Bo = [Buf(), Buf()]
            NW = 3
            ublk = [sb(p4, "ublk%d" % i, [128, JB, 1024], BF16) for i in range(NW)]
            vblk = [sb(p4, "vblk%d" % i, [128, JB, 1024], BF16) for i in range(NW)]
            Bw = [Buf() for _ in range(NW)]
            ga1 = [sb(p4, "ga1_%d" % i, [128, TG], BF16) for i in range(4)]
            Bga1 = [Buf() for _ in range(4)]
            GA = [sb(p4, "GA_%d" % i, [128, TG], BF16) for i in range(4)]
            BGA = [Buf() for _ in range(4)]
            x1t = [sb(p4, "x1t%d" % i, [128, D], F32) for i in range(2)]
            Bx1t = [Buf(), Buf()]
            t1b = sb(p4, "t1b", [128, D], F32)
            Bt1b = Buf()
            x2 = sb(p4, "x2", [128, D], F32)
            Bx2 = Buf()
            junk4f = sb(p4, "junk4f", [128, D], F32)
            mhalf = sb(p4, "mhalf", [128, 1], F32)
            S.add("dve", I("memset", mhalf[:], -0.5), writes=[B_c4])
            Bjunk4 = Buf()
            fss = sb(p4, "fss", [128, 1], F32)
            fsq = sb(p4, "fsq", [128, 1], F32)
            frs = sb(p4, "frs", [128, 1], F32)
            Bfs = Buf()
            yo = [sb(p4, "yo%d" % i, [128, D], F32) for i in range(2)]
            Byo = [Buf(), Buf()]
            wl = {"n": 0}
            gcnt = {"n": 0}
            wslot = {}

            def load_w(gg, jb):
                ws = wl["n"] % NW
                wl["n"] += 1
                wslot[(gg, jb)] = ws
                S.dma("sp", ublk[ws][:], uTb_d[jb * JB * 128:(jb + 1) * JB * 128, :].rearrange("(p j) n -> p j n", j=JB),
                      reads=[Bu[jb]], writes=[Bw[ws]])
                S.dma("sp", vblk[ws][:], vb_d[jb * JB * 128:(jb + 1) * JB * 128, :].rearrange("(p j) n -> p j n", j=JB),
                      reads=[Bvb[jb]], writes=[Bw[ws]])

            def load_grp(gg):
                gs_ = gg % 2
                t0 = gg * TG
                S.dma("sp", h2g[gs_][:], h2T_d[:, :, t0:t0 + TG], reads=[Bh2[2 * gg], Bh2[2 * gg + 1]], writes=[Bg[gs_]])
                S.dma("sp", rtg[gs_][:], rt_d[:, :, t0:t0 + TG], reads=[Brt[2 * gg], Brt[2 * gg + 1]], writes=[Bg[gs_]])

            def build_s1(gg, hb, c, part=None):
                gs_ = gg % 2
                s_ = c % 2
                if part in (None, 0):
                    S.add("dve", I("tensor_tensor", out=P0[s_][:], in0=iota_rep[:],
                                   in1=V(rtg[gs_][:], 0 * TG + c * 16, [[0, 128], [1, 16]]), op=ALU.is_equal),
                          reads=[Bg[gs_], B_c4], writes=[BP0[s_]])
                if part in (None, 1):
                    S.add("dve", I("tensor_tensor", out=P1[s_][:], in0=iota_rep[:, hb * 64:(hb + 1) * 64, :],
                                   in1=V(rtg[gs_][:], 1 * TG + c * 16, [[0, 64], [1, 16]]), op=ALU.is_equal),
                          reads=[Bg[gs_], B_c4], writes=[BP1[s_]])
                if part in (None, 2):
                    S.add("dve", I("tensor_tensor", out=P1w[s_][:], in0=P1[s_][:], in1=V(rtg[gs_][:], 2 * TG + c * 16, [[0, 64], [1, 16]]), op=ALU.mult),
                          reads=[BP1[s_], Bg[gs_]], writes=[BP1w[s_]])

            gslot = {}

            def build_s2(gg, hb, c, part=None):
                s_ = c % 2
                for q8 in range(2):
                    if part in (None, 0):
                        gb = gcnt["n"] % 2
                        gcnt["n"] += 1
                        gslot[(gg, hb, c, q8)] = gb
                        for tt in range(8):
                            t = q8 * 8 + tt
                            S.add("pe", I("matmul", G_ps[gb][:, tt, :], lhsT=P0[s_][:, :, t], rhs=P1w[s_][:, :, t], start=True, stop=True),
                                  reads=[BP0[s_], BP1w[s_]], writes=[BGp[gb]], inc=(tt == 7))
                    if part in (None, 1 + q8):
                        gb = gslot[(gg, hb, c, q8)]
                        tg0 = c * 16 + q8 * 8
                        S.add("act", I("activation", out=V(G_half[hb][:], tg0, [[TG, 64], [1, 8]]), in_=V(G_ps[gb][:], 0, [[1, 64], [64, 8]]), func=AF.Copy),
                              reads=[BGp[gb]], writes=[BGh[hb][c]])

            load_grp(0)
            for c in range(NCH):
                build_s1(0, 0, c)
                build_s2(0, 0, c)
            import os
            NA = int(os.environ.get('K_NA', '2'))
            DEPTH = NA // 2
            for gg in range(NG):
                gs_ = gg % 2
                def emit_A(j):
                    jl = j % JB
                    if jl == 0:
                        load_w(gg, j // JB)
                    ws = wslot[(gg, j // JB)]
                    a_ = j % NA
                    hb = j // 64
                    Ap = A_ps[a_ // 2][:, (a_ % 2) * 256:(a_ % 2) * 256 + TG] if NA == 4 else A_ps[a_][:, 0:TG]
                    for k in range(8):
                        S.add("pe", I("matmul", Ap, lhsT=ublk[ws][:, jl, k * 128:(k + 1) * 128], rhs=h2g[gs_][:, k, :], start=(k == 0), stop=(k == 7)),
                              reads=[Bw[ws], Bg[gs_]], writes=[BA[a_]], inc=(k == 7))
                    S.add("act", I("activation", out=ga1[a_][:], in_=Ap, func=AF.Gelu), reads=[BA[a_]], writes=[Bga1[a_]])
                    S.add("dve", I("tensor_tensor", out=GA[a_][:], in0=ga1[a_][:], in1=G_half[hb][:, j % 64, :], op=ALU.mult),
                          reads=[Bga1[a_]] + BGh[hb], writes=[BGA[a_]])

                def emit_o(j):
                    jl = j % JB
                    ws = wslot[(gg, j // JB)]
                    a_ = j % NA
                    for tt in range(TG // 128):
                        for hf in range(2):
                            S.add("pe", I("matmul", o_ps[tt][:, hf * 512:(hf + 1) * 512], lhsT=GA[a_][:, tt * 128:(tt + 1) * 128],
                                          rhs=vblk[ws][:, jl, hf * 512:(hf + 1) * 512], start=(j == 0), stop=(j == 127)),
                                  reads=[BGA[a_], Bw[ws]], writes=[Bo[tt]], inc=(tt == TG // 128 - 1 and hf == 1))

                for tt in range(TG // 128):
                    ti = gg * (TG // 128) + tt
                    S.dma("sp", x1t[ti % 2][:], x1_d[ti * 128:(ti + 1) * 128, :], reads=[Bx1[ti]], writes=[Bx1t[ti % 2]])
                for j0 in range(DEPTH):
                    emit_A(j0)
                for j in range(128):
                    jj = j % 64
                    if j < 64:
                        nb = (gg, 1)
                    else:
                        nb = (gg + 1, 0) if gg + 1 < NG else None
                    if j == 64 and gg + 1 < NG:
                        load_grp(gg + 1)
                    if j + DEPTH < 128:
                        emit_A(j + DEPTH)
                    if nb is not None:
                        for c in range(NCH):
                            base = (c * 52) // NCH
                            for part in range(3):
                                if base + part == jj:
                                    build_s1(nb[0], nb[1], c, part)
                            for part in range(3):
                                if base + 4 + part == jj:
                                    build_s2(nb[0], nb[1], c, part)
                    emit_o(j)
                for tt in range(TG // 128):
                    ti = gg * (TG // 128) + tt
                    ys = ti % 2
                    S.add("dve", I("tensor_tensor", out=t1b[:], in0=o_ps[tt][:], in1=g2row[:], op=ALU.mult), reads=[Bo[tt], B_const], writes=[Bt1b])
                    S.add("pool", I("tensor_tensor", out=x2[:], in0=t1b[:], in1=x1t[ys][:], op=ALU.add), reads=[Bt1b, Bx1t[ys]], writes=[Bx2])
                    S.add("dve", I("scalar_tensor_tensor", out=junk4f[:], in0=x2[:], scalar=1.0, in1=x2[:], op0=ALU.mult, op1=ALU.mult, accum_out=fss[:]),
                          reads=[Bx2], writes=[Bjunk4, Bfs])
                    S.add("dve", I("tensor_scalar", out=fsq[:], in0=fss[:], scalar1=1.0 / D, scalar2=EPS, op0=ALU.mult, op1=ALU.add), reads=[], writes=[Bfs])
                    S.add("pool", I("tensor_tensor", out=frs[:], in0=fsq[:], in1=mhalf[:], op=ALU.pow), reads=[B_c4], writes=[Bfs])
                    S.add("dve", I("scalar_tensor_tensor", out=yo[ys][:], in0=x2[:], scalar=frs[:], in1=fgrow[:], op0=ALU.mult, op1=ALU.mult),
                          reads=[Bx2, Bfs, B_const], writes=[Byo[ys]])
                    S.dma("pool", out_d[ti * 128:(ti + 1) * 128, :], yo[ys][:], reads=[Byo[ys]], writes=[Buf()], is_output=True)

        S.finish()
        with nc.Block() as blk:
            S.replay(blk)


    return nc


def _host_inputs(inputs):
    f = np.float32
    x = np.asarray(inputs["x"], f)
    c = np.asarray(inputs["c"], f)
    ctx = np.asarray(inputs["ctx"], f)
    c_ctx = np.asarray(inputs["c_ctx"], f)
    cst = _consts()
    rpb = np.asarray(inputs["na_rpb"], f)[0]
    p = np.arange(128)
    rho = p // 64
    qc = p % 64
    m = np.arange(17)
    kc = np.arange(64)
    a = m[None, :] - 1 - rho[:, None]
    b = kc[None, :] - qc[:, None] + 15
    va = (a >= 0) & (a <= 14)
    vb = (b >= 0) & (b <= 30)
    tt = rpb[:, np.clip(a, 0, 14)[:, :, None], np.clip(b, 0, 30)[:, None, :]]
    tt = np.where((va[:, :, None] & vb[:, None, :])[None], tt, 0.0).astype(f)
    tt = np.ascontiguousarray(tt.transpose(1, 0, 2, 3)).reshape(128, 8 * 17 * 64)

    def pk(v):
        return np.ascontiguousarray(np.asarray(v, f).reshape(8, 128).T)

    u = np.asarray(inputs["peer_u"], f)[0]
    uT = np.ascontiguousarray(u.reshape(128, 128, 8, 128).transpose(1, 3, 2, 0)).reshape(128 * 128, 1024)
    v = np.asarray(inputs["peer_v"], f)[0]
    vv = np.ascontiguousarray(v.reshape(128, 128, 1024).transpose(1, 0, 2)).reshape(128 * 128, 1024)
    keys = np.asarray(inputs["peer_keys"], f)[0].reshape(16, 128, 128)
    keysT = np.ascontiguousarray(keys.transpose(2, 0, 1)).reshape(128, 16 * 128)
    shared = {
        "ada_w": np.ascontiguousarray(np.asarray(inputs["ada_w"], f)[0]),
        "ada_b": np.ascontiguousarray(np.asarray(inputs["ada_b"], f)[0].reshape(48, 128).T),
        "ada_brow": np.ascontiguousarray(np.asarray(inputs["ada_b"], f)[0].reshape(1, 6 * D)),
        "g1n": pk(inputs["norm1_g"][0]), "g2n": pk(inputs["norm2_g"][0]),
        "fg": np.asarray(inputs["final_g"], f).reshape(1, D),
        "w_in": np.ascontiguousarray(np.asarray(inputs["w_in"], f)[0]),
        "pool_w": np.ascontiguousarray(np.asarray(inputs["pool_w"], f)[0]),
        "pscale": np.ascontiguousarray(np.asarray(inputs["pool_scale"], f)[0].reshape(4, 128).T),
        "tt": tt,
        "w_out": np.ascontiguousarray(np.asarray(inputs["w_out"], f)[0]),
        "peer_wq": np.ascontiguousarray(np.asarray(inputs["peer_wq"], f)[0]),
        "keysT": keysT, "uT": uT, "vv": vv,
        "colmask": cst["colmask"], "rowmask": cst["rowmask"], "ident": cst["ident"], "MT": cst["MT"],
        "iota16": cst["iota16"], "iota128": cst["iota128"],
    }
    maps = []
    for bi in range(8):
        mm = dict(shared)
        mm["x"] = np.ascontiguousarray(x[bi])
        mm["ctx"] = np.ascontiguousarray(ctx[bi])
        cc = np.stack([pk(c[bi]), pk(c_ctx)], axis=-1).reshape(128, 16)
        mm["cc"] = np.ascontiguousarray(cc)
        maps.append(mm)
    return maps


def kernel(**inputs):
    maps = _host_inputs(inputs)
    nc = build_nc()
    res = run_bass_kernel_spmd(nc, maps, core_ids=list(range(8)))
    return np.stack([np.asarray(r["out"], np.float32) for r in res.results], axis=0)
```

```python
import numpy as np
from contextlib import ExitStack
import concourse.bass as bass
import concourse.mybir as mybir
from concourse.bass_utils import run_bass_kernel_spmd

F32 = mybir.dt.float32
BF16 = mybir.dt.bfloat16
U32 = mybir.dt.uint32
I32 = mybir.dt.int32
AF = mybir.ActivationFunctionType
ALU = mybir.AluOpType
AX = mybir.AxisListType

D = 1024
SEQ = 4096
NT = SEQ // 128
NEG = -30000.0
EPS = 1e-6


class Buf:
    __slots__ = ("w", "r", "name")

    def __init__(self, name=""):
        self.w = None
        self.r = []
        self.name = name


class Sched:
    ENG = ("pe", "act", "dve", "pool", "sp")

    def __init__(self, nc, es, n_dma_sems=40):
        self.nc = nc
        self.streams = {e: [] for e in self.ENG}
        self.sem = {e: es.enter_context(nc.semaphore("s_" + e)) for e in self.ENG}
        self.cnt = {e: 0 for e in self.ENG}
        self.waited = {e: {} for e in self.ENG}
        self.dq = {"sp": list(range(0, 26)), "pool": list(range(26, 40)), "conv": list(range(40, 52))}
        n_dma_sems = 52
        self.dsem = [es.enter_context(nc.semaphore("s_dma%d" % i)) for i in range(n_dma_sems)]
        self.duse = [0] * n_dma_sems
        self.drr = {"sp": 0, "pool": 0, "conv": 0}
        self.out_toks = []

    def _waits(self, eng, reads, writes):
        waits = {}

        def need(tok):
            if tok is None:
                return
            sem, val, st = tok
            if st == "pe" and eng == "pe":
                return
            if self.waited[eng].get(id(sem), (None, 0))[1] >= val:
                return
            if id(sem) not in waits or waits[id(sem)][1] < val:
                waits[id(sem)] = (sem, val)

        for b in reads:
            need(b.w)
        for b in writes:
            need(b.w)
            for t in b.r:
                need(t)
        for k, v in waits.items():
            self.waited[eng][k] = v
        return list(waits.values())

    def _commit(self, tok, reads, writes):
        for b in reads:
            b.r.append(tok)
        for b in writes:
            b.w = tok
            b.r = []

    def add(self, eng, fn, reads=(), writes=(), inc=True):
        waits = self._waits(eng, reads, writes)
        sem = self.sem[eng]
        if inc:
            self.cnt[eng] += 1
            n = self.cnt[eng]
            tok = (sem, n, eng)
        else:
            assert eng == "pe"
            tok = (sem, self.cnt[eng] + 1, eng)
        assert self.cnt[eng] < 60000

        def th(e, waits=waits, fn=fn, inc=inc, sem=sem):
            for s, v in waits:
                e.wait_ge(s, v)
            ins = getattr(e, fn[0])(*fn[1], **fn[2])
            if inc:
                ins.then_inc(sem, 1)

        self.streams[eng].append(th)
        self._commit(tok, reads, writes)

    def dma(self, eng, out, in_, reads=(), writes=(), is_output=False, slow=False, qname=None):
        waits = self._waits(eng, reads, writes)
        qname = qname or eng
        q = self.dq[qname]
        k = q[self.drr[qname] % len(q)]
        self.drr[qname] += 1
        sem = self.dsem[k]
        prev = 16 * self.duse[k]
        if prev > 0 and self.waited[eng].get(id(sem), (None, 0))[1] < prev:
            self.waited[eng][id(sem)] = (sem, prev)
            waits = [w for w in waits if w[0] is not sem] + [(sem, prev)]
        self.duse[k] += 1
        tok = (sem, 16 * self.duse[k], "dma")

        def th(e, waits=waits, sem=sem, out=out, in_=in_, slow=slow):
            for s, v in waits:
                e.wait_ge(s, v)
            if slow:
                e.dma_start(out=out, in_=in_, allow_slow_non_contiguous=True).then_inc(sem, 16)
            else:
                e.dma_start(out=out, in_=in_).then_inc(sem, 16)

        self.streams[eng].append(th)
        self._commit(tok, reads, writes)
        if is_output:
            self.out_toks.append(tok)

    def barrier(self):
        toks = [(self.sem[e], self.cnt[e]) for e in self.ENG if self.cnt[e] > 0]
        toks += [(self.dsem[k], 16 * self.duse[k]) for k in range(len(self.dsem)) if self.duse[k] > 0 and k not in self.dq["conv"]]
        for eng in self.ENG:
            waits = []
            for s, v in toks:
                if self.waited[eng].get(id(s), (None, 0))[1] >= v:
                    continue
                self.waited[eng][id(s)] = (s, v)
                waits.append((s, v))

            def th(e, waits=waits):
                for s, v in waits:
                    e.wait_ge(s, v)

            self.streams[eng].append(th)

    def finish(self):
        final = {}
        for sem, val, _ in self.out_toks:
            if id(sem) not in final or final[id(sem)][1] < val:
                final[id(sem)] = (sem, val)
        fl = list(final.values())

        def th(e):
            for s, v in fl:
                e.wait_ge(s, v)

        self.streams["sp"].append(th)

    def replay(self, blk):
        st = self.streams

        @blk.sync
        def _(e):
            for th in st["sp"]:
                th(e)

        @blk.tensor
        def _(e):
            for th in st["pe"]:
                th(e)

        @blk.scalar
        def _(e):
            for th in st["act"]:
                th(e)

        @blk.vector
        def _(e):
            for th in st["dve"]:
                th(e)

        @blk.gpsimd
        def _(e):
            for th in st["pool"]:
                th(e)


def I(_opname, *a, **k):
    return (_opname, a, k)


def V(ap, off, dims):
    return bass.AP(ap.tensor, ap.offset + off, [list(ap.ap[0])] + [list(d) for d in dims])


def _consts():
    c = {}
    p = np.arange(128)
    qc = p % 64
    rho = p // 64
    kc = np.arange(64)
    c0 = np.clip(qc - 8, 0, 48)
    colmask = np.where((kc[None, :] >= c0[:, None]) & (kc[None, :] < c0[:, None] + 16), 0.0, NEG)
    c["colmask"] = colmask.astype(np.float32)
    rm = np.zeros((3, 128, 9, 64), np.float32)
    rm[0, :, 8, :] = NEG
    rm[1, :64, 8, :] = NEG
    rm[1, 64:, 0, :] = NEG
    rm[2, :, 0, :] = NEG
    c["rowmask"] = rm.reshape(3, 128, 576)
    c["ident"] = np.eye(128, dtype=np.float32)
    L = SEQ
    MT = np.zeros((5, 4, 128, 128), np.float32)

    def Mfull(w, i, rel):
        t = i * 128 + np.arange(128)
        s = (i + rel) * 128 + np.arange(128)
        lo = np.clip(t - w // 2, 0, L)
        hi = np.clip(t + w // 2, 0, L)
        cnt = (hi - lo).astype(np.float64)
        m = ((s[None, :] >= lo[:, None]) & (s[None, :] < hi[:, None])) / cnt[:, None] - (s[None, :] == t[:, None])
        return m.T

    for g, w in enumerate((2, 4, 8, 16)):
        MT[0, g] = Mfull(w, 5, 0)
        MT[1, g] = Mfull(w, 5, -1)
        MT[2, g] = Mfull(w, 5, 1)
        MT[3, g] = Mfull(w, 0, 0)
        MT[4, g] = Mfull(w, NT - 1, 0)
    c["MT"] = np.ascontiguousarray(MT.transpose(2, 0, 1, 3)).reshape(128, 20 * 128).astype(np.float32)
    c["iota16"] = np.tile(np.arange(16, dtype=np.float32)[None, :], (128, 1))
    c["iota128"] = np.tile(np.arange(128, dtype=np.float32)[None, :], (128, 1))
    return c


def _tile_geom(i):
    if i == 0:
        return 0, 0
    if i == 1:
        return 0, -2
    if i <= 29:
        return 1, -4
    if i == 30:
        return 2, -5
    return 2, -7


def build_nc(dbg=(), stop_after=None):
    nc = bass.Bass("TRN2", target_bir_lowering=False)

    def din(name, shape, dt=F32):
        return nc.dram_tensor(name, list(shape), dt, kind="ExternalInput").ap()

    def dscr(name, shape, dt):
        kind = "ExternalOutput" if name in dbg else "Internal"
        return nc.dram_tensor(name, list(shape), dt, kind=kind).ap()

    x_d = din("x", [SEQ, D])
    ctx_d = din("ctx", [256, D])
    cc_d = din("cc", [128, 16])
    adaw_d = din("ada_w", [D, 6 * D])
    adab_d = din("ada_b", [128, 48])
    adabrow_d = din("ada_brow", [1, 6 * D])
    g1n_d = din("g1n", [128, 8])
    g2n_d = din("g2n", [128, 8])
    fg_d = din("fg", [1, D])
    win_d = din("w_in", [D, 2048])
    poolw_d = din("pool_w", [4, 128, 128])
    pscale_d = din("pscale", [128, 4])
    tt_d = din("tt", [128, 8 * 17 * 64])
    wout_d = din("w_out", [D, D])
    wq_d = din("peer_wq", [D, 2048])
    keysT_d = din("keysT", [128, 16 * 128])
    uT_d = din("uT", [128 * 128, 1024])
    vv_d = din("vv", [128 * 128, 1024])
    colmask_d = din("colmask", [128, 64])
    rowmask_d = din("rowmask", [3, 128, 576])
    ident_d = din("ident", [128, 128])
    MT_d = din("MT", [128, 20 * 128])
    iota16_d = din("iota16", [128, 16])
    iota128_d = din("iota128", [128, 128])

    out_d = nc.dram_tensor("out", [SEQ, D], F32, kind="ExternalOutput").ap()

    gvec_d = dscr("gvec", [2, D], F32)
    qT_d = dscr("qT_scr", [128, 4, SEQ], BF16)
    kT_d = dscr("kT_scr", [128, 4, SEQ], BF16)
    p_d = dscr("p_scr", [SEQ, 512], BF16)
    v_d = dscr("v_scr", [SEQ, 512], BF16)
    x1_d = dscr("x1_scr", [SEQ, D], F32)
    h2T_d = dscr("h2T_scr", [128, 8, SEQ], BF16)
    rt_d = dscr("rt_scr", [128, 3, SEQ], BF16)
    uTb_d = dscr("uTb_scr", [128 * 128, 1024], BF16)
    vb_d = dscr("vb_scr", [128 * 128, 1024], BF16)

    es = ExitStack()
    with es:
        S = Sched(nc, es)

        def sb(st, name, shape, dt):
            return st.enter_context(nc.sbuf_tensor("sb_" + name, list(shape), dt))

        def ps(st, name, shape, dt=F32):
            return st.enter_context(nc.psum_tensor("ps_" + name, list(shape), dt))

        ident = sb(es, "ident", [128, 128], BF16)
        identf = sb(es, "identf", [128, 128], F32)
        mod = sb(es, "mod", [128, 48, 2], F32)
        gsc1 = sb(es, "gsc1", [128, 8], F32)
        gsc1c = sb(es, "gsc1c", [128, 8], F32)
        gsc2 = sb(es, "gsc2", [128, 8], F32)
        g1n = sb(es, "g1n", [128, 8], F32)
        g2n = sb(es, "g2n", [128, 8], F32)
        g1row = sb(es, "g1row", [128, D], F32)
        g2row = sb(es, "g2row", [128, D], F32)
        fgrow = sb(es, "fgrow", [128, D], F32)
        B_const = Buf("const")
        B_mod = Buf("mod")

        def rmsnorm_tile(xt, Bx, junk, Bjunk, ss, sq, rstd, Bst, xn, Bxn):
            S.add("act", I("activation", out=junk[:], in_=xt, func=AF.Square, accum_out=ss[:]),
                  reads=[Bx], writes=[Bjunk, Bst])
            S.add("act", I("activation", out=sq[:], in_=ss[:], func=AF.Sqrt, scale=1.0 / D, bias=epsc[:]),
                  reads=[B_const], writes=[Bst])
            S.add("dve", I("reciprocal", out=rstd[:], in_=sq[:]), reads=[], writes=[Bst])
            S.add("act", I("activation", out=xn[:], in_=xt, func=AF.Copy, scale=rstd[:]),
                  reads=[Bx, Bst], writes=[Bxn])

        epsc = sb(es, "epsc", [128, 1], F32)

        with ExitStack() as p0:
            cc = sb(p0, "cc", [128, 8, 2], F32)
            scc = sb(p0, "scc", [128, 8, 2], F32)
            adab = sb(p0, "adab", [128, 48], F32)
            awr = [sb(p0, "awr%d" % i, [128, 8, 512], F32) for i in range(2)]
            Bawr = [Buf(), Buf()]
            row_ps = [ps(p0, "row_ps%d" % i, [128, 512], F32) for i in range(4)]
            Brow = [Buf() for _ in range(4)]
            mod_ps = ps(p0, "mod_ps", [128, 48, 2], F32)
            Bmodps = Buf()
            tmp8 = sb(p0, "tmp8", [128, 8], F32)
            identtmp = sb(p0, "identtmp", [128, 128], F32)
            Bcc = Buf()

            S.add("dve", I("memset", epsc[:], EPS), writes=[B_const])
            S.dma("sp", cc[:].rearrange("p k t -> p (k t)"), cc_d, writes=[Bcc])
            S.dma("sp", adab[:], adab_d, writes=[Bcc])
            S.dma("sp", g1n[:], g1n_d, writes=[Bcc])
            S.dma("sp", g2n[:], g2n_d, writes=[Bcc])
            S.dma("sp", identf[:], ident_d, writes=[B_const])
            S.dma("sp", fgrow[:], fg_d[0:1, :].partition_broadcast(128), writes=[B_const])
            S.add("dve", I("tensor_copy", out=ident[:], in_=identf[:]), reads=[], writes=[B_const])
            S.add("act", I("activation", out=scc[:], in_=cc[:], func=AF.Silu), reads=[Bcc], writes=[Bcc])
            modrow = sb(p0, "modrow", [2, 6 * D], F32)
            Bmr = Buf()
            for cb in range(12):
                q = cb % 2
                S.dma("sp", awr[q][:], adaw_d[:, cb * 512:(cb + 1) * 512].rearrange("(k p) n -> p k n", p=128), writes=[Bawr[q]])
                for k in range(8):
                    S.add("pe", I("matmul", row_ps[cb % 4][0:2, :], lhsT=scc[:, k, :], rhs=awr[q][:, k, :], start=(k == 0), stop=(k == 7)),
                          reads=[Bawr[q], Bcc], writes=[Brow[cb % 4]], inc=(k == 7))
                S.add("act", I("activation", out=modrow[:, cb * 512:(cb + 1) * 512], in_=row_ps[cb % 4][0:2, :], func=AF.Copy),
                      reads=[Brow[cb % 4]], writes=[Bmr])
            for m in range(48):
                S.add("pe", I("transpose", out=mod_ps[:, m, :], in_=modrow[:, m * 128:(m + 1) * 128], identity=identf[0:2, 0:2]),
                      reads=[Bmr, B_const], writes=[Bmodps], inc=(m == 47))
            S.add("dve", I("tensor_tensor", out=mod[:], in0=mod_ps[:], in1=V(adab[:], 0, [[1, 48], [0, 2]]), op=ALU.add),
                  reads=[Bmodps, Bcc], writes=[B_mod])

            def mk_gsc(dst, gn, lo, col):
                S.add("dve", I("tensor_scalar", out=tmp8[:], in0=mod[:, lo:lo + 8, col], scalar1=1.0, scalar2=None, op0=ALU.add),
                      reads=[B_mod], writes=[Bcc])
                S.add("dve", I("tensor_tensor", out=dst[:], in0=tmp8[:], in1=gn[:], op=ALU.mult),
                      reads=[Bcc], writes=[B_mod])

            mk_gsc(gsc1, g1n, 8, 0)
            mk_gsc(gsc1c, g1n, 8, 1)
            mk_gsc(gsc2, g2n, 32, 0)
            abrow = sb(p0, "abrow", [128, 2, D], F32)
            Bab = Buf()
            onesr = sb(p0, "onesr", [1, 128], F32)
            S.add("dve", I("memset", onesr[:], 1.0), writes=[Bab])
            S.dma("sp", abrow[:, 0, :], adabrow_d[0:1, 2048:3072].partition_broadcast(128), writes=[Bab])
            S.dma("sp", abrow[:, 1, :], adabrow_d[0:1, 5120:6144].partition_broadcast(128), writes=[Bab])
            for gi, (col0, dst) in enumerate(((2048, g1row), (5120, g2row))):
                for hf in range(2):
                    q = (gi * 2 + hf) % 4
                    c0_ = col0 + hf * 512
                    S.add("pe", I("matmul", row_ps[q][:], lhsT=onesr[:], rhs=modrow[0:1, c0_:c0_ + 512], start=True, stop=True),
                          reads=[Bmr, Bab], writes=[Brow[q]], inc=True)
                    S.add("dve", I("tensor_tensor", out=dst[:, hf * 512:(hf + 1) * 512], in0=row_ps[q][:], in1=abrow[:, gi, hf * 512:(hf + 1) * 512], op=ALU.add),
                          reads=[Brow[q], Bab], writes=[B_const])

        S.barrier()
        if stop_after == 0:
            S.dma("sp", out_d[0:128, 0:96].rearrange("p (a b) -> p a b", b=2), mod[:], reads=[B_mod], writes=[Buf()], is_output=True, slow=True)
            S.dma("sp", out_d[128:256, :], g1row[:], reads=[B_const], writes=[Buf()], is_output=True)
            S.dma("sp", out_d[256:384, 0:8], gsc1[:], reads=[B_mod], writes=[Buf()], is_output=True)
            S.finish()
            with nc.Block() as blk:
                S.replay(blk)
            return nc

        rr = {"act": 0}

        def evac(out, in_, reads, writes, scale=None):
            rr["act"] ^= 1
            if rr["act"]:
                if scale is None:
                    S.add("act", I("activation", out=out, in_=in_, func=AF.Copy), reads=reads, writes=writes)
                else:
                    S.add("act", I("activation", out=out, in_=in_, func=AF.Copy, scale=float(scale)), reads=reads, writes=writes)
            else:
                if scale is None:
                    S.add("dve", I("tensor_copy", out=out, in_=in_), reads=reads, writes=writes)
                else:
                    S.add("dve", I("tensor_scalar", out=out, in0=in_, scalar1=float(scale), scalar2=None, op0=ALU.mult), reads=reads, writes=writes)

        Bu = [Buf() for _ in range(32)]
        Bvb = [Buf() for _ in range(32)]
        def convert(jb0, jb1):
            for jb in range(jb0, jb1):
                S.dma("pool", uTb_d[jb * 512:(jb + 1) * 512, :].rearrange("(p j) n -> j p n", j=4),
                      uT_d[jb * 512:(jb + 1) * 512, :].rearrange("(j p) n -> j p n", p=128), writes=[Bu[jb]], qname="conv")
                S.dma("pool", vb_d[jb * 512:(jb + 1) * 512, :].rearrange("(p j) n -> j p n", j=4),
                      vv_d[jb * 512:(jb + 1) * 512, :].rearrange("(j p) n -> j p n", p=128), writes=[Bvb[jb]], qname="conv")

        pA = ExitStack()
        es.enter_context(pA)
        kcT = sb(pA, "kcT", [128, 4, 256], BF16)
        vc = sb(pA, "vc", [128, 2, 512], BF16)
        B_ckv = Buf()
        Bq = [Buf() for _ in range(8)]
        Bk = [Buf() for _ in range(8)]
        Bp = [Buf() for _ in range(8)]
        Bv = [Buf() for _ in range(8)]
        Bx1 = [Buf() for _ in range(NT)]

        with ExitStack() as p01:
            w_in = sb(p01, "w_in", [128, 8, 2048], BF16)
            B_win = Buf()
            for k in range(8):
                S.dma("pool", w_in[:, k, :], win_d[k * 128:(k + 1) * 128, :], writes=[B_win])
            convert(0, 8)
            xin = [sb(p01, "xin%d" % i, [128, D], F32) for i in range(2)]
            Bxin = [Buf(), Buf()]
            junk = sb(p01, "junk", [128, D], BF16)
            Bjunk = Buf()
            xn = [sb(p01, "xn%d" % i, [128, D], BF16) for i in range(2)]
            Bxn = [Buf(), Buf()]
            st_ss = [sb(p01, "ss%d" % i, [128, 1], F32) for i in range(2)]
            st_sq = [sb(p01, "sq%d" % i, [128, 1], F32) for i in range(2)]
            st_rs = [sb(p01, "rs%d" % i, [128, 1], F32) for i in range(2)]
            Bst = [Buf(), Buf()]
            psT = [ps(p01, "psT%d" % i, [128, 8, 128], BF16) for i in range(2)]
            BpsT = [Buf(), Buf()]
            z_ps = [ps(p01, "z_ps%d" % i, [128, 512], F32) for i in range(3)]
            Bz = [Buf() for _ in range(3)]
            zc = {"i": 0}

            def norm_T(src_ap, slot, dstT, col0, Bdst, gs, shcol):
                S.dma("sp", xin[slot][:], src_ap, writes=[Bxin[slot]])
                rmsnorm_tile(xin[slot][:], Bxin[slot], junk, Bjunk, st_ss[slot], st_sq[slot], st_rs[slot], Bst[slot], xn[slot], Bxn[slot])
                for k in range(8):
                    S.add("pe", I("transpose", out=psT[slot][:, k, :], in_=xn[slot][:, k * 128:(k + 1) * 128], identity=ident[:]),
                          reads=[Bxn[slot], B_const], writes=[BpsT[slot]], inc=(k == 7))
                for k in range(8):
                    S.add("dve", I("tensor_scalar", out=dstT[:, k, col0:col0 + 128], in0=psT[slot][:, k, :],
                                                                 scalar1=gs[:, k:k + 1], scalar2=mod[:, k + shcol[0], shcol[1]:shcol[1] + 1],
                                                                 op0=ALU.mult, op1=ALU.add),
                          reads=[BpsT[slot], B_mod], writes=[Bdst])

            def proj(out_ps_i, lhs_fn, rhs_fn, reads):
                zi = zc["i"] % 3
                zc["i"] += 1
                for k in range(8):
                    S.add("pe", I("matmul", z_ps[zi][:, 0:out_ps_i], lhsT=lhs_fn(k), rhs=rhs_fn(k), start=(k == 0), stop=(k == 7)),
                          reads=reads, writes=[Bz[zi]], inc=(k == 7))
                return zi

            with ExitStack() as p0b:
                hcT = sb(p0b, "hcT", [128, 8, 256], BF16)
                BhcT = Buf()
                for tl in range(2):
                    norm_T(ctx_d[tl * 128:(tl + 1) * 128, :], tl, hcT, tl * 128, BhcT, gsc1c, (0, 1))
                for j in range(4):
                    zi = proj(256, lambda k, j=j: w_in[:, k, 1024 + j * 128:1024 + (j + 1) * 128], lambda k: hcT[:, k, :], [B_win, BhcT])
                    evac(kcT[:, j, :], z_ps[zi][:, 0:256], [Bz[zi]], [B_ckv])
                for tl in range(2):
                    zi = proj(512, lambda k, tl=tl: hcT[:, k, tl * 128:(tl + 1) * 128], lambda k: w_in[:, k, 1536:2048], [B_win, BhcT])
                    evac(vc[:, tl, :], z_ps[zi][:, :], [Bz[zi]], [B_ckv])

            if stop_after != 1:
                S.barrier()
            if stop_after == 1:
                dbg_d = nc.dram_tensor("dbg0", [128, 4 * 256 + 2 * 512], BF16, kind="ExternalOutput").ap()
                S.dma("sp", dbg_d[:, 0:1024], kcT[:].rearrange("p a b -> p (a b)"), reads=[B_ckv], writes=[Buf()], is_output=True)
                S.dma("sp", dbg_d[:, 1024:2048], vc[:].rearrange("p a b -> p (a b)"), reads=[B_ckv], writes=[Buf()], is_output=True)
                dbg1_d = nc.dram_tensor("dbg1", [128, 4096], BF16, kind="ExternalOutput").ap()
                S.dma("sp", dbg1_d[:, 0:1024], w_in[:, 0, 0:1024], reads=[B_win], writes=[Buf()], is_output=True)
                S.dma("sp", dbg1_d[:, 1024:3072], hcT[:].rearrange("p a b -> p (a b)"), reads=[BhcT], writes=[Buf()], is_output=True)
                S.dma("sp", dbg1_d[:, 3072:4096], xn[1][:], reads=[Bxn[1]], writes=[Buf()], is_output=True)
                for qi, tl_ in enumerate((st_ss[1], st_sq[1], st_rs[1])):
                    dq = nc.dram_tensor("dbg2_%d" % qi, [128, 1], F32, kind="ExternalOutput").ap()
                    S.dma("sp", dq, tl_[:], reads=[Bst[1]], writes=[Buf()], is_output=True)
                S.finish()
                with nc.Block() as blk:
                    S.replay(blk)
                return nc

            hT = [sb(p01, "hT%d" % i, [128, 8, 512], BF16) for i in range(2)]
            BhT = [Buf(), Buf()]
            qTs = [sb(p01, "qTs%d" % i, [128, 4, 512], BF16) for i in range(2)]
            kTs = [sb(p01, "kTs%d" % i, [128, 4, 512], BF16) for i in range(2)]
            pss = [sb(p01, "pss%d" % i, [128, 4, 512], BF16) for i in range(2)]
            vss = [sb(p01, "vss%d" % i, [128, 4, 512], BF16) for i in range(2)]
            Bqs = [Buf(), Buf()]
            Bks = [Buf(), Buf()]
            Bpss = [Buf(), Buf()]
            Bvss = [Buf(), Buf()]
            pend = []
            for g in range(8):
                gs_ = g % 2
                for tl in range(4):
                    ti = g * 4 + tl
                    norm_T(x_d[ti * 128:(ti + 1) * 128, :], ti % 2, hT[gs_], tl * 128, BhT[gs_], gsc1, (0, 0))
                    if tl == 1:
                        for f in pend:
                            f()
                        pend = []
                for j in range(4):
                    zi = proj(512, lambda k, j=j: w_in[:, k, 512 + j * 128:512 + (j + 1) * 128], lambda k: hT[gs_][:, k, :], [B_win, BhT[gs_]])
                    evac(qTs[gs_][:, j, :], z_ps[zi][:, :], [Bz[zi]], [Bqs[gs_]], scale=0.125)
                for j in range(4):
                    zi = proj(512, lambda k, j=j: w_in[:, k, 1024 + j * 128:1024 + (j + 1) * 128], lambda k: hT[gs_][:, k, :], [B_win, BhT[gs_]])
                    evac(kTs[gs_][:, j, :], z_ps[zi][:, :], [Bz[zi]], [Bks[gs_]])
                for tl in range(4):
                    zi = proj(512, lambda k, tl=tl: hT[gs_][:, k, tl * 128:(tl + 1) * 128], lambda k: w_in[:, k, 0:512], [B_win, BhT[gs_]])
                    evac(pss[gs_][:, tl, :], z_ps[zi][:, :], [Bz[zi]], [Bpss[gs_]])
                    zi = proj(512, lambda k, tl=tl: hT[gs_][:, k, tl * 128:(tl + 1) * 128], lambda k: w_in[:, k, 1536:2048], [B_win, BhT[gs_]])
                    evac(vss[gs_][:, tl, :], z_ps[zi][:, :], [Bz[zi]], [Bvss[gs_]])
                def stores(g=g, gs_=gs_):
                    S.dma("sp", qT_d[:, :, g * 512:(g + 1) * 512], qTs[gs_][:], reads=[Bqs[gs_]], writes=[Bq[g]])
                    S.dma("sp", kT_d[:, :, g * 512:(g + 1) * 512], kTs[gs_][:], reads=[Bks[gs_]], writes=[Bk[g]])
                    S.dma("sp", p_d[g * 512:(g + 1) * 512, :].rearrange("(t p) n -> p t n", p=128), pss[gs_][:], reads=[Bpss[gs_]], writes=[Bp[g]])
                    S.dma("sp", v_d[g * 512:(g + 1) * 512, :].rearrange("(t p) n -> p t n", p=128), vss[gs_][:], reads=[Bvss[gs_]], writes=[Bv[g]])
                pend.append(stores)
            for f in pend:
                f()

        S.barrier()
        if stop_after == 2:
            S.dma("sp", out_d[0:128, 0:8], gsc1[:], reads=[Bq[7], Bk[7], Bp[7], Bv[7]] + Bq + Bk + Bp + Bv, writes=[Buf()], is_output=True)
            S.finish()
            with nc.Block() as blk:
                S.replay(blk)
            return nc

        with ExitStack() as p2:
            ttf = sb(p2, "ttf", [128, 8 * 17 * 64], F32)
            cmask = sb(p2, "cmask", [128, 64], F32)
            TTb = sb(p2, "TTb", [128, 8, 17, 64], BF16)
            rmask = sb(p2, "rmask", [128, 3, 576], BF16)
            MTs = sb(p2, "MTs", [128, 20, 128], BF16)
            poolw = sb(p2, "poolw", [128, 4, 128], BF16)
            pscale = sb(p2, "pscale", [128, 4], F32)
            w_out = sb(p2, "w_out", [128, 8, D], BF16)
            B_c2 = Buf()
            Btt = Buf()
            S.dma("sp", ttf[:], tt_d, writes=[Btt])
            S.dma("sp", cmask[:], colmask_d, writes=[Btt])
            S.dma("sp", pscale[:], pscale_d, writes=[B_c2])
            for a in range(3):
                S.dma("pool", rmask[:, a, :], rowmask_d[a], writes=[B_c2])
            S.dma("pool", MTs[:].rearrange("p a b -> p (a b)"), MT_d, writes=[B_c2])
            for g in range(4):
                S.dma("pool", poolw[:, g, :], poolw_d[g], writes=[B_c2])
            for k in range(8):
                S.dma("pool", w_out[:, k, :], wout_d[k * 128:(k + 1) * 128, :], writes=[B_c2])
            convert(8, 20)
            S.add("dve", I("tensor_tensor", out=TTb[:].rearrange("p h m c -> p (h m) c"),
                                                   in0=ttf[:].rearrange("p (a c) -> p a c", c=64),
                                                   in1=V(cmask[:], 0, [[0, 136], [1, 64]]), op=ALU.add),
                  reads=[Btt], writes=[B_c2])

            TTi = sb(p2, "TTi", [128, 8, 9, 64], BF16)
            S.add("dve", I("tensor_tensor", out=TTi[:].rearrange("p h m c -> p h (m c)"), in0=TTb[:, :, 4:13, :].rearrange("p h m c -> p h (m c)"),
                           in1=V(rmask[:], 576, [[0, 8], [1, 576]]), op=ALU.add),
                  reads=[B_c2], writes=[B_c2])
            QT = [sb(p2, "QT%d" % i, [128, 4, 128], BF16) for i in range(2)]
            KT = [sb(p2, "KT%d" % i, [128, 4, 576], BF16) for i in range(2)]
            Vt = [sb(p2, "Vt%d" % i, [128, 5, 512], BF16) for i in range(2)]
            Pt = [sb(p2, "Pt%d" % i, [128, 3, 512], BF16) for i in range(2)]
            xr = [sb(p2, "xr%d" % i, [128, D], F32) for i in range(2)]
            Bld = [Buf(), Buf()]
            Bxr = [Buf(), Buf()]
            S_ps = [ps(p2, "S_ps%d" % i, [128, 1024], F32) for i in range(2)]
            BS = [Buf(), Buf()]
            ET_ps = ps(p2, "ET_ps", [128, 7, 128], BF16)
            BETp = Buf()
            O_ps = ps(p2, "O_ps", [128, 512], F32)
            BO = Buf()
            pl_ps = ps(p2, "pl_ps", [128, 4, 128], F32)
            Bpl = Buf()
            out_ps = ps(p2, "out_ps", [128, 512], F32)
            Bout = Buf()
            E_sb = [sb(p2, "E_sb%d" % i, [128, 832], BF16) for i in range(2)]
            BE = [Buf(), Buf()]
            ET_sb = [sb(p2, "ET_sb%d" % i, [128, 7, 128], BF16) for i in range(2)]
            BET = [Buf(), Buf()]
            nmx = [sb(p2, "nmx%d" % i, [128, 1], F32) for i in range(2)]
            Bnmx = [Buf(), Buf()]
            rsum = [sb(p2, "rsum%d" % i, [128, 8], F32) for i in range(2)]
            rinv = [sb(p2, "rinv%d" % i, [128, 8], F32) for i in range(2)]
            Brs = [Buf(), Buf()]
            attn = sb(p2, "attn", [128, 512], BF16)
            Battn = Buf()
            pooledT = sb(p2, "pooledT", [128, 4, 128], BF16)
            Bpooled = Buf()
            mixT = [sb(p2, "mixT%d" % i, [128, 8, 128], BF16) for i in range(2)]
            Bmix = [Buf(), Buf()]
            t1 = sb(p2, "t1", [128, D], F32)
            Bt1 = Buf()
            x1s = [sb(p2, "x1s%d" % i, [128, D], F32) for i in range(2)]
            Bx1s = [Buf(), Buf()]

            def grp_range(lo_tok, hi_tok, arr):
                return [arr[g] for g in range(lo_tok // 512, (hi_tok - 1) // 512 + 1)]

            Bqt = [Buf(), Buf()]
            Bkt = [Buf(), Buf()]
            Bvta = [Buf(), Buf()]
            Bvtb = [Buf(), Buf()]
            Bpt = [Buf(), Buf()]

            def loads2(i):
                sl = i % 2
                mtype, off = _tile_geom(i)
                base = 2 * i + off
                kt0 = base * 64
                S.dma("sp", QT[sl][:], qT_d[:, :, i * 128:(i + 1) * 128], reads=[Bq[i // 4]], writes=[Bqt[sl]])
                S.dma("sp", KT[sl][:], kT_d[:, :, kt0:kt0 + 576], reads=grp_range(kt0, kt0 + 576, Bk), writes=[Bkt[sl]])
                S.dma("sp", Vt[sl][:, 0:4, :], v_d[kt0:kt0 + 512, :].rearrange("(c p) d -> p c d", p=128),
                      reads=grp_range(kt0, kt0 + 576, Bv), writes=[Bvta[sl]])
                S.dma("sp", Vt[sl][0:64, 4, :], v_d[kt0 + 512:kt0 + 576, :], reads=grp_range(kt0, kt0 + 576, Bv), writes=[Bvtb[sl]])
                plo = max(i - 1, 0)
                phi = min(i + 1, NT - 1)
                S.dma("sp", Pt[sl][:, plo - (i - 1):phi - (i - 1) + 1, :],
                      p_d[plo * 128:(phi + 1) * 128, :].rearrange("(c p) d -> p c d", p=128),
                      reads=grp_range(plo * 128, (phi + 1) * 128, Bp), writes=[Bpt[sl]])
                S.dma("sp", xr[sl][:], x_d[i * 128:(i + 1) * 128, :], writes=[Bxr[sl]])

            loads2(0)
            for i in range(NT):
                sl = i % 2
                mtype, off = _tile_geom(i)
                base = 2 * i + off
                m0 = off + 8
                kt0 = base * 64
                if i + 1 < NT:
                    loads2(i + 1)
                cols = [(0, 128), (128, 128), (256, 128), (384, 128), (576, 128), (704, 128), (512, 64)]

                def emit_qk(h):
                    j = h // 2
                    po = (h % 2) * 64
                    hs = h % 2
                    q_ap = QT[sl][po:po + 64, j, :]
                    rd = [Bqt[sl], Bkt[sl], B_c2, B_const, B_ckv]
                    Sp = S_ps[hs]
                    S.add("pe", I("matmul", Sp[:, 0:512], lhsT=q_ap, rhs=KT[sl][po:po + 64, j, 0:512], start=True, stop=False),
                          reads=rd, writes=[BS[hs]], inc=False)
                    if mtype == 1:
                        S.add("pe", I("matmul", Sp[:, 0:512], lhsT=ident[:], rhs=TTi[:, h, 0:8, :].rearrange("p a b -> p (a b)"), start=False, stop=True),
                              reads=rd, writes=[BS[hs]], inc=False)
                    else:
                        S.add("pe", I("matmul", Sp[:, 0:512], lhsT=ident[:], rhs=TTb[:, h, m0:m0 + 8, :].rearrange("p a b -> p (a b)"), start=False, stop=False),
                              reads=rd, writes=[BS[hs]], inc=False)
                        S.add("pe", I("matmul", Sp[:, 0:512], lhsT=ident[:], rhs=rmask[:, mtype, 0:512], start=False, stop=True),
                              reads=rd, writes=[BS[hs]], inc=False)
                    S.add("pe", I("matmul", Sp[:, 512:576], lhsT=q_ap, rhs=KT[sl][po:po + 64, j, 512:576], start=True, stop=False),
                          reads=rd, writes=[BS[hs]], inc=False)
                    if mtype == 1:
                        S.add("pe", I("matmul", Sp[:, 512:576], lhsT=ident[:], rhs=TTi[:, h, 8, :], start=False, stop=True),
                              reads=rd, writes=[BS[hs]], inc=False)
                    else:
                        S.add("pe", I("matmul", Sp[:, 512:576], lhsT=ident[:], rhs=TTb[:, h, m0 + 8, :], start=False, stop=False),
                              reads=rd, writes=[BS[hs]], inc=False)
                        S.add("pe", I("matmul", Sp[:, 512:576], lhsT=ident[:], rhs=rmask[:, mtype, 512:576], start=False, stop=True),
                              reads=rd, writes=[BS[hs]], inc=False)
                    S.add("pe", I("matmul", Sp[:, 576:832], lhsT=q_ap, rhs=kcT[po:po + 64, j, :], start=True, stop=True),
                          reads=rd, writes=[BS[hs]], inc=True)
                    S.add("dve", I("tensor_reduce", out=nmx[hs][:], in_=Sp[:, 0:832], axis=AX.X, op=ALU.max, negate=True),
                          reads=[BS[hs]], writes=[Bnmx[hs]])
                    S.add("act", I("activation", out=E_sb[hs][:], in_=Sp[:, 0:832], func=AF.Exp, bias=nmx[hs][:], scale=1.0,
                                   accum_out=rsum[sl][:, h:h + 1]),
                          reads=[BS[hs], Bnmx[hs]], writes=[BE[hs], Brs[sl]])

                def emit_T(h):
                    hs = h % 2
                    for c, (c0_, cw) in enumerate(cols):
                        S.add("pe", I("transpose", out=ET_ps[0:cw, c, :], in_=E_sb[hs][:, c0_:c0_ + cw], identity=ident[:]),
                              reads=[BE[hs], B_const], writes=[BETp], inc=(c == 6))
                    S.add("dve", I("tensor_copy", out=ET_sb[hs][:, 0:6, :], in_=ET_ps[:, 0:6, :]), reads=[BETp], writes=[BET[hs]])
                    S.add("act", I("activation", out=ET_sb[hs][0:64, 6, :], in_=ET_ps[0:64, 6, :], func=AF.Copy), reads=[BETp], writes=[BET[hs]])

                def emit_pv(h):
                    hs = h % 2
                    vsrc = [Vt[sl][:, 0, h * 64:(h + 1) * 64], Vt[sl][:, 1, h * 64:(h + 1) * 64], Vt[sl][:, 2, h * 64:(h + 1) * 64],
                            Vt[sl][:, 3, h * 64:(h + 1) * 64], vc[:, 0, h * 64:(h + 1) * 64], vc[:, 1, h * 64:(h + 1) * 64],
                            Vt[sl][0:64, 4, h * 64:(h + 1) * 64]]
                    for c in range(7):
                        lw = 64 if c == 6 else 128
                        S.add("pe", I("matmul", O_ps[:, h * 64:(h + 1) * 64], lhsT=ET_sb[hs][0:lw, c, :], rhs=vsrc[c],
                                      start=(c == 0), stop=(c == 6)),
                              reads=[BET[hs], Bvta[sl], Bvtb[sl], B_ckv], writes=[BO], inc=(c == 6))

                emit_qk(0)
                for h in range(8):
                    if h + 1 < 8:
                        emit_qk(h + 1)
                    emit_T(h)
                    if h >= 1:
                        emit_pv(h - 1)
                emit_pv(7)
                S.add("dve", I("reciprocal", out=rinv[sl][:], in_=rsum[sl][:]), reads=[Brs[sl]], writes=[Brs[sl]])
                S.add("dve", I("tensor_tensor", out=attn[:].rearrange("p (h d) -> p h d", d=64), in0=O_ps[:].rearrange("p (h d) -> p h d", d=64),
                                                       in1=V(rinv[sl][:], 0, [[1, 8], [0, 64]]), op=ALU.mult),
                      reads=[BO, Brs[sl]], writes=[Battn])
                for c in range(4):
                    S.add("pe", I("transpose", out=ET_ps[:, c, :], in_=attn[:, c * 128:(c + 1) * 128], identity=ident[:]),
                          reads=[Battn, B_const], writes=[BETp], inc=(c == 3))
                S.add("act", I("activation", out=mixT[sl][:, 4:8, :], in_=ET_ps[:, 0:4, :], func=AF.Copy), reads=[BETp], writes=[Bmix[sl]])
                rels = []
                if i > 0:
                    rels.append((0, 1))
                rels.append((1, 3 if i == 0 else (4 if i == NT - 1 else 0)))
                if i < NT - 1:
                    rels.append((2, 2))
                for g in range(4):
                    for ri, (slot, kind) in enumerate(rels):
                        S.add("pe", I("matmul", pl_ps[:, g, :], lhsT=Pt[sl][:, slot, g * 128:(g + 1) * 128], rhs=MTs[:, kind * 4 + g, :],
                                                                                   start=(ri == 0), stop=(ri == len(rels) - 1)),
                              reads=[Bpt[sl], B_c2], writes=[Bpl], inc=(g == 3 and ri == len(rels) - 1))
                S.add("act", I("activation", out=pooledT[:], in_=pl_ps[:], func=AF.Copy), reads=[Bpl], writes=[Bpooled])
                for g in range(4):
                    S.add("pe", I("matmul", pl_ps[:, g, :], lhsT=poolw[:, g, :], rhs=pooledT[:, g, :], start=True, stop=True),
                          reads=[Bpooled, B_c2], writes=[Bpl], inc=(g == 3))
                for g in range(4):
                    S.add("dve", I("tensor_scalar", out=mixT[sl][:, g, :], in0=pl_ps[:, g, :], scalar1=pscale[:, g:g + 1], scalar2=None, op0=ALU.mult),
                          reads=[Bpl, B_c2], writes=[Bmix[sl]])
                for hf in range(2):
                    for k in range(8):
                        S.add("pe", I("matmul", out_ps[:], lhsT=mixT[sl][:, k, :], rhs=w_out[:, k, hf * 512:(hf + 1) * 512],
                                      start=(k == 0), stop=(k == 7)),
                              reads=[Bmix[sl], B_c2], writes=[Bout], inc=(k == 7))
                    S.add("dve", I("tensor_tensor", out=t1[:, hf * 512:(hf + 1) * 512], in0=out_ps[:], in1=g1row[:, hf * 512:(hf + 1) * 512], op=ALU.mult),
                          reads=[Bout, B_const], writes=[Bt1])
                S.add("dve", I("tensor_tensor", out=x1s[sl][:], in0=t1[:], in1=xr[sl][:], op=ALU.add), reads=[Bt1, Bxr[sl]], writes=[Bx1s[sl]])
                S.dma("sp", x1_d[i * 128:(i + 1) * 128, :], x1s[sl][:], reads=[Bx1s[sl]], writes=[Bx1[i]])

        S.barrier()
        if stop_after == 3:
            S.dma("sp", out_d[0:128, 0:8], gsc1[:], reads=Bx1, writes=[Buf()], is_output=True)
            S.finish()
            with nc.Block() as blk:
                S.replay(blk)
            return nc

        Bh2 = [Buf() for _ in range(NT)]
        Brt = [Buf() for _ in range(NT)]
        with ExitStack() as p3:
            wq = sb(p3, "wq", [128, 8, 2048], BF16)
            keysT = sb(p3, "keysT", [128, 16, 128], BF16)
            iota16 = sb(p3, "iota16", [128, 16], F32)
            B_c3 = Buf()
            for k in range(8):
                S.dma("pool", wq[:, k, :], wq_d[k * 128:(k + 1) * 128, :], writes=[B_c3])
            S.dma("pool", keysT[:].rearrange("p a b -> p (a b)"), keysT_d, writes=[B_c3])
            convert(20, 32)
            S.dma("sp", iota16[:], iota16_d, writes=[B_c3])
            xin = [sb(p3, "xin3_%d" % i, [128, D], F32) for i in range(2)]
            Bxin = [Buf(), Buf()]
            junk = sb(p3, "junk3", [128, D], BF16)
            Bjunk = Buf()
            xn = [sb(p3, "xn3_%d" % i, [128, D], BF16) for i in range(2)]
            Bxn = [Buf(), Buf()]
            st_ss = [sb(p3, "ss3_%d" % i, [128, 1], F32) for i in range(2)]
            st_sq = [sb(p3, "sq3_%d" % i, [128, 1], F32) for i in range(2)]
            st_rs = [sb(p3, "rs3_%d" % i, [128, 1], F32) for i in range(2)]
            Bst = [Buf(), Buf()]
            psT = [ps(p3, "psT3_%d" % i, [128, 8, 128], BF16) for i in range(2)]
            BpsT = [Buf(), Buf()]
            h2T = [sb(p3, "h2T%d" % i, [128, 8, 128], BF16) for i in range(2)]
            Bh2s = [Buf(), Buf()]
            qp_ps = ps(p3, "qp_ps", [128, 16, 128], F32)
            Bqp = [Buf() for _ in range(4)]
            rt_ps = ps(p3, "rt_ps", [128, 3, 128], F32)
            Brtp = Buf()
            qpT = sb(p3, "qpT", [128, 16, 128], BF16)
            BqpT = [Buf() for _ in range(4)]
            s_sbs = [sb(p3, "s_sb%d" % i, [128, 16, 128], F32) for i in range(2)]
            Bss = [[Buf() for _ in range(4)] for _ in range(2)]
            s2 = sb(p3, "s2", [128, 16, 128], F32)
            Bs2 = [Buf() for _ in range(16)]
            Bsva = [Buf() for _ in range(16)]
            Bsvb = [Buf() for _ in range(16)]
            Bsia = [Buf() for _ in range(16)]
            Bsib = [Buf() for _ in range(16)]
            Btva = [Buf() for _ in range(8)]
            Btvb = [Buf() for _ in range(8)]
            Btia = [Buf() for _ in range(8)]
            Btib = [Buf() for _ in range(8)]
            Bc2 = [Buf() for _ in range(8)]
            sv = sb(p3, "sv", [128, 16, 16], F32)
            si_u = sb(p3, "si_u", [128, 16, 16], U32)
            si_f = sb(p3, "si_f", [128, 16, 16], F32)
            Bsv = Buf()
            cand = sb(p3, "cand", [128, 8, 256], F32)
            cand2 = sb(p3, "cand2", [128, 8, 256], F32)
            Bcand = Buf()
            tv = sb(p3, "tv", [128, 8, 16], F32)
            tvc = sb(p3, "tvc", [128, 8, 16], F32)
            ti_u = sb(p3, "ti_u", [128, 8, 16], U32)
            ta_u = sb(p3, "ta_u", [128, 8, 16], U32)
            tb_u = sb(p3, "tb_u", [128, 8, 16], U32)
            ta_f = sb(p3, "ta_f", [128, 8, 16], F32)
            tb_f = sb(p3, "tb_f", [128, 8, 16], F32)
            Btv = Buf()
            eqb = sb(p3, "eqb", [128, 8, 16, 16], F32)
            prod = sb(p3, "prod", [128, 8, 16, 16], F32)
            Beq = Buf()
            dlt = sb(p3, "dlt", [128, 8, 16], F32)
            ex = sb(p3, "ex", [128, 8, 16], F32)
            zz = sb(p3, "zz", [128, 8], F32)
            rz = sb(p3, "rz", [128, 8], F32)
            Bex = Buf()
            RTf = sb(p3, "RTf", [128, 3, 128], F32)
            BRTf = Buf()
            RTs = [sb(p3, "RTs%d" % i, [128, 3, 128], BF16) for i in range(2)]
            BRTs = [Buf(), Buf()]

            def part_A(i):
                sl = i % 2
                s_sb = s_sbs[sl]
                Bs = Bss[sl]
                S.dma("sp", xin[sl][:], x1_d[i * 128:(i + 1) * 128, :], reads=[Bx1[i]], writes=[Bxin[sl]])
                rmsnorm_tile(xin[sl][:], Bxin[sl], junk, Bjunk, st_ss[sl], st_sq[sl], st_rs[sl], Bst[sl], xn[sl], Bxn[sl])
                for k in range(8):
                    S.add("pe", I("transpose", out=psT[sl][:, k, :], in_=xn[sl][:, k * 128:(k + 1) * 128], identity=ident[:]),
                          reads=[Bxn[sl], B_const], writes=[BpsT[sl]], inc=(k == 7))
                for k in range(8):
                    S.add("act", I("activation", out=h2T[sl][:, k, :], in_=psT[sl][:, k, :], func=AF.Identity, scale=gsc2[:, k:k + 1],
                                   bias=mod[:, 24 + k, 0:1]),
                          reads=[BpsT[sl], B_mod], writes=[Bh2s[sl]])
                S.dma("sp", h2T_d[:, :, i * 128:(i + 1) * 128], h2T[sl][:], reads=[Bh2s[sl]], writes=[Bh2[i]])
                for jq in range(16):
                    for k in range(8):
                        S.add("pe", I("matmul", qp_ps[:, jq, :], lhsT=wq[:, k, jq * 128:(jq + 1) * 128], rhs=h2T[sl][:, k, :], start=(k == 0), stop=(k == 7)),
                              reads=[B_c3, Bh2s[sl]], writes=[Bqp[jq // 4]], inc=(k == 7 and jq % 4 == 3))
                for b4 in range(4):
                    evac(qpT[:, b4 * 4:(b4 + 1) * 4, :], qp_ps[:, b4 * 4:(b4 + 1) * 4, :], [Bqp[b4]], [BqpT[b4]])
                for jq in range(16):
                    S.add("pe", I("matmul", qp_ps[:, jq, :], lhsT=qpT[:, jq, :], rhs=keysT[:, jq, :], start=True, stop=True),
                          reads=[B_c3, BqpT[jq // 4]], writes=[Bqp[jq // 4]], inc=(jq % 4 == 3))
                for b4 in range(4):
                    S.add("act", I("activation", out=s_sb[:, b4 * 4:(b4 + 1) * 4, :], in_=qp_ps[:, b4 * 4:(b4 + 1) * 4, :], func=AF.Copy),
                          reads=[Bqp[b4]], writes=[Bs[b4]])

            def part_B(i):
                sl = i % 2
                s_sb = s_sbs[sl]
                Bs = Bss[sl]
                for jq in range(16):
                    S.add("dve", I("max", out=sv[:, jq, 0:8], in_=s_sb[:, jq, :]), reads=[Bs[jq // 4]], writes=[Bsva[jq]])
                for jq in range(16):
                    S.add("dve", I("max_index", out=si_u[:, jq, 0:8], in_max=sv[:, jq, 0:8], in_values=s_sb[:, jq, :]),
                          reads=[Bs[jq // 4], Bsva[jq]], writes=[Bsia[jq]])
                for jq in range(16):
                    S.add("dve", I("match_replace", out=s2[:, jq, :], in_to_replace=sv[:, jq, 0:8], in_values=s_sb[:, jq, :], imm_value=-1e30),
                          reads=[Bs[jq // 4], Bsva[jq]], writes=[Bs2[jq]])
                for jq in range(16):
                    S.add("dve", I("max", out=sv[:, jq, 8:16], in_=s2[:, jq, :]), reads=[Bs2[jq]], writes=[Bsvb[jq]])
                for jq in range(16):
                    S.add("dve", I("max_index", out=si_u[:, jq, 8:16], in_max=sv[:, jq, 8:16], in_values=s2[:, jq, :]),
                          reads=[Bs2[jq], Bsvb[jq]], writes=[Bsib[jq]])
                S.add("dve", I("tensor_copy", out=si_f[:], in_=si_u[:]), reads=Bsia + Bsib, writes=[Bsv])
                S.add("dve", I("tensor_tensor", out=cand[:].rearrange("p h (a b) -> p h a b", b=16),
                               in0=V(sv[:], 0, [[32, 8], [1, 16], [0, 16]]), in1=V(sv[:], 16, [[32, 8], [0, 16], [1, 16]]), op=ALU.add),
                      reads=Bsva + Bsvb, writes=[Bcand])
                for h in range(8):
                    S.add("dve", I("max", out=tv[:, h, 0:8], in_=cand[:, h, :]), reads=[Bcand], writes=[Btva[h]])
                for h in range(8):
                    S.add("dve", I("max_index", out=ti_u[:, h, 0:8], in_max=tv[:, h, 0:8], in_values=cand[:, h, :]), reads=[Bcand, Btva[h]], writes=[Btia[h]])
                for h in range(8):
                    S.add("dve", I("match_replace", out=cand2[:, h, :], in_to_replace=tv[:, h, 0:8], in_values=cand[:, h, :], imm_value=-1e30),
                          reads=[Bcand, Btva[h]], writes=[Bc2[h]])
                for h in range(8):
                    S.add("dve", I("max", out=tv[:, h, 8:16], in_=cand2[:, h, :]), reads=[Bc2[h]], writes=[Btvb[h]])
                for h in range(8):
                    S.add("dve", I("max_index", out=ti_u[:, h, 8:16], in_max=tv[:, h, 8:16], in_values=cand2[:, h, :]), reads=[Bc2[h], Btvb[h]], writes=[Btib[h]])
                S.add("dve", I("tensor_copy", out=tvc[:], in_=tv[:]), reads=Btva + Btvb, writes=[Btv])
                S.add("dve", I("tensor_scalar", out=ta_u[:], in0=ti_u[:], scalar1=4, scalar2=None, op0=ALU.logical_shift_right), reads=Btia + Btib, writes=[Btv])
                S.add("dve", I("tensor_scalar", out=tb_u[:], in0=ti_u[:], scalar1=15, scalar2=None, op0=ALU.bitwise_and), reads=Btia + Btib, writes=[Btv])
                S.add("dve", I("tensor_copy", out=ta_f[:], in_=ta_u[:]), reads=[], writes=[Btv])
                S.add("dve", I("tensor_copy", out=tb_f[:], in_=tb_u[:]), reads=[], writes=[Btv])
                for pp, tf in ((0, ta_f), (1, tb_f)):
                    S.add("dve", I("tensor_tensor", out=eqb[:], in0=V(tf[:], 0, [[16, 8], [1, 16], [0, 16]]),
                                   in1=V(iota16[:], 0, [[0, 8], [0, 16], [1, 16]]), op=ALU.is_equal),
                          reads=[Btv, B_c3], writes=[Beq])
                    S.add("dve", I("tensor_tensor", out=prod[:], in0=eqb[:], in1=V(si_f[:], pp * 16, [[32, 8], [0, 16], [1, 16]]), op=ALU.mult),
                          reads=[Bsv], writes=[Beq])
                    S.add("dve", I("tensor_reduce", out=RTf[:, pp, :].rearrange("p (h r) -> p h r", r=16), in_=prod[:], axis=AX.X, op=ALU.add),
                          reads=[Beq], writes=[BRTf])
                S.add("dve", I("tensor_tensor", out=dlt[:], in0=tvc[:], in1=V(tvc[:], 0, [[16, 8], [0, 16]]), op=ALU.subtract), reads=[Btv], writes=[Bex])
                S.add("act", I("activation", out=ex[:], in_=dlt[:], func=AF.Exp), reads=[], writes=[Bex])
                S.add("dve", I("tensor_reduce", out=zz[:], in_=ex[:], axis=AX.X, op=ALU.add), reads=[], writes=[Bex])
                S.add("dve", I("reciprocal", out=rz[:], in_=zz[:]), reads=[], writes=[Bex])
                S.add("dve", I("tensor_tensor", out=RTf[:, 2, :].rearrange("p (h r) -> p h r", r=16), in0=ex[:], in1=V(rz[:], 0, [[1, 8], [0, 16]]), op=ALU.mult),
                      reads=[Bex], writes=[BRTf])
                for c in range(3):
                    S.add("pe", I("transpose", out=rt_ps[:, c, :], in_=RTf[:, c, :], identity=identf[:]), reads=[BRTf, B_const], writes=[Brtp], inc=(c == 2))
                S.add("act", I("activation", out=RTs[sl][:], in_=rt_ps[:], func=AF.Copy), reads=[Brtp], writes=[BRTs[sl]])
                S.dma("sp", rt_d[:, :, i * 128:(i + 1) * 128], RTs[sl][:], reads=[BRTs[sl]], writes=[Brt[i]])

            part_A(0)
            for i in range(NT):
                if i + 1 < NT:
                    part_A(i + 1)
                part_B(i)

        S.barrier()
        if stop_after == 4:
            S.dma("sp", out_d[0:128, 0:8], gsc1[:], reads=Brt + Bh2, writes=[Buf()], is_output=True)
            S.finish()
            with nc.Block() as blk:
                S.replay(blk)
            return nc

        TG = 256
        NG = SEQ // TG
        JB = 4
        with ExitStack() as p4:
            iotab = sb(p4, "iotab", [128, 128], BF16)
            B_c4 = Buf()
            S.dma("pool", iotab[:], iota128_d, writes=[B_c4])
            h2g = [sb(p4, "h2g%d" % i, [128, 8, TG], BF16) for i in range(2)]
            rtg = [sb(p4, "rtg%d" % i, [128, 3, TG], BF16) for i in range(2)]
            Bg = [Buf(), Buf()]
            iota_rep = sb(p4, "iota_rep", [128, 128, 16], BF16)
            S.add("dve", I("tensor_copy", out=iota_rep[:], in_=V(iotab[:], 0, [[1, 128], [0, 16]])), reads=[B_c4], writes=[B_c4])
            P0 = [sb(p4, "P0_%d" % i, [128, 128, 16], BF16) for i in range(2)]
            P1 = [sb(p4, "P1_%d" % i, [128, 64, 16], BF16) for i in range(2)]
            P1w = [sb(p4, "P1w_%d" % i, [128, 64, 16], BF16) for i in range(2)]
            BP0 = [Buf(), Buf()]
            BP1 = [Buf(), Buf()]
            BP1w = [Buf(), Buf()]
            NCH = TG // 16
            G_half = [sb(p4, "G_half%d" % i, [128, 64, TG], BF16) for i in range(2)]
            BGh = [[Buf() for _ in range(NCH)] for _ in range(2)]
            G_ps = [ps(p4, "G_ps%d" % i, [128, 8, 64], F32) for i in range(2)]
            BGp = [Buf(), Buf()]
            A_ps = [ps(p4, "A_ps%d" % i, [128, 512], F32) for i in range(2)]
            BA = [Buf() for _ in range(4)]
            o_ps = [ps(p4, "o_ps%d" % i, [128, D], F32) for i in range(2)]
            Bo = [Buf(), Buf()]
            NW = 3
            ublk = [sb(p4, "ublk%d" % i, [128, JB, 1024], BF16) for i in range(NW)]
            vblk = [sb(p4, "vblk%d" % i, [128, JB, 1024], BF16) for i in range(NW)]
            Bw = [Buf() for _ in range(NW)]
            ga1 = [sb(p4, "ga1_%d" % i, [128, TG], BF16) for i in range(4)]
            Bga1 = [Buf() for _ in range(4)]
            GA = [sb(p4, "GA_%d" % i, [128, TG], BF16) for i in range(4)]
            BGA = [Buf() for _ in range(4)]
            x1t = [sb(p4, "x1t%d" % i, [128, D], F32) for i in range(2)]
            Bx1t = [Buf(), Buf()]
            t1bs = [sb(p4, "t1b%d" % i, [128, D], F32) for i in range(2)]
            Bt1bs = [Buf(), Buf()]
            x2s = [sb(p4, "x2_%d" % i, [128, D], F32) for i in range(2)]
            Bx2s = [Buf(), Buf()]
            junk4f = sb(p4, "junk4f", [128, D], F32)
            mhalf = sb(p4, "mhalf", [128, 1], F32)
            S.add("dve", I("memset", mhalf[:], -0.5), writes=[B_c4])
            Bjunk4 = Buf()
            fss = [sb(p4, "fss%d" % i, [128, 1], F32) for i in range(2)]
            fsq = [sb(p4, "fsq%d" % i, [128, 1], F32) for i in range(2)]
            frs = [sb(p4, "frs%d" % i, [128, 1], F32) for i in range(2)]
            Bfss = [Buf(), Buf()]
            yo = [sb(p4, "yo%d" % i, [128, D], F32) for i in range(2)]
            Byo = [Buf(), Buf()]
            wl = {"n": 0}
            gcnt = {"n": 0}
            wslot = {}

            def load_w(gg, jb):
                ws = wl["n"] % NW
                wl["n"] += 1
                wslot[(gg, jb)] = ws
                S.dma("sp", ublk[ws][:], uTb_d[jb * JB * 128:(jb + 1) * JB * 128, :].rearrange("(p j) n -> p j n", j=JB),
                      reads=[Bu[jb]], writes=[Bw[ws]])
                S.dma("sp", vblk[ws][:], vb_d[jb * JB * 128:(jb + 1) * JB * 128, :].rearrange("(p j) n -> p j n", j=JB),
                      reads=[Bvb[jb]], writes=[Bw[ws]])

            def load_grp(gg):
                gs_ = gg % 2
                t0 = gg * TG
                S.dma("sp", h2g[gs_][:], h2T_d[:, :, t0:t0 + TG], reads=[Bh2[2 * gg], Bh2[2 * gg + 1]], writes=[Bg[gs_]])
                S.dma("sp", rtg[gs_][:], rt_d[:, :, t0:t0 + TG], reads=[Brt[2 * gg], Brt[2 * gg + 1]], writes=[Bg[gs_]])

            def build_s1(gg, hb, c, part=None):
                gs_ = gg % 2
                s_ = c % 2
                if part in (None, 0):
                    S.add("dve", I("tensor_tensor", out=P0[s_][:], in0=iota_rep[:],
                                   in1=V(rtg[gs_][:], 0 * TG + c * 16, [[0, 128], [1, 16]]), op=ALU.is_equal),
                          reads=[Bg[gs_], B_c4], writes=[BP0[s_]])
                if part in (None, 1):
                    S.add("dve", I("tensor_tensor", out=P1[s_][:], in0=iota_rep[:, hb * 64:(hb + 1) * 64, :],
                                   in1=V(rtg[gs_][:], 1 * TG + c * 16, [[0, 64], [1, 16]]), op=ALU.is_equal),
                          reads=[Bg[gs_], B_c4], writes=[BP1[s_]])
                if part in (None, 2):
                    S.add("dve", I("tensor_tensor", out=P1w[s_][:], in0=P1[s_][:], in1=V(rtg[gs_][:], 2 * TG + c * 16, [[0, 64], [1, 16]]), op=ALU.mult),
                          reads=[BP1[s_], Bg[gs_]], writes=[BP1w[s_]])

            gslot = {}

            def build_s2(gg, hb, c, part=None):
                s_ = c % 2
                for q8 in range(2):
                    if part in (None, 0):
                        gb = gcnt["n"] % 2
                        gcnt["n"] += 1
                        gslot[(gg, hb, c, q8)] = gb
                        for tt in range(8):
                            t = q8 * 8 + tt
                            S.add("pe", I("matmul", G_ps[gb][:, tt, :], lhsT=P0[s_][:, :, t], rhs=P1w[s_][:, :, t], start=True, stop=True),
                                  reads=[BP0[s_], BP1w[s_]], writes=[BGp[gb]], inc=(tt == 7))
                    if part in (None, 1 + q8):
                        gb = gslot[(gg, hb, c, q8)]
                        tg0 = c * 16 + q8 * 8
                        S.add("act", I("activation", out=V(G_half[hb][:], tg0, [[TG, 64], [1, 8]]), in_=V(G_ps[gb][:], 0, [[1, 64], [64, 8]]), func=AF.Copy),
                              reads=[BGp[gb]], writes=[BGh[hb][c]])

            load_grp(0)
            for c in range(NCH):
                build_s1(0, 0, c)
                build_s2(0, 0, c)
            import os
            NA = int(os.environ.get('K_NA', '2'))
            DEPTH = NA // 2
            pending_tail = []
            for gg in range(NG):
                gs_ = gg % 2
                def emit_A(j):
                    jl = j % JB
                    if jl == 0 and (gg, j // JB) not in wslot:
                        load_w(gg, j // JB)
                    ws = wslot[(gg, j // JB)]
                    a_ = j % NA
                    hb = j // 64
                    Ap = A_ps[a_ // 2][:, (a_ % 2) * 256:(a_ % 2) * 256 + TG] if NA == 4 else A_ps[a_][:, 0:TG]
                    for k in range(8):
                        S.add("pe", I("matmul", Ap, lhsT=ublk[ws][:, jl, k * 128:(k + 1) * 128], rhs=h2g[gs_][:, k, :], start=(k == 0), stop=(k == 7)),
                              reads=[Bw[ws], Bg[gs_]], writes=[BA[a_]], inc=(k == 7))
                    S.add("act", I("activation", out=ga1[a_][:], in_=Ap, func=AF.Gelu), reads=[BA[a_]], writes=[Bga1[a_]])
                    S.add("dve", I("tensor_tensor", out=GA[a_][:], in0=ga1[a_][:], in1=G_half[hb][:, j % 64, :], op=ALU.mult),
                          reads=[Bga1[a_]] + BGh[hb], writes=[BGA[a_]])

                def emit_o(j):
                    jl = j % JB
                    ws = wslot[(gg, j // JB)]
                    a_ = j % NA
                    for tt in range(TG // 128):
                        for hf in range(2):
                            S.add("pe", I("matmul", o_ps[tt][:, hf * 512:(hf + 1) * 512], lhsT=GA[a_][:, tt * 128:(tt + 1) * 128],
                                          rhs=vblk[ws][:, jl, hf * 512:(hf + 1) * 512], start=(j == 0), stop=(j == 127)),
                                  reads=[BGA[a_], Bw[ws]], writes=[Bo[tt]], inc=(tt == TG // 128 - 1 and hf == 1))

                for j0 in range(DEPTH):
                    emit_A(j0)
                for j in range(128):
                    jj = j % 64
                    if j < 64:
                        nb = (gg, 1)
                    else:
                        nb = (gg + 1, 0) if gg + 1 < NG else None
                    while pending_tail and pending_tail[0][0] <= j:
                        pending_tail.pop(0)[1]()
                    if j == 64:
                        for tt in range(TG // 128):
                            ti = gg * (TG // 128) + tt
                            S.dma("sp", x1t[ti % 2][:], x1_d[ti * 128:(ti + 1) * 128, :], reads=[Bx1[ti]], writes=[Bx1t[ti % 2]])
                        if gg + 1 < NG:
                            load_grp(gg + 1)
                    if j == 125 and gg + 1 < NG:
                        load_w(gg + 1, 0)
                    if j + DEPTH < 128:
                        emit_A(j + DEPTH)
                    if nb is not None:
                        for c in range(NCH):
                            base = (c * 52) // NCH
                            for part in range(3):
                                if base + part == jj:
                                    build_s1(nb[0], nb[1], c, part)
                            for part in range(3):
                                if base + 4 + part == jj:
                                    build_s2(nb[0], nb[1], c, part)
                    emit_o(j)
                deferred = []
                for tt in range(TG // 128):
                    ti = gg * (TG // 128) + tt
                    ys = ti % 2
                    S.add("dve", I("tensor_tensor", out=t1bs[tt][:], in0=o_ps[tt][:], in1=g2row[:], op=ALU.mult), reads=[Bo[tt], B_const], writes=[Bt1bs[tt]])
                    base = 1 + tt * 10

                    def f_add(tt=tt, ys=ys):
                        S.add("pool", I("tensor_tensor", out=x2s[tt][:], in0=t1bs[tt][:], in1=x1t[ys][:], op=ALU.add), reads=[Bt1bs[tt], Bx1t[ys]], writes=[Bx2s[tt]])

                    def f_ss(tt=tt):
                        S.add("dve", I("scalar_tensor_tensor", out=junk4f[:], in0=x2s[tt][:], scalar=1.0, in1=x2s[tt][:], op0=ALU.mult, op1=ALU.mult, accum_out=fss[tt][:]),
                              reads=[Bx2s[tt]], writes=[Bjunk4, Bfss[tt]])

                    def f_ts(tt=tt):
                        S.add("dve", I("tensor_scalar", out=fsq[tt][:], in0=fss[tt][:], scalar1=1.0 / D, scalar2=EPS, op0=ALU.mult, op1=ALU.add), reads=[], writes=[Bfss[tt]])

                    def f_pow(tt=tt):
                        S.add("pool", I("tensor_tensor", out=frs[tt][:], in0=fsq[tt][:], in1=mhalf[:], op=ALU.pow), reads=[B_c4], writes=[Bfss[tt]])

                    def f_y(tt=tt, ys=ys):
                        S.add("dve", I("scalar_tensor_tensor", out=yo[ys][:], in0=x2s[tt][:], scalar=frs[tt][:], in1=fgrow[:], op0=ALU.mult, op1=ALU.mult),
                              reads=[Bx2s[tt], Bfss[tt], B_const], writes=[Byo[ys]])

                    def f_st(ti=ti, ys=ys):
                        S.dma("pool", out_d[ti * 128:(ti + 1) * 128, :], yo[ys][:], reads=[Byo[ys]], writes=[Buf()], is_output=True)

                    deferred += [(base, f_add), (base + 2, f_ss), (base + 3, f_ts), (base + 4, f_pow), (base + 6, f_y), (base + 7, f_st)]
                if gg + 1 < NG:
                    pending_tail = sorted(deferred, key=lambda x: x[0])
                else:
                    for _, f in sorted(deferred, key=lambda x: x[0]):
                        f()

        S.finish()
        with nc.Block() as blk:
            S.replay(blk)


    return nc


def _host_inputs(inputs):
    f = np.float32
    x = np.asarray(inputs["x"], f)
    c = np.asarray(inputs["c"], f)
    ctx = np.asarray(inputs["ctx"], f)
    c_ctx = np.asarray(inputs["c_ctx"], f)
    cst = _consts()
    rpb = np.asarray(inputs["na_rpb"], f)[0]
    p = np.arange(128)
    rho = p // 64
    qc = p % 64
    m = np.arange(17)
    kc = np.arange(64)
    a = m[None, :] - 1 - rho[:, None]
    b = kc[None, :] - qc[:, None] + 15
    va = (a >= 0) & (a <= 14)
    vb = (b >= 0) & (b <= 30)
    tt = rpb[:, np.clip(a, 0, 14)[:, :, None], np.clip(b, 0, 30)[:, None, :]]
    tt = np.where((va[:, :, None] & vb[:, None, :])[None], tt, 0.0).astype(f)
    tt = np.ascontiguousarray(tt.transpose(1, 0, 2, 3)).reshape(128, 8 * 17 * 64)

    def pk(v):
        return np.ascontiguousarray(np.asarray(v, f).reshape(8, 128).T)

    u = np.asarray(inputs["peer_u"], f)[0]
    uT = np.ascontiguousarray(u.reshape(128, 128, 8, 128).transpose(1, 3, 2, 0)).reshape(128 * 128, 1024)
    v = np.asarray(inputs["peer_v"], f)[0]
    vv = np.ascontiguousarray(v.reshape(128, 128, 1024).transpose(1, 0, 2)).reshape(128 * 128, 1024)
    keys = np.asarray(inputs["peer_keys"], f)[0].reshape(16, 128, 128)
    keysT = np.ascontiguousarray(keys.transpose(2, 0, 1)).reshape(128, 16 * 128)
    shared = {
        "ada_w": np.ascontiguousarray(np.asarray(inputs["ada_w"], f)[0]),
        "ada_b": np.ascontiguousarray(np.asarray(inputs["ada_b"], f)[0].reshape(48, 128).T),
        "ada_brow": np.ascontiguousarray(np.asarray(inputs["ada_b"], f)[0].reshape(1, 6 * D)),
        "g1n": pk(inputs["norm1_g"][0]), "g2n": pk(inputs["norm2_g"][0]),
        "fg": np.asarray(inputs["final_g"], f).reshape(1, D),
        "w_in": np.ascontiguousarray(np.asarray(inputs["w_in"], f)[0]),
        "pool_w": np.ascontiguousarray(np.asarray(inputs["pool_w"], f)[0]),
        "pscale": np.ascontiguousarray(np.asarray(inputs["pool_scale"], f)[0].reshape(4, 128).T),
        "tt": tt,
        "w_out": np.ascontiguousarray(np.asarray(inputs["w_out"], f)[0]),
        "peer_wq": np.ascontiguousarray(np.asarray(inputs["peer_wq"], f)[0]),
        "keysT": keysT, "uT": uT, "vv": vv,
        "colmask": cst["colmask"], "rowmask": cst["rowmask"], "ident": cst["ident"], "MT": cst["MT"],
        "iota16": cst["iota16"], "iota128": cst["iota128"],
    }
    maps = []
    for bi in range(8):
        mm = dict(shared)
        mm["x"] = np.ascontiguousarray(x[bi])
        mm["ctx"] = np.ascontiguousarray(ctx[bi])
        cc = np.stack([pk(c[bi]), pk(c_ctx)], axis=-1).reshape(128, 16)
        mm["cc"] = np.ascontiguousarray(cc)
        maps.append(mm)
    return maps


def kernel(**inputs):
    maps = _host_inputs(inputs)
    nc = build_nc()
    res = run_bass_kernel_spmd(nc, maps, core_ids=list(range(8)))
    return np.stack([np.asarray(r["out"], np.float32) for r in res.results], axis=0)
```

```python
import numpy as np
from contextlib import ExitStack
import concourse.bass as bass
import concourse.mybir as mybir
from concourse.bass_utils import run_bass_kernel_spmd

F32 = mybir.dt.float32
BF16 = mybir.dt.bfloat16
U32 = mybir.dt.uint32
I32 = mybir.dt.int32
AF = mybir.ActivationFunctionType
ALU = mybir.AluOpType
AX = mybir.AxisListType

D = 1024
SEQ = 4096
NT = SEQ // 128
NEG = -30000.0
EPS = 1e-6


class Buf:
    __slots__ = ("w", "r", "name")

    def __init__(self, name=""):
        self.w = None
        self.r = []
        self.name = name


class Sched:
    ENG = ("pe", "act", "dve", "pool", "sp")

    def __init__(self, nc, es, n_dma_sems=40):
        self.nc = nc
        self.streams = {e: [] for e in self.ENG}
        self.sem = {e: es.enter_context(nc.semaphore("s_" + e)) for e in self.ENG}
        self.cnt = {e: 0 for e in self.ENG}
        self.waited = {e: {} for e in self.ENG}
        self.dq = {"sp": list(range(0, 26)), "pool": list(range(26, 40)), "conv": list(range(40, 52))}
        n_dma_sems = 52
        self.dsem = [es.enter_context(nc.semaphore("s_dma%d" % i)) for i in range(n_dma_sems)]
        self.duse = [0] * n_dma_sems
        self.drr = {"sp": 0, "pool": 0, "conv": 0}
        self.out_toks = []

    def _waits(self, eng, reads, writes):
        waits = {}

        def need(tok):
            if tok is None:
                return
            sem, val, st = tok
            if st == "pe" and eng == "pe":
                return
            if self.waited[eng].get(id(sem), (None, 0))[1] >= val:
                return
            if id(sem) not in waits or waits[id(sem)][1] < val:
                waits[id(sem)] = (sem, val)

        for b in reads:
            need(b.w)
        for b in writes:
            need(b.w)
            for t in b.r:
                need(t)
        for k, v in waits.items():
            self.waited[eng][k] = v
        return list(waits.values())

    def _commit(self, tok, reads, writes):
        for b in reads:
            b.r.append(tok)
        for b in writes:
            b.w = tok
            b.r = []

    def add(self, eng, fn, reads=(), writes=(), inc=True):
        waits = self._waits(eng, reads, writes)
        sem = self.sem[eng]
        if inc:
            self.cnt[eng] += 1
            n = self.cnt[eng]
            tok = (sem, n, eng)
        else:
            assert eng == "pe"
            tok = (sem, self.cnt[eng] + 1, eng)
        assert self.cnt[eng] < 60000

        def th(e, waits=waits, fn=fn, inc=inc, sem=sem):
            for s, v in waits:
                e.wait_ge(s, v)
            ins = getattr(e, fn[0])(*fn[1], **fn[2])
            if inc:
                ins.then_inc(sem, 1)

        self.streams[eng].append(th)
        self._commit(tok, reads, writes)

    def dma(self, eng, out, in_, reads=(), writes=(), is_output=False, slow=False, qname=None):
        waits = self._waits(eng, reads, writes)
        qname = qname or eng
        q = self.dq[qname]
        k = q[self.drr[qname] % len(q)]
        self.drr[qname] += 1
        sem = self.dsem[k]
        prev = 16 * self.duse[k]
        if prev > 0 and self.waited[eng].get(id(sem), (None, 0))[1] < prev:
            self.waited[eng][id(sem)] = (sem, prev)
            waits = [w for w in waits if w[0] is not sem] + [(sem, prev)]
        self.duse[k] += 1
        tok = (sem, 16 * self.duse[k], "dma")

        def th(e, waits=waits, sem=sem, out=out, in_=in_, slow=slow):
            for s, v in waits:
                e.wait_ge(s, v)
            if slow:
                e.dma_start(out=out, in_=in_, allow_slow_non_contiguous=True).then_inc(sem, 16)
            else:
                e.dma_start(out=out, in_=in_).then_inc(sem, 16)

        self.streams[eng].append(th)
        self._commit(tok, reads, writes)
        if is_output:
            self.out_toks.append(tok)

    def barrier(self):
        toks = [(self.sem[e], self.cnt[e]) for e in self.ENG if self.cnt[e] > 0]
        toks += [(self.dsem[k], 16 * self.duse[k]) for k in range(len(self.dsem)) if self.duse[k] > 0 and k not in self.dq["conv"]]
        for eng in self.ENG:
            waits = []
            for s, v in toks:
                if self.waited[eng].get(id(s), (None, 0))[1] >= v:
                    continue
                self.waited[eng][id(s)] = (s, v)
                waits.append((s, v))

            def th(e, waits=waits):
                for s, v in waits:
                    e.wait_ge(s, v)

            self.streams[eng].append(th)

    def finish(self):
        final = {}
        for sem, val, _ in self.out_toks:
            if id(sem) not in final or final[id(sem)][1] < val:
                final[id(sem)] = (sem, val)
        fl = list(final.values())

        def th(e):
            for s, v in fl:
                e.wait_ge(s, v)

        self.streams["sp"].append(th)

    def replay(self, blk):
        st = self.streams

        @blk.sync
        def _(e):
            for th in st["sp"]:
                th(e)

        @blk.tensor
        def _(e):
            for th in st["pe"]:
                th(e)

        @blk.scalar
        def _(e):
            for th in st["act"]:
                th(e)

        @blk.vector
        def _(e):
            for th in st["dve"]:
                th(e)

        @blk.gpsimd
        def _(e):
            for th in st["pool"]:
                th(e)


def I(_opname, *a, **k):
    return (_opname, a, k)


def V(ap, off, dims):
    return bass.AP(ap.tensor, ap.offset + off, [list(ap.ap[0])] + [list(d) for d in dims])


def _consts():
    c = {}
    p = np.arange(128)
    qc = p % 64
    rho = p // 64
    kc = np.arange(64)
    c0 = np.clip(qc - 8, 0, 48)
    colmask = np.where((kc[None, :] >= c0[:, None]) & (kc[None, :] < c0[:, None] + 16), 0.0, NEG)
    c["colmask"] = colmask.astype(np.float32)
    rm = np.zeros((3, 128, 9, 64), np.float32)
    rm[0, :, 8, :] = NEG
    rm[1, :64, 8, :] = NEG
    rm[1, 64:, 0, :] = NEG
    rm[2, :, 0, :] = NEG
    c["rowmask"] = rm.reshape(3, 128, 576)
    c["ident"] = np.eye(128, dtype=np.float32)
    L = SEQ
    MT = np.zeros((5, 4, 128, 128), np.float32)

    def Mfull(w, i, rel):
        t = i * 128 + np.arange(128)
        s = (i + rel) * 128 + np.arange(128)
        lo = np.clip(t - w // 2, 0, L)
        hi = np.clip(t + w // 2, 0, L)
        cnt = (hi - lo).astype(np.float64)
        m = ((s[None, :] >= lo[:, None]) & (s[None, :] < hi[:, None])) / cnt[:, None] - (s[None, :] == t[:, None])
        return m.T

    for g, w in enumerate((2, 4, 8, 16)):
        MT[0, g] = Mfull(w, 5, 0)
        MT[1, g] = Mfull(w, 5, -1)
        MT[2, g] = Mfull(w, 5, 1)
        MT[3, g] = Mfull(w, 0, 0)
        MT[4, g] = Mfull(w, NT - 1, 0)
    c["MT"] = np.ascontiguousarray(MT.transpose(2, 0, 1, 3)).reshape(128, 20 * 128).astype(np.float32)
    c["iota16"] = np.tile(np.arange(16, dtype=np.float32)[None, :], (128, 1))
    c["iota128"] = np.tile(np.arange(128, dtype=np.float32)[None, :], (128, 1))
    return c


def _tile_geom(i):
    if i == 0:
        return 0, 0
    if i == 1:
        return 0, -2
    if i <= 29:
        return 1, -4
    if i == 30:
        return 2, -5
    return 2, -7


def build_nc(dbg=(), stop_after=None):
    nc = bass.Bass("TRN2", target_bir_lowering=False)

    def din(name, shape, dt=F32):
        return nc.dram_tensor(name, list(shape), dt, kind="ExternalInput").ap()

    def dscr(name, shape, dt):
        kind = "ExternalOutput" if name in dbg else "Internal"
        return nc.dram_tensor(name, list(shape), dt, kind=kind).ap()

    x_d = din("x", [SEQ, D])
    ctx_d = din("ctx", [256, D])
    cc_d = din("cc", [128, 16])
    adaw_d = din("ada_w", [D, 6 * D])
    adab_d = din("ada_b", [128, 48])
    adabrow_d = din("ada_brow", [1, 6 * D])
    g1n_d = din("g1n", [128, 8])
    g2n_d = din("g2n", [128, 8])
    fg_d = din("fg", [1, D])
    win_d = din("w_in", [D, 2048])
    poolw_d = din("pool_w", [4, 128, 128])
    pscale_d = din("pscale", [128, 4])
    tt_d = din("tt", [128, 8 * 17 * 64])
    wout_d = din("w_out", [D, D])
    wq_d = din("peer_wq", [D, 2048])
    keysT_d = din("keysT", [128, 16 * 128])
    uT_d = din("uT", [128 * 128, 1024])
    vv_d = din("vv", [128 * 128, 1024])
    colmask_d = din("colmask", [128, 64])
    rowmask_d = din("rowmask", [3, 128, 576])
    ident_d = din("ident", [128, 128])
    MT_d = din("MT", [128, 20 * 128])
    iota16_d = din("iota16", [128, 16])
    iota128_d = din("iota128", [128, 128])

    out_d = nc.dram_tensor("out", [SEQ, D], F32, kind="ExternalOutput").ap()

    gvec_d = dscr("gvec", [2, D], F32)
    qT_d = dscr("qT_scr", [128, 4, SEQ], BF16)
    kT_d = dscr("kT_scr", [128, 4, SEQ], BF16)
    p_d = dscr("p_scr", [SEQ, 512], BF16)
    v_d = dscr("v_scr", [SEQ, 512], BF16)
    x1_d = dscr("x1_scr", [SEQ, D], F32)
    h2T_d = dscr("h2T_scr", [128, 8, SEQ], BF16)
    rt_d = dscr("rt_scr", [128, 3, SEQ], BF16)
    uTb_d = dscr("uTb_scr", [128 * 128, 1024], BF16)
    vb_d = dscr("vb_scr", [128 * 128, 1024], BF16)

    es = ExitStack()
    with es:
        S = Sched(nc, es)

        def sb(st, name, shape, dt):
            return st.enter_context(nc.sbuf_tensor("sb_" + name, list(shape), dt))

        def ps(st, name, shape, dt=F32):
            return st.enter_context(nc.psum_tensor("ps_" + name, list(shape), dt))

        ident = sb(es, "ident", [128, 128], BF16)
        identf = sb(es, "identf", [128, 128], F32)
        mod = sb(es, "mod", [128, 48, 2], F32)
        gsc1 = sb(es, "gsc1", [128, 8], F32)
        gsc1c = sb(es, "gsc1c", [128, 8], F32)
        gsc2 = sb(es, "gsc2", [128, 8], F32)
        g1n = sb(es, "g1n", [128, 8], F32)
        g2n = sb(es, "g2n", [128, 8], F32)
        g1row = sb(es, "g1row", [128, D], F32)
        g2row = sb(es, "g2row", [128, D], F32)
        fgrow = sb(es, "fgrow", [128, D], F32)
        B_const = Buf("const")
        B_mod = Buf("mod")

        def rmsnorm_tile(xt, Bx, junk, Bjunk, ss, sq, rstd, Bst, xn, Bxn):
            S.add("act", I("activation", out=junk[:], in_=xt, func=AF.Square, accum_out=ss[:]),
                  reads=[Bx], writes=[Bjunk, Bst])
            S.add("act", I("activation", out=sq[:], in_=ss[:], func=AF.Sqrt, scale=1.0 / D, bias=epsc[:]),
                  reads=[B_const], writes=[Bst])
            S.add("dve", I("reciprocal", out=rstd[:], in_=sq[:]), reads=[], writes=[Bst])
            S.add("act", I("activation", out=xn[:], in_=xt, func=AF.Copy, scale=rstd[:]),
                  reads=[Bx, Bst], writes=[Bxn])

        epsc = sb(es, "epsc", [128, 1], F32)

        with ExitStack() as p0:
            cc = sb(p0, "cc", [128, 8, 2], F32)
            scc = sb(p0, "scc", [128, 8, 2], F32)
            adab = sb(p0, "adab", [128, 48], F32)
            awr = [sb(p0, "awr%d" % i, [128, 8, 512], F32) for i in range(2)]
            Bawr = [Buf(), Buf()]
            row_ps = [ps(p0, "row_ps%d" % i, [128, 512], F32) for i in range(4)]
            Brow = [Buf() for _ in range(4)]
            mod_ps = ps(p0, "mod_ps", [128, 48, 2], F32)
            Bmodps = Buf()
            tmp8 = sb(p0, "tmp8", [128, 8], F32)
            identtmp = sb(p0, "identtmp", [128, 128], F32)
            Bcc = Buf()

            S.add("dve", I("memset", epsc[:], EPS), writes=[B_const])
            S.dma("sp", cc[:].rearrange("p k t -> p (k t)"), cc_d, writes=[Bcc])
            S.dma("sp", adab[:], adab_d, writes=[Bcc])
            S.dma("sp", g1n[:], g1n_d, writes=[Bcc])
            S.dma("sp", g2n[:], g2n_d, writes=[Bcc])
            S.dma("sp", identf[:], ident_d, writes=[B_const])
            S.dma("sp", fgrow[:], fg_d[0:1, :].partition_broadcast(128), writes=[B_const])
            S.add("dve", I("tensor_copy", out=ident[:], in_=identf[:]), reads=[], writes=[B_const])
            S.add("act", I("activation", out=scc[:], in_=cc[:], func=AF.Silu), reads=[Bcc], writes=[Bcc])
            modrow = sb(p0, "modrow", [2, 6 * D], F32)
            Bmr = Buf()
            for cb in range(12):
                q = cb % 2
                S.dma("sp", awr[q][:], adaw_d[:, cb * 512:(cb + 1) * 512].rearrange("(k p) n -> p k n", p=128), writes=[Bawr[q]])
                for k in range(8):
                    S.add("pe", I("matmul", row_ps[cb % 4][0:2, :], lhsT=scc[:, k, :], rhs=awr[q][:, k, :], start=(k == 0), stop=(k == 7)),
                          reads=[Bawr[q], Bcc], writes=[Brow[cb % 4]], inc=(k == 7))
                S.add("act", I("activation", out=modrow[:, cb * 512:(cb + 1) * 512], in_=row_ps[cb % 4][0:2, :], func=AF.Copy),
                      reads=[Brow[cb % 4]], writes=[Bmr])
            for m in range(48):
                S.add("pe", I("transpose", out=mod_ps[:, m, :], in_=modrow[:, m * 128:(m + 1) * 128], identity=identf[0:2, 0:2]),
                      reads=[Bmr, B_const], writes=[Bmodps], inc=(m == 47))
            S.add("dve", I("tensor_tensor", out=mod[:], in0=mod_ps[:], in1=V(adab[:], 0, [[1, 48], [0, 2]]), op=ALU.add),
                  reads=[Bmodps, Bcc], writes=[B_mod])

            def mk_gsc(dst, gn, lo, col):
                S.add("dve", I("tensor_scalar", out=tmp8[:], in0=mod[:, lo:lo + 8, col], scalar1=1.0, scalar2=None, op0=ALU.add),
                      reads=[B_mod], writes=[Bcc])
                S.add("dve", I("tensor_tensor", out=dst[:], in0=tmp8[:], in1=gn[:], op=ALU.mult),
                      reads=[Bcc], writes=[B_mod])

            mk_gsc(gsc1, g1n, 8, 0)
            mk_gsc(gsc1c, g1n, 8, 1)
            mk_gsc(gsc2, g2n, 32, 0)
            abrow = sb(p0, "abrow", [128, 2, D], F32)
            Bab = Buf()
            onesr = sb(p0, "onesr", [1, 128], F32)
            S.add("dve", I("memset", onesr[:], 1.0), writes=[Bab])
            S.dma("sp", abrow[:, 0, :], adabrow_d[0:1, 2048:3072].partition_broadcast(128), writes=[Bab])
            S.dma("sp", abrow[:, 1, :], adabrow_d[0:1, 5120:6144].partition_broadcast(128), writes=[Bab])
            for gi, (col0, dst) in enumerate(((2048, g1row), (5120, g2row))):
                for hf in range(2):
                    q = (gi * 2 + hf) % 4
                    c0_ = col0 + hf * 512
                    S.add("pe", I("matmul", row_ps[q][:], lhsT=onesr[:], rhs=modrow[0:1, c0_:c0_ + 512], start=True, stop=True),
                          reads=[Bmr, Bab], writes=[Brow[q]], inc=True)
                    S.add("dve", I("tensor_tensor", out=dst[:, hf * 512:(hf + 1) * 512], in0=row_ps[q][:], in1=abrow[:, gi, hf * 512:(hf + 1) * 512], op=ALU.add),
                          reads=[Brow[q], Bab], writes=[B_const])

        S.barrier()
        if stop_after == 0:
            S.dma("sp", out_d[0:128, 0:96].rearrange("p (a b) -> p a b", b=2), mod[:], reads=[B_mod], writes=[Buf()], is_output=True, slow=True)
            S.dma("sp", out_d[128:256, :], g1row[:], reads=[B_const], writes=[Buf()], is_output=True)
            S.dma("sp", out_d[256:384, 0:8], gsc1[:], reads=[B_mod], writes=[Buf()], is_output=True)
            S.finish()
            with nc.Block() as blk:
                S.replay(blk)
            return nc

        rr = {"act": 0}

        def evac(out, in_, reads, writes, scale=None):
            rr["act"] ^= 1
            if rr["act"]:
                if scale is None:
                    S.add("act", I("activation", out=out, in_=in_, func=AF.Copy), reads=reads, writes=writes)
                else:
                    S.add("act", I("activation", out=out, in_=in_, func=AF.Copy, scale=float(scale)), reads=reads, writes=writes)
            else:
                if scale is None:
                    S.add("dve", I("tensor_copy", out=out, in_=in_), reads=reads, writes=writes)
                else:
                    S.add("dve", I("tensor_scalar", out=out, in0=in_, scalar1=float(scale), scalar2=None, op0=ALU.mult), reads=reads, writes=writes)

        Bu = [Buf() for _ in range(32)]
        Bvb = [Buf() for _ in range(32)]
        def convert(jb0, jb1):
            for jb in range(jb0, jb1):
                S.dma("pool", uTb_d[jb * 512:(jb + 1) * 512, :].rearrange("(p j) n -> j p n", j=4),
                      uT_d[jb * 512:(jb + 1) * 512, :].rearrange("(j p) n -> j p n", p=128), writes=[Bu[jb]], qname="conv")
                S.dma("pool", vb_d[jb * 512:(jb + 1) * 512, :].rearrange("(p j) n -> j p n", j=4),
                      vv_d[jb * 512:(jb + 1) * 512, :].rearrange("(j p) n -> j p n", p=128), writes=[Bvb[jb]], qname="conv")

        pA = ExitStack()
        es.enter_context(pA)
        kcT = sb(pA, "kcT", [128, 4, 256], BF16)
        vc = sb(pA, "vc", [128, 2, 512], BF16)
        B_ckv = Buf()
        Bq = [Buf() for _ in range(8)]
        Bk = [Buf() for _ in range(8)]
        Bp = [Buf() for _ in range(8)]
        Bv = [Buf() for _ in range(8)]
        Bx1 = [Buf() for _ in range(NT)]

        pC2 = ExitStack()
        ttf = sb(pC2, "ttf", [128, 8 * 17 * 64], F32)
        cmask = sb(pC2, "cmask", [128, 64], F32)
        TTb = sb(pC2, "TTb", [128, 8, 17, 64], BF16)
        rmask = sb(pC2, "rmask", [128, 3, 576], BF16)
        MTs = sb(pC2, "MTs", [128, 20, 128], BF16)
        poolw = sb(pC2, "poolw", [128, 4, 128], BF16)
        pscale = sb(pC2, "pscale", [128, 4], F32)
        w_out = sb(pC2, "w_out", [128, 8, D], BF16)
        B_c2 = Buf()
        Btt = Buf()
        with ExitStack() as p01:
            w_in = sb(p01, "w_in", [128, 8, 2048], BF16)
            B_win = Buf()
            for k in range(8):
                S.dma("pool", w_in[:, k, :], win_d[k * 128:(k + 1) * 128, :], writes=[B_win])
            convert(0, 8)
            xin = [sb(p01, "xin%d" % i, [128, D], F32) for i in range(2)]
            Bxin = [Buf(), Buf()]
            junk = sb(p01, "junk", [128, D], BF16)
            Bjunk = Buf()
            xn = [sb(p01, "xn%d" % i, [128, D], BF16) for i in range(2)]
            Bxn = [Buf(), Buf()]
            st_ss = [sb(p01, "ss%d" % i, [128, 1], F32) for i in range(2)]
            st_sq = [sb(p01, "sq%d" % i, [128, 1], F32) for i in range(2)]
            st_rs = [sb(p01, "rs%d" % i, [128, 1], F32) for i in range(2)]
            Bst = [Buf(), Buf()]
            psT = [ps(p01, "psT%d" % i, [128, 8, 128], BF16) for i in range(2)]
            BpsT = [Buf(), Buf()]
            z_ps = [ps(p01, "z_ps%d" % i, [128, 512], F32) for i in range(3)]
            Bz = [Buf() for _ in range(3)]
            zc = {"i": 0}

            def norm_T(src_ap, slot, dstT, col0, Bdst, gs, shcol):
                S.dma("sp", xin[slot][:], src_ap, writes=[Bxin[slot]])
                rmsnorm_tile(xin[slot][:], Bxin[slot], junk, Bjunk, st_ss[slot], st_sq[slot], st_rs[slot], Bst[slot], xn[slot], Bxn[slot])
                for k in range(8):
                    S.add("pe", I("transpose", out=psT[slot][:, k, :], in_=xn[slot][:, k * 128:(k + 1) * 128], identity=ident[:]),
                          reads=[Bxn[slot], B_const], writes=[BpsT[slot]], inc=(k == 7))
                for k in range(8):
                    S.add("dve", I("tensor_scalar", out=dstT[:, k, col0:col0 + 128], in0=psT[slot][:, k, :],
                                                                 scalar1=gs[:, k:k + 1], scalar2=mod[:, k + shcol[0], shcol[1]:shcol[1] + 1],
                                                                 op0=ALU.mult, op1=ALU.add),
                          reads=[BpsT[slot], B_mod], writes=[Bdst])

            def proj(out_ps_i, lhs_fn, rhs_fn, reads):
                zi = zc["i"] % 3
                zc["i"] += 1
                for k in range(8):
                    S.add("pe", I("matmul", z_ps[zi][:, 0:out_ps_i], lhsT=lhs_fn(k), rhs=rhs_fn(k), start=(k == 0), stop=(k == 7)),
                          reads=reads, writes=[Bz[zi]], inc=(k == 7))
                return zi

            with ExitStack() as p0b:
                hcT = sb(p0b, "hcT", [128, 8, 256], BF16)
                BhcT = Buf()
                for tl in range(2):
                    norm_T(ctx_d[tl * 128:(tl + 1) * 128, :], tl, hcT, tl * 128, BhcT, gsc1c, (0, 1))
                for j in range(4):
                    zi = proj(256, lambda k, j=j: w_in[:, k, 1024 + j * 128:1024 + (j + 1) * 128], lambda k: hcT[:, k, :], [B_win, BhcT])
                    evac(kcT[:, j, :], z_ps[zi][:, 0:256], [Bz[zi]], [B_ckv])
                for tl in range(2):
                    zi = proj(512, lambda k, tl=tl: hcT[:, k, tl * 128:(tl + 1) * 128], lambda k: w_in[:, k, 1536:2048], [B_win, BhcT])
                    evac(vc[:, tl, :], z_ps[zi][:, :], [Bz[zi]], [B_ckv])

            if stop_after != 1:
                S.barrier()
            if stop_after == 1:
                dbg_d = nc.dram_tensor("dbg0", [128, 4 * 256 + 2 * 512], BF16, kind="ExternalOutput").ap()
                S.dma("sp", dbg_d[:, 0:1024], kcT[:].rearrange("p a b -> p (a b)"), reads=[B_ckv], writes=[Buf()], is_output=True)
                S.dma("sp", dbg_d[:, 1024:2048], vc[:].rearrange("p a b -> p (a b)"), reads=[B_ckv], writes=[Buf()], is_output=True)
                dbg1_d = nc.dram_tensor("dbg1", [128, 4096], BF16, kind="ExternalOutput").ap()
                S.dma("sp", dbg1_d[:, 0:1024], w_in[:, 0, 0:1024], reads=[B_win], writes=[Buf()], is_output=True)
                S.dma("sp", dbg1_d[:, 1024:3072], hcT[:].rearrange("p a b -> p (a b)"), reads=[BhcT], writes=[Buf()], is_output=True)
                S.dma("sp", dbg1_d[:, 3072:4096], xn[1][:], reads=[Bxn[1]], writes=[Buf()], is_output=True)
                for qi, tl_ in enumerate((st_ss[1], st_sq[1], st_rs[1])):
                    dq = nc.dram_tensor("dbg2_%d" % qi, [128, 1], F32, kind="ExternalOutput").ap()
                    S.dma("sp", dq, tl_[:], reads=[Bst[1]], writes=[Buf()], is_output=True)
                S.finish()
                with nc.Block() as blk:
                    S.replay(blk)
                return nc

            for a in range(3):
                S.dma("pool", rmask[:, a, :], rowmask_d[a], writes=[B_c2])
            S.dma("pool", MTs[:].rearrange("p a b -> p (a b)"), MT_d, writes=[B_c2])
            for g in range(4):
                S.dma("pool", poolw[:, g, :], poolw_d[g], writes=[B_c2])
            for k in range(8):
                S.dma("pool", w_out[:, k, :], wout_d[k * 128:(k + 1) * 128, :], writes=[B_c2])
            convert(8, 20)
            hT = [sb(p01, "hT%d" % i, [128, 8, 512], BF16) for i in range(2)]
            BhT = [Buf(), Buf()]
            qTs = [sb(p01, "qTs%d" % i, [128, 4, 512], BF16) for i in range(2)]
            kTs = [sb(p01, "kTs%d" % i, [128, 4, 512], BF16) for i in range(2)]
            pss = [sb(p01, "pss%d" % i, [128, 4, 512], BF16) for i in range(2)]
            vss = [sb(p01, "vss%d" % i, [128, 4, 512], BF16) for i in range(2)]
            Bqs = [Buf(), Buf()]
            Bks = [Buf(), Buf()]
            Bpss = [Buf(), Buf()]
            Bvss = [Buf(), Buf()]
            pend = []
            for g in range(8):
                gs_ = g % 2
                for tl in range(4):
                    ti = g * 4 + tl
                    norm_T(x_d[ti * 128:(ti + 1) * 128, :], ti % 2, hT[gs_], tl * 128, BhT[gs_], gsc1, (0, 0))
                    if tl == 1:
                        for f in pend:
                            f()
                        pend = []
                for j in range(4):
                    zi = proj(512, lambda k, j=j: w_in[:, k, 512 + j * 128:512 + (j + 1) * 128], lambda k: hT[gs_][:, k, :], [B_win, BhT[gs_]])
                    evac(qTs[gs_][:, j, :], z_ps[zi][:, :], [Bz[zi]], [Bqs[gs_]], scale=0.125)
                for j in range(4):
                    zi = proj(512, lambda k, j=j: w_in[:, k, 1024 + j * 128:1024 + (j + 1) * 128], lambda k: hT[gs_][:, k, :], [B_win, BhT[gs_]])
                    evac(kTs[gs_][:, j, :], z_ps[zi][:, :], [Bz[zi]], [Bks[gs_]])
                for tl in range(4):
                    zi = proj(512, lambda k, tl=tl: hT[gs_][:, k, tl * 128:(tl + 1) * 128], lambda k: w_in[:, k, 0:512], [B_win, BhT[gs_]])
                    evac(pss[gs_][:, tl, :], z_ps[zi][:, :], [Bz[zi]], [Bpss[gs_]])
                    zi = proj(512, lambda k, tl=tl: hT[gs_][:, k, tl * 128:(tl + 1) * 128], lambda k: w_in[:, k, 1536:2048], [B_win, BhT[gs_]])
                    evac(vss[gs_][:, tl, :], z_ps[zi][:, :], [Bz[zi]], [Bvss[gs_]])
                def stores(g=g, gs_=gs_):
                    S.dma("sp", qT_d[:, :, g * 512:(g + 1) * 512], qTs[gs_][:], reads=[Bqs[gs_]], writes=[Bq[g]])
                    S.dma("sp", kT_d[:, :, g * 512:(g + 1) * 512], kTs[gs_][:], reads=[Bks[gs_]], writes=[Bk[g]])
                    S.dma("sp", p_d[g * 512:(g + 1) * 512, :].rearrange("(t p) n -> p t n", p=128), pss[gs_][:], reads=[Bpss[gs_]], writes=[Bp[g]])
                    S.dma("sp", v_d[g * 512:(g + 1) * 512, :].rearrange("(t p) n -> p t n", p=128), vss[gs_][:], reads=[Bvss[gs_]], writes=[Bv[g]])
                pend.append(stores)
            for f in pend:
                f()
            S.dma("sp", ttf[:], tt_d, writes=[Btt])
            S.dma("sp", cmask[:], colmask_d, writes=[Btt])
            S.dma("sp", pscale[:], pscale_d, writes=[B_c2])

        S.barrier()
        if stop_after == 2:
            S.dma("sp", out_d[0:128, 0:8], gsc1[:], reads=[Bq[7], Bk[7], Bp[7], Bv[7]] + Bq + Bk + Bp + Bv, writes=[Buf()], is_output=True)
            S.finish()
            with nc.Block() as blk:
                S.replay(blk)
            return nc

        with ExitStack() as p2:
            S.add("dve", I("tensor_tensor", out=TTb[:].rearrange("p h m c -> p (h m) c"),
                                                   in0=ttf[:].rearrange("p (a c) -> p a c", c=64),
                                                   in1=V(cmask[:], 0, [[0, 136], [1, 64]]), op=ALU.add),
                  reads=[Btt], writes=[B_c2])

            TTi = sb(p2, "TTi", [128, 8, 9, 64], BF16)
            S.add("dve", I("tensor_tensor", out=TTi[:].rearrange("p h m c -> p h (m c)"), in0=TTb[:, :, 4:13, :].rearrange("p h m c -> p h (m c)"),
                           in1=V(rmask[:], 576, [[0, 8], [1, 576]]), op=ALU.add),
                  reads=[B_c2], writes=[B_c2])
            QT = [sb(p2, "QT%d" % i, [128, 4, 128], BF16) for i in range(2)]
            KT = [sb(p2, "KT%d" % i, [128, 4, 576], BF16) for i in range(2)]
            Vt = [sb(p2, "Vt%d" % i, [128, 5, 512], BF16) for i in range(2)]
            Pt = [sb(p2, "Pt%d" % i, [128, 3, 512], BF16) for i in range(2)]
            xr = [sb(p2, "xr%d" % i, [128, D], F32) for i in range(2)]
            Bld = [Buf(), Buf()]
            Bxr = [Buf(), Buf()]
            S_ps = [ps(p2, "S_ps%d" % i, [128, 1024], F32) for i in range(2)]
            BS = [Buf(), Buf()]
            ET_ps = ps(p2, "ET_ps", [128, 7, 128], BF16)
            BETp = Buf()
            O_ps = ps(p2, "O_ps", [128, 512], F32)
            BO = Buf()
            pl_ps = ps(p2, "pl_ps", [128, 4, 128], F32)
            Bpl = Buf()
            out_ps = ps(p2, "out_ps", [128, 512], F32)
            Bout = Buf()
            E_sb = [sb(p2, "E_sb%d" % i, [128, 832], BF16) for i in range(2)]
            BE = [Buf(), Buf()]
            ET_sb = [sb(p2, "ET_sb%d" % i, [128, 7, 128], BF16) for i in range(2)]
            BET = [Buf(), Buf()]
            nmx = [sb(p2, "nmx%d" % i, [128, 1], F32) for i in range(2)]
            Bnmx = [Buf(), Buf()]
            rsum = [sb(p2, "rsum%d" % i, [128, 8], F32) for i in range(2)]
            rinv = [sb(p2, "rinv%d" % i, [128, 8], F32) for i in range(2)]
            Brs = [Buf(), Buf()]
            attn = sb(p2, "attn", [128, 512], BF16)
            Battn = Buf()
            pooledT = sb(p2, "pooledT", [128, 4, 128], BF16)
            Bpooled = Buf()
            mixT = [sb(p2, "mixT%d" % i, [128, 8, 128], BF16) for i in range(2)]
            Bmix = [Buf(), Buf()]
            t1 = sb(p2, "t1", [128, D], F32)
            Bt1 = Buf()
            x1s = [sb(p2, "x1s%d" % i, [128, D], F32) for i in range(2)]
            Bx1s = [Buf(), Buf()]

            def grp_range(lo_tok, hi_tok, arr):
                return [arr[g] for g in range(lo_tok // 512, (hi_tok - 1) // 512 + 1)]

            Bqt = [Buf(), Buf()]
            Bkt = [Buf(), Buf()]
            Bvta = [Buf(), Buf()]
            Bvtb = [Buf(), Buf()]
            Bpt = [Buf(), Buf()]

            def loads2(i):
                sl = i % 2
                mtype, off = _tile_geom(i)
                base = 2 * i + off
                kt0 = base * 64
                S.dma("sp", QT[sl][:], qT_d[:, :, i * 128:(i + 1) * 128], reads=[Bq[i // 4]], writes=[Bqt[sl]])
                S.dma("sp", KT[sl][:], kT_d[:, :, kt0:kt0 + 576], reads=grp_range(kt0, kt0 + 576, Bk), writes=[Bkt[sl]])
                S.dma("sp", Vt[sl][:, 0:4, :], v_d[kt0:kt0 + 512, :].rearrange("(c p) d -> p c d", p=128),
                      reads=grp_range(kt0, kt0 + 576, Bv), writes=[Bvta[sl]])
                S.dma("sp", Vt[sl][0:64, 4, :], v_d[kt0 + 512:kt0 + 576, :], reads=grp_range(kt0, kt0 + 576, Bv), writes=[Bvtb[sl]])
                plo = max(i - 1, 0)
                phi = min(i + 1, NT - 1)
                S.dma("sp", Pt[sl][:, plo - (i - 1):phi - (i - 1) + 1, :],
                      p_d[plo * 128:(phi + 1) * 128, :].rearrange("(c p) d -> p c d", p=128),
                      reads=grp_range(plo * 128, (phi + 1) * 128, Bp), writes=[Bpt[sl]])
                S.dma("sp", xr[sl][:], x_d[i * 128:(i + 1) * 128, :], writes=[Bxr[sl]])

            loads2(0)
            for i in range(NT):
                sl = i % 2
                mtype, off = _tile_geom(i)
                base = 2 * i + off
                m0 = off + 8
                kt0 = base * 64
                if i + 1 < NT:
                    loads2(i + 1)
                cols = [(0, 128), (128, 128), (256, 128), (384, 128), (576, 128), (704, 128), (512, 64)]

                def emit_qk(h):
                    j = h // 2
                    po = (h % 2) * 64
                    hs = h % 2
                    q_ap = QT[sl][po:po + 64, j, :]
                    rd = [Bqt[sl], Bkt[sl], B_c2, B_const, B_ckv]
                    Sp = S_ps[hs]
                    S.add("pe", I("matmul", Sp[:, 0:512], lhsT=q_ap, rhs=KT[sl][po:po + 64, j, 0:512], start=True, stop=False),
                          reads=rd, writes=[BS[hs]], inc=False)
                    if mtype == 1:
                        S.add("pe", I("matmul", Sp[:, 0:512], lhsT=ident[:], rhs=TTi[:, h, 0:8, :].rearrange("p a b -> p (a b)"), start=False, stop=True),
                              reads=rd, writes=[BS[hs]], inc=False)
                    else:
                        S.add("pe", I("matmul", Sp[:, 0:512], lhsT=ident[:], rhs=TTb[:, h, m0:m0 + 8, :].rearrange("p a b -> p (a b)"), start=False, stop=False),
                              reads=rd, writes=[BS[hs]], inc=False)
                        S.add("pe", I("matmul", Sp[:, 0:512], lhsT=ident[:], rhs=rmask[:, mtype, 0:512], start=False, stop=True),
                              reads=rd, writes=[BS[hs]], inc=False)
                    S.add("pe", I("matmul", Sp[:, 512:576], lhsT=q_ap, rhs=KT[sl][po:po + 64, j, 512:576], start=True, stop=False),
                          reads=rd, writes=[BS[hs]], inc=False)
                    if mtype == 1:
                        S.add("pe", I("matmul", Sp[:, 512:576], lhsT=ident[:], rhs=TTi[:, h, 8, :], start=False, stop=True),
                              reads=rd, writes=[BS[hs]], inc=False)
                    else:
                        S.add("pe", I("matmul", Sp[:, 512:576], lhsT=ident[:], rhs=TTb[:, h, m0 + 8, :], start=False, stop=False),
                              reads=rd, writes=[BS[hs]], inc=False)
                        S.add("pe", I("matmul", Sp[:, 512:576], lhsT=ident[:], rhs=rmask[:, mtype, 512:576], start=False, stop=True),
                              reads=rd, writes=[BS[hs]], inc=False)
                    S.add("pe", I("matmul", Sp[:, 576:832], lhsT=q_ap, rhs=kcT[po:po + 64, j, :], start=True, stop=True),
                          reads=rd, writes=[BS[hs]], inc=True)
                    S.add("dve", I("tensor_reduce", out=nmx[hs][:], in_=Sp[:, 0:832], axis=AX.X, op=ALU.max, negate=True),
                          reads=[BS[hs]], writes=[Bnmx[hs]])
                    S.add("act", I("activation", out=E_sb[hs][:], in_=Sp[:, 0:832], func=AF.Exp, bias=nmx[hs][:], scale=1.0,
                                   accum_out=rsum[sl][:, h:h + 1]),
                          reads=[BS[hs], Bnmx[hs]], writes=[BE[hs], Brs[sl]])

                def emit_T(h):
                    hs = h % 2
                    for c, (c0_, cw) in enumerate(cols):
                        S.add("pe", I("transpose", out=ET_ps[0:cw, c, :], in_=E_sb[hs][:, c0_:c0_ + cw], identity=ident[:]),
                              reads=[BE[hs], B_const], writes=[BETp], inc=(c == 6))
                    S.add("dve", I("tensor_copy", out=ET_sb[hs][:, 0:6, :], in_=ET_ps[:, 0:6, :]), reads=[BETp], writes=[BET[hs]])
                    S.add("act", I("activation", out=ET_sb[hs][0:64, 6, :], in_=ET_ps[0:64, 6, :], func=AF.Copy), reads=[BETp], writes=[BET[hs]])

                def emit_pv(h):
                    hs = h % 2
                    vsrc = [Vt[sl][:, 0, h * 64:(h + 1) * 64], Vt[sl][:, 1, h * 64:(h + 1) * 64], Vt[sl][:, 2, h * 64:(h + 1) * 64],
                            Vt[sl][:, 3, h * 64:(h + 1) * 64], vc[:, 0, h * 64:(h + 1) * 64], vc[:, 1, h * 64:(h + 1) * 64],
                            Vt[sl][0:64, 4, h * 64:(h + 1) * 64]]
                    for c in range(7):
                        lw = 64 if c == 6 else 128
                        S.add("pe", I("matmul", O_ps[:, h * 64:(h + 1) * 64], lhsT=ET_sb[hs][0:lw, c, :], rhs=vsrc[c],
                                      start=(c == 0), stop=(c == 6)),
                              reads=[BET[hs], Bvta[sl], Bvtb[sl], B_ckv], writes=[BO], inc=(c == 6))

                emit_qk(0)
                for h in range(8):
                    if h + 1 < 8:
                        emit_qk(h + 1)
                    emit_T(h)
                    if h >= 1:
                        emit_pv(h - 1)
                emit_pv(7)
                S.add("dve", I("reciprocal", out=rinv[sl][:], in_=rsum[sl][:]), reads=[Brs[sl]], writes=[Brs[sl]])
                S.add("dve", I("tensor_tensor", out=attn[:].rearrange("p (h d) -> p h d", d=64), in0=O_ps[:].rearrange("p (h d) -> p h d", d=64),
                                                       in1=V(rinv[sl][:], 0, [[1, 8], [0, 64]]), op=ALU.mult),
                      reads=[BO, Brs[sl]], writes=[Battn])
                for c in range(4):
                    S.add("pe", I("transpose", out=ET_ps[:, c, :], in_=attn[:, c * 128:(c + 1) * 128], identity=ident[:]),
                          reads=[Battn, B_const], writes=[BETp], inc=(c == 3))
                S.add("act", I("activation", out=mixT[sl][:, 4:8, :], in_=ET_ps[:, 0:4, :], func=AF.Copy), reads=[BETp], writes=[Bmix[sl]])
                rels = []
                if i > 0:
                    rels.append((0, 1))
                rels.append((1, 3 if i == 0 else (4 if i == NT - 1 else 0)))
                if i < NT - 1:
                    rels.append((2, 2))
                for g in range(4):
                    for ri, (slot, kind) in enumerate(rels):
                        S.add("pe", I("matmul", pl_ps[:, g, :], lhsT=Pt[sl][:, slot, g * 128:(g + 1) * 128], rhs=MTs[:, kind * 4 + g, :],
                                                                                   start=(ri == 0), stop=(ri == len(rels) - 1)),
                              reads=[Bpt[sl], B_c2], writes=[Bpl], inc=(g == 3 and ri == len(rels) - 1))
                S.add("act", I("activation", out=pooledT[:], in_=pl_ps[:], func=AF.Copy), reads=[Bpl], writes=[Bpooled])
                for g in range(4):
                    S.add("pe", I("matmul", pl_ps[:, g, :], lhsT=poolw[:, g, :], rhs=pooledT[:, g, :], start=True, stop=True),
                          reads=[Bpooled, B_c2], writes=[Bpl], inc=(g == 3))
                for g in range(4):
                    S.add("dve", I("tensor_scalar", out=mixT[sl][:, g, :], in0=pl_ps[:, g, :], scalar1=pscale[:, g:g + 1], scalar2=None, op0=ALU.mult),
                          reads=[Bpl, B_c2], writes=[Bmix[sl]])
                for hf in range(2):
                    for k in range(8):
                        S.add("pe", I("matmul", out_ps[:], lhsT=mixT[sl][:, k, :], rhs=w_out[:, k, hf * 512:(hf + 1) * 512],
                                      start=(k == 0), stop=(k == 7)),
                              reads=[Bmix[sl], B_c2], writes=[Bout], inc=(k == 7))
                    S.add("dve", I("tensor_tensor", out=t1[:, hf * 512:(hf + 1) * 512], in0=out_ps[:], in1=g1row[:, hf * 512:(hf + 1) * 512], op=ALU.mult),
                          reads=[Bout, B_const], writes=[Bt1])
                S.add("dve", I("tensor_tensor", out=x1s[sl][:], in0=t1[:], in1=xr[sl][:], op=ALU.add), reads=[Bt1, Bxr[sl]], writes=[Bx1s[sl]])
                S.dma("sp", x1_d[i * 128:(i + 1) * 128, :], x1s[sl][:], reads=[Bx1s[sl]], writes=[Bx1[i]])

        pC2.close()
        S.barrier()
        if stop_after == 3:
            S.dma("sp", out_d[0:128, 0:8], gsc1[:], reads=Bx1, writes=[Buf()], is_output=True)
            S.finish()
            with nc.Block() as blk:
                S.replay(blk)
            return nc

        Bh2 = [Buf() for _ in range(NT)]
        Brt = [Buf() for _ in range(NT)]
        with ExitStack() as p3:
            wq = sb(p3, "wq", [128, 8, 2048], BF16)
            keysT = sb(p3, "keysT", [128, 16, 128], BF16)
            iota16 = sb(p3, "iota16", [128, 16], F32)
            B_c3 = Buf()
            for k in range(8):
                S.dma("pool", wq[:, k, :], wq_d[k * 128:(k + 1) * 128, :], writes=[B_c3])
            S.dma("pool", keysT[:].rearrange("p a b -> p (a b)"), keysT_d, writes=[B_c3])
            convert(20, 32)
            S.dma("sp", iota16[:], iota16_d, writes=[B_c3])
            xin = [sb(p3, "xin3_%d" % i, [128, D], F32) for i in range(2)]
            Bxin = [Buf(), Buf()]
            junk = sb(p3, "junk3", [128, D], BF16)
            Bjunk = Buf()
            xn = [sb(p3, "xn3_%d" % i, [128, D], BF16) for i in range(2)]
            Bxn = [Buf(), Buf()]
            st_ss = [sb(p3, "ss3_%d" % i, [128, 1], F32) for i in range(2)]
            st_sq = [sb(p3, "sq3_%d" % i, [128, 1], F32) for i in range(2)]
            st_rs = [sb(p3, "rs3_%d" % i, [128, 1], F32) for i in range(2)]
            Bst = [Buf(), Buf()]
            psT = [ps(p3, "psT3_%d" % i, [128, 8, 128], BF16) for i in range(2)]
            BpsT = [Buf(), Buf()]
            h2T = [sb(p3, "h2T%d" % i, [128, 8, 128], BF16) for i in range(2)]
            Bh2s = [Buf(), Buf()]
            qp_ps = ps(p3, "qp_ps", [128, 16, 128], F32)
            Bqp = [Buf() for _ in range(4)]
            rt_ps = ps(p3, "rt_ps", [128, 3, 128], F32)
            Brtp = Buf()
            qpT = sb(p3, "qpT", [128, 16, 128], BF16)
            BqpT = [Buf() for _ in range(4)]
            s_sbs = [sb(p3, "s_sb%d" % i, [128, 16, 128], F32) for i in range(2)]
            Bss = [[Buf() for _ in range(4)] for _ in range(2)]
            s2 = sb(p3, "s2", [128, 16, 128], F32)
            Bs2 = [Buf() for _ in range(16)]
            Bsva = [Buf() for _ in range(16)]
            Bsvb = [Buf() for _ in range(16)]
            Bsia = [Buf() for _ in range(16)]
            Bsib = [Buf() for _ in range(16)]
            Btva = [Buf() for _ in range(8)]
            Btvb = [Buf() for _ in range(8)]
            Btia = [Buf() for _ in range(8)]
            Btib = [Buf() for _ in range(8)]
            Bc2 = [Buf() for _ in range(8)]
            sv = sb(p3, "sv", [128, 16, 16], F32)
            si_u = sb(p3, "si_u", [128, 16, 16], U32)
            si_f = sb(p3, "si_f", [128, 16, 16], F32)
            Bsv = Buf()
            cand = sb(p3, "cand", [128, 8, 256], F32)
            cand2 = sb(p3, "cand2", [128, 8, 256], F32)
            Bcand = Buf()
            tv = sb(p3, "tv", [128, 8, 16], F32)
            tvc = sb(p3, "tvc", [128, 8, 16], F32)
            ti_u = sb(p3, "ti_u", [128, 8, 16], U32)
            ta_u = sb(p3, "ta_u", [128, 8, 16], U32)
            tb_u = sb(p3, "tb_u", [128, 8, 16], U32)
            ta_f = sb(p3, "ta_f", [128, 8, 16], F32)
            tb_f = sb(p3, "tb_f", [128, 8, 16], F32)
            Btv = Buf()
            eqb = sb(p3, "eqb", [128, 8, 16, 16], F32)
            prod = sb(p3, "prod", [128, 8, 16, 16], F32)
            Beq = Buf()
            dlt = sb(p3, "dlt", [128, 8, 16], F32)
            ex = sb(p3, "ex", [128, 8, 16], F32)
            zz = sb(p3, "zz", [128, 8], F32)
            rz = sb(p3, "rz", [128, 8], F32)
            Bex = Buf()
            RTf = sb(p3, "RTf", [128, 3, 128], F32)
            BRTf = Buf()
            RTs = [sb(p3, "RTs%d" % i, [128, 3, 128], BF16) for i in range(2)]
            BRTs = [Buf(), Buf()]

            def part_A(i):
                sl = i % 2
                s_sb = s_sbs[sl]
                Bs = Bss[sl]
                S.dma("sp", xin[sl][:], x1_d[i * 128:(i + 1) * 128, :], reads=[Bx1[i]], writes=[Bxin[sl]])
                rmsnorm_tile(xin[sl][:], Bxin[sl], junk, Bjunk, st_ss[sl], st_sq[sl], st_rs[sl], Bst[sl], xn[sl], Bxn[sl])
                for k in range(8):
                    S.add("pe", I("transpose", out=psT[sl][:, k, :], in_=xn[sl][:, k * 128:(k + 1) * 128], identity=ident[:]),
                          reads=[Bxn[sl], B_const], writes=[BpsT[sl]], inc=(k == 7))
                for k in range(8):
                    S.add("act", I("activation", out=h2T[sl][:, k, :], in_=psT[sl][:, k, :], func=AF.Identity, scale=gsc2[:, k:k + 1],
                                   bias=mod[:, 24 + k, 0:1]),
                          reads=[BpsT[sl], B_mod], writes=[Bh2s[sl]])
                S.dma("sp", h2T_d[:, :, i * 128:(i + 1) * 128], h2T[sl][:], reads=[Bh2s[sl]], writes=[Bh2[i]])
                for jq in range(16):
                    for k in range(8):
                        S.add("pe", I("matmul", qp_ps[:, jq, :], lhsT=wq[:, k, jq * 128:(jq + 1) * 128], rhs=h2T[sl][:, k, :], start=(k == 0), stop=(k == 7)),
                              reads=[B_c3, Bh2s[sl]], writes=[Bqp[jq // 4]], inc=(k == 7 and jq % 4 == 3))
                for b4 in range(4):
                    evac(qpT[:, b4 * 4:(b4 + 1) * 4, :], qp_ps[:, b4 * 4:(b4 + 1) * 4, :], [Bqp[b4]], [BqpT[b4]])
                for jq in range(16):
                    S.add("pe", I("matmul", qp_ps[:, jq, :], lhsT=qpT[:, jq, :], rhs=keysT[:, jq, :], start=True, stop=True),
                          reads=[B_c3, BqpT[jq // 4]], writes=[Bqp[jq // 4]], inc=(jq % 4 == 3))
                for b4 in range(4):
                    S.add("act", I("activation", out=s_sb[:, b4 * 4:(b4 + 1) * 4, :], in_=qp_ps[:, b4 * 4:(b4 + 1) * 4, :], func=AF.Copy),
                          reads=[Bqp[b4]], writes=[Bs[b4]])

            def part_B(i):
                sl = i % 2
                s_sb = s_sbs[sl]
                Bs = Bss[sl]
                for jq in range(16):
                    S.add("dve", I("max", out=sv[:, jq, 0:8], in_=s_sb[:, jq, :]), reads=[Bs[jq // 4]], writes=[Bsva[jq]])
                for jq in range(16):
                    S.add("dve", I("max_index", out=si_u[:, jq, 0:8], in_max=sv[:, jq, 0:8], in_values=s_sb[:, jq, :]),
                          reads=[Bs[jq // 4], Bsva[jq]], writes=[Bsia[jq]])
                for jq in range(16):
                    S.add("dve", I("match_replace", out=s2[:, jq, :], in_to_replace=sv[:, jq, 0:8], in_values=s_sb[:, jq, :], imm_value=-1e30),
                          reads=[Bs[jq // 4], Bsva[jq]], writes=[Bs2[jq]])
                for jq in range(16):
                    S.add("dve", I("max", out=sv[:, jq, 8:16], in_=s2[:, jq, :]), reads=[Bs2[jq]], writes=[Bsvb[jq]])
                for jq in range(16):
                    S.add("dve", I("max_index", out=si_u[:, jq, 8:16], in_max=sv[:, jq, 8:16], in_values=s2[:, jq, :]),
                          reads=[Bs2[jq], Bsvb[jq]], writes=[Bsib[jq]])
                S.add("dve", I("tensor_copy", out=si_f[:], in_=si_u[:]), reads=Bsia + Bsib, writes=[Bsv])
                S.add("dve", I("tensor_tensor", out=cand[:].rearrange("p h (a b) -> p h a b", b=16),
                               in0=V(sv[:], 0, [[32, 8], [1, 16], [0, 16]]), in1=V(sv[:], 16, [[32, 8], [0, 16], [1, 16]]), op=ALU.add),
                      reads=Bsva + Bsvb, writes=[Bcand])
                for h in range(8):
                    S.add("dve", I("max", out=tv[:, h, 0:8], in_=cand[:, h, :]), reads=[Bcand], writes=[Btva[h]])
                for h in range(8):
                    S.add("dve", I("max_index", out=ti_u[:, h, 0:8], in_max=tv[:, h, 0:8], in_values=cand[:, h, :]), reads=[Bcand, Btva[h]], writes=[Btia[h]])
                for h in range(8):
                    S.add("dve", I("match_replace", out=cand2[:, h, :], in_to_replace=tv[:, h, 0:8], in_values=cand[:, h, :], imm_value=-1e30),
                          reads=[Bcand, Btva[h]], writes=[Bc2[h]])
                for h in range(8):
                    S.add("dve", I("max", out=tv[:, h, 8:16], in_=cand2[:, h, :]), reads=[Bc2[h]], writes=[Btvb[h]])
                for h in range(8):
                    S.add("dve", I("max_index", out=ti_u[:, h, 8:16], in_max=tv[:, h, 8:16], in_values=cand2[:, h, :]), reads=[Bc2[h], Btvb[h]], writes=[Btib[h]])
                S.add("dve", I("tensor_copy", out=tvc[:], in_=tv[:]), reads=Btva + Btvb, writes=[Btv])
                S.add("dve", I("tensor_scalar", out=ta_u[:], in0=ti_u[:], scalar1=4, scalar2=None, op0=ALU.logical_shift_right), reads=Btia + Btib, writes=[Btv])
                S.add("dve", I("tensor_scalar", out=tb_u[:], in0=ti_u[:], scalar1=15, scalar2=None, op0=ALU.bitwise_and), reads=Btia + Btib, writes=[Btv])
                S.add("dve", I("tensor_copy", out=ta_f[:], in_=ta_u[:]), reads=[], writes=[Btv])
                S.add("dve", I("tensor_copy", out=tb_f[:], in_=tb_u[:]), reads=[], writes=[Btv])
                for pp, tf in ((0, ta_f), (1, tb_f)):
                    S.add("dve", I("tensor_tensor", out=eqb[:], in0=V(tf[:], 0, [[16, 8], [1, 16], [0, 16]]),
                                   in1=V(iota16[:], 0, [[0, 8], [0, 16], [1, 16]]), op=ALU.is_equal),
                          reads=[Btv, B_c3], writes=[Beq])
                    S.add("dve", I("tensor_tensor", out=prod[:], in0=eqb[:], in1=V(si_f[:], pp * 16, [[32, 8], [0, 16], [1, 16]]), op=ALU.mult),
                          reads=[Bsv], writes=[Beq])
                    S.add("dve", I("tensor_reduce", out=RTf[:, pp, :].rearrange("p (h r) -> p h r", r=16), in_=prod[:], axis=AX.X, op=ALU.add),
                          reads=[Beq], writes=[BRTf])
                S.add("dve", I("tensor_tensor", out=dlt[:], in0=tvc[:], in1=V(tvc[:], 0, [[16, 8], [0, 16]]), op=ALU.subtract), reads=[Btv], writes=[Bex])
                S.add("act", I("activation", out=ex[:], in_=dlt[:], func=AF.Exp), reads=[], writes=[Bex])
                S.add("dve", I("tensor_reduce", out=zz[:], in_=ex[:], axis=AX.X, op=ALU.add), reads=[], writes=[Bex])
                S.add("dve", I("reciprocal", out=rz[:], in_=zz[:]), reads=[], writes=[Bex])
                S.add("dve", I("tensor_tensor", out=RTf[:, 2, :].rearrange("p (h r) -> p h r", r=16), in0=ex[:], in1=V(rz[:], 0, [[1, 8], [0, 16]]), op=ALU.mult),
                      reads=[Bex], writes=[BRTf])
                for c in range(3):
                    S.add("pe", I("transpose", out=rt_ps[:, c, :], in_=RTf[:, c, :], identity=identf[:]), reads=[BRTf, B_const], writes=[Brtp], inc=(c == 2))
                S.add("act", I("activation", out=RTs[sl][:], in_=rt_ps[:], func=AF.Copy), reads=[Brtp], writes=[BRTs[sl]])
                S.dma("sp", rt_d[:, :, i * 128:(i + 1) * 128], RTs[sl][:], reads=[BRTs[sl]], writes=[Brt[i]])

            part_A(0)
            for i in range(NT):
                if i + 1 < NT:
                    part_A(i + 1)
                part_B(i)

        S.barrier()
        if stop_after == 4:
            S.dma("sp", out_d[0:128, 0:8], gsc1[:], reads=Brt + Bh2, writes=[Buf()], is_output=True)
            S.finish()
            with nc.Block() as blk:
                S.replay(blk)
            return nc

        TG = 256
        NG = SEQ // TG
        JB = 4
        with ExitStack() as p4:
            iotab = sb(p4, "iotab", [128, 128], BF16)
            B_c4 = Buf()
            S.dma("pool", iotab[:], iota128_d, writes=[B_c4])
            h2g = [sb(p4, "h2g%d" % i, [128, 8, TG], BF16) for i in range(2)]
            rtg = [sb(p4, "rtg%d" % i, [128, 3, TG], BF16) for i in range(2)]
            Bg = [Buf(), Buf()]
            iota_rep = sb(p4, "iota_rep", [128, 128, 16], BF16)
            S.add("dve", I("tensor_copy", out=iota_rep[:], in_=V(iotab[:], 0, [[1, 128], [0, 16]])), reads=[B_c4], writes=[B_c4])
            P0 = [sb(p4, "P0_%d" % i, [128, 128, 16], BF16) for i in range(2)]
            P1 = [sb(p4, "P1_%d" % i, [128, 64, 16], BF16) for i in range(2)]
            P1w = [sb(p4, "P1w_%d" % i, [128, 64, 16], BF16) for i in range(2)]
            BP0 = [Buf(), Buf()]
            BP1 = [Buf(), Buf()]
            BP1w = [Buf(), Buf()]
            NCH = TG // 16
            G_half = [sb(p4, "G_half%d" % i, [128, 64, TG], BF16) for i in range(2)]
            BGh = [[Buf() for _ in range(NCH)] for _ in range(2)]
            G_ps = [ps(p4, "G_ps%d" % i, [128, 8, 64], F32) for i in range(2)]
            BGp = [Buf(), Buf()]
            A_ps = [ps(p4, "A_ps%d" % i, [128, 512], F32) for i in range(2)]
            BA = [Buf() for _ in range(4)]
            o_ps = [ps(p4, "o_ps%d" % i, [128, D], F32) for i in range(2)]
            Bo = [Buf(), Buf()]
            NW = 3
            ublk = [sb(p4, "ublk%d" % i, [128, JB, 1024], BF16) for i in range(NW)]
            vblk = [sb(p4, "vblk%d" % i, [128, JB, 1024], BF16) for i in range(NW)]
            Bw = [Buf() for _ in range(NW)]
            ga1 = [sb(p4, "ga1_%d" % i, [128, TG], BF16) for i in range(4)]
            Bga1 = [Buf() for _ in range(4)]
            GA = [sb(p4, "GA_%d" % i, [128, TG], BF16) for i in range(4)]
            BGA = [Buf() for _ in range(4)]
            x1t = [sb(p4, "x1t%d" % i, [128, D], F32) for i in range(2)]
            Bx1t = [Buf(), Buf()]
            t1bs = [sb(p4, "t1b%d" % i, [128, D], F32) for i in range(2)]
            Bt1bs = [Buf(), Buf()]
            x2s = [sb(p4, "x2_%d" % i, [128, D], F32) for i in range(2)]
            Bx2s = [Buf(), Buf()]
            junk4f = sb(p4, "junk4f", [128, D], F32)
            mhalf = sb(p4, "mhalf", [128, 1], F32)
            S.add("dve", I("memset", mhalf[:], -0.5), writes=[B_c4])
            Bjunk4 = Buf()
            fss = [sb(p4, "fss%d" % i, [128, 1], F32) for i in range(2)]
            fsq = [sb(p4, "fsq%d" % i, [128, 1], F32) for i in range(2)]
            frs = [sb(p4, "frs%d" % i, [128, 1], F32) for i in range(2)]
            Bfss = [Buf(), Buf()]
            yo = [sb(p4, "yo%d" % i, [128, D], F32) for i in range(2)]
            Byo = [Buf(), Buf()]
            wl = {"n": 0}
            gcnt = {"n": 0}
            wslot = {}

            def load_w(gg, jb):
                ws = wl["n"] % NW
                wl["n"] += 1
                wslot[(gg, jb)] = ws
                S.dma("sp", ublk[ws][:], uTb_d[jb * JB * 128:(jb + 1) * JB * 128, :].rearrange("(p j) n -> p j n", j=JB),
                      reads=[Bu[jb]], writes=[Bw[ws]])
                S.dma("sp", vblk[ws][:], vb_d[jb * JB * 128:(jb + 1) * JB * 128, :].rearrange("(p j) n -> p j n", j=JB),
                      reads=[Bvb[jb]], writes=[Bw[ws]])

            def load_grp(gg):
                gs_ = gg % 2
                t0 = gg * TG
                S.dma("sp", h2g[gs_][:], h2T_d[:, :, t0:t0 + TG], reads=[Bh2[2 * gg], Bh2[2 * gg + 1]], writes=[Bg[gs_]])
                S.dma("sp", rtg[gs_][:], rt_d[:, :, t0:t0 + TG], reads=[Brt[2 * gg], Brt[2 * gg + 1]], writes=[Bg[gs_]])

            def build_s1(gg, hb, c, part=None):
                gs_ = gg % 2
                s_ = c % 2
                if part in (None, 0):
                    S.add("dve", I("tensor_tensor", out=P0[s_][:], in0=iota_rep[:],
                                   in1=V(rtg[gs_][:], 0 * TG + c * 16, [[0, 128], [1, 16]]), op=ALU.is_equal),
                          reads=[Bg[gs_], B_c4], writes=[BP0[s_]])
                if part in (None, 1):
                    S.add("dve", I("tensor_tensor", out=P1[s_][:], in0=iota_rep[:, hb * 64:(hb + 1) * 64, :],
                                   in1=V(rtg[gs_][:], 1 * TG + c * 16, [[0, 64], [1, 16]]), op=ALU.is_equal),
                          reads=[Bg[gs_], B_c4], writes=[BP1[s_]])
                if part in (None, 2):
                    S.add("dve", I("tensor_tensor", out=P1w[s_][:], in0=P1[s_][:], in1=V(rtg[gs_][:], 2 * TG + c * 16, [[0, 64], [1, 16]]), op=ALU.mult),
                          reads=[BP1[s_], Bg[gs_]], writes=[BP1w[s_]])

            gslot = {}

            def build_s2(gg, hb, c, part=None):
                s_ = c % 2
                for q8 in range(2):
                    if part in (None, 0):
                        gb = gcnt["n"] % 2
                        gcnt["n"] += 1
                        gslot[(gg, hb, c, q8)] = gb
                        for tt in range(8):
                            t = q8 * 8 + tt
                            S.add("pe", I("matmul", G_ps[gb][:, tt, :], lhsT=P0[s_][:, :, t], rhs=P1w[s_][:, :, t], start=True, stop=True),
                                  reads=[BP0[s_], BP1w[s_]], writes=[BGp[gb]], inc=(tt == 7))
                    if part in (None, 1 + q8):
                        gb = gslot[(gg, hb, c, q8)]
                        tg0 = c * 16 + q8 * 8
                        S.add("act", I("activation", out=V(G_half[hb][:], tg0, [[TG, 64], [1, 8]]), in_=V(G_ps[gb][:], 0, [[1, 64], [64, 8]]), func=AF.Copy),
                              reads=[BGp[gb]], writes=[BGh[hb][c]])

            load_grp(0)
            for c in range(NCH):
                build_s1(0, 0, c)
                build_s2(0, 0, c)
            import os
            NA = int(os.environ.get('K_NA', '2'))
            DEPTH = NA // 2
            pending_tail = []
            for gg in range(NG):
                gs_ = gg % 2
                def emit_A(j):
                    jl = j % JB
                    if jl == 0 and (gg, j // JB) not in wslot:
                        load_w(gg, j // JB)
                    ws = wslot[(gg, j // JB)]
                    a_ = j % NA
                    hb = j // 64
                    Ap = A_ps[a_ // 2][:, (a_ % 2) * 256:(a_ % 2) * 256 + TG] if NA == 4 else A_ps[a_][:, 0:TG]
                    for k in range(8):
                        S.add("pe", I("matmul", Ap, lhsT=ublk[ws][:, jl, k * 128:(k + 1) * 128], rhs=h2g[gs_][:, k, :], start=(k == 0), stop=(k == 7)),
                              reads=[Bw[ws], Bg[gs_]], writes=[BA[a_]], inc=(k == 7))
                    S.add("act", I("activation", out=ga1[a_][:], in_=Ap, func=AF.Gelu), reads=[BA[a_]], writes=[Bga1[a_]])
                    S.add("dve", I("tensor_tensor", out=GA[a_][:], in0=ga1[a_][:], in1=G_half[hb][:, j % 64, :], op=ALU.mult),
                          reads=[Bga1[a_]] + BGh[hb], writes=[BGA[a_]])

                def emit_o(j):
                    jl = j % JB
                    ws = wslot[(gg, j // JB)]
                    a_ = j % NA
                    for tt in range(TG // 128):
                        for hf in range(2):
                            S.add("pe", I("matmul", o_ps[tt][:, hf * 512:(hf + 1) * 512], lhsT=GA[a_][:, tt * 128:(tt + 1) * 128],
                                          rhs=vblk[ws][:, jl, hf * 512:(hf + 1) * 512], start=(j == 0), stop=(j == 127)),
                                  reads=[BGA[a_], Bw[ws]], writes=[Bo[tt]], inc=(tt == TG // 128 - 1 and hf == 1))

                for j0 in range(DEPTH):
                    emit_A(j0)
                for j in range(128):
                    jj = j % 64
                    if j < 64:
                        nb = (gg, 1)
                    else:
                        nb = (gg + 1, 0) if gg + 1 < NG else None
                    while pending_tail and pending_tail[0][0] <= j:
                        pending_tail.pop(0)[1]()
                    if j == 64:
                        for tt in range(TG // 128):
                            ti = gg * (TG // 128) + tt
                            S.dma("sp", x1t[ti % 2][:], x1_d[ti * 128:(ti + 1) * 128, :], reads=[Bx1[ti]], writes=[Bx1t[ti % 2]])
                        if gg + 1 < NG:
                            load_grp(gg + 1)
                    if j == 125 and gg + 1 < NG:
                        load_w(gg + 1, 0)
                    if j + DEPTH < 128:
                        emit_A(j + DEPTH)
                    if nb is not None:
                        for c in range(NCH):
                            base = (c * 52) // NCH
                            for part in range(3):
                                if base + part == jj:
                                    build_s1(nb[0], nb[1], c, part)
                            for part in range(3):
                                if base + 4 + part == jj:
                                    build_s2(nb[0], nb[1], c, part)
                    emit_o(j)
                deferred = []
                for tt in range(TG // 128):
                    ti = gg * (TG // 128) + tt
                    ys = ti % 2
                    S.add("dve", I("tensor_tensor", out=t1bs[tt][:], in0=o_ps[tt][:], in1=g2row[:], op=ALU.mult), reads=[Bo[tt], B_const], writes=[Bt1bs[tt]])
                    base = 1 + tt * 10

                    def f_add(tt=tt, ys=ys):
                        S.add("pool", I("tensor_tensor", out=x2s[tt][:], in0=t1bs[tt][:], in1=x1t[ys][:], op=ALU.add), reads=[Bt1bs[tt], Bx1t[ys]], writes=[Bx2s[tt]])

                    def f_ss(tt=tt):
                        S.add("dve", I("scalar_tensor_tensor", out=junk4f[:], in0=x2s[tt][:], scalar=1.0, in1=x2s[tt][:], op0=ALU.mult, op1=ALU.mult, accum_out=fss[tt][:]),
                              reads=[Bx2s[tt]], writes=[Bjunk4, Bfss[tt]])

                    def f_ts(tt=tt):
                        S.add("dve", I("tensor_scalar", out=fsq[tt][:], in0=fss[tt][:], scalar1=1.0 / D, scalar2=EPS, op0=ALU.mult, op1=ALU.add), reads=[], writes=[Bfss[tt]])

                    def f_pow(tt=tt):
                        S.add("pool", I("tensor_tensor", out=frs[tt][:], in0=fsq[tt][:], in1=mhalf[:], op=ALU.pow), reads=[B_c4], writes=[Bfss[tt]])

                    def f_y(tt=tt, ys=ys):
                        S.add("dve", I("scalar_tensor_tensor", out=yo[ys][:], in0=x2s[tt][:], scalar=frs[tt][:], in1=fgrow[:], op0=ALU.mult, op1=ALU.mult),
                              reads=[Bx2s[tt], Bfss[tt], B_const], writes=[Byo[ys]])

                    def f_st(ti=ti, ys=ys):
                        S.dma("pool", out_d[ti * 128:(ti + 1) * 128, :], yo[ys][:], reads=[Byo[ys]], writes=[Buf()], is_output=True)

                    deferred += [(base, f_add), (base + 2, f_ss), (base + 3, f_ts), (base + 4, f_pow), (base + 6, f_y), (base + 7, f_st)]
                if gg + 1 < NG:
                    pending_tail = sorted(deferred, key=lambda x: x[0])
                else:
                    for _, f in sorted(deferred, key=lambda x: x[0]):
                        f()

        S.finish()
        with nc.Block() as blk:
            S.replay(blk)


    return nc


def _host_inputs(inputs):
    f = np.float32
    x = np.asarray(inputs["x"], f)
    c = np.asarray(inputs["c"], f)
    ctx = np.asarray(inputs["ctx"], f)
    c_ctx = np.asarray(inputs["c_ctx"], f)
    cst = _consts()
    rpb = np.asarray(inputs["na_rpb"], f)[0]
    p = np.arange(128)
    rho = p // 64
    qc = p % 64
    m = np.arange(17)
    kc = np.arange(64)
    a = m[None, :] - 1 - rho[:, None]
    b = kc[None, :] - qc[:, None] + 15
    va = (a >= 0) & (a <= 14)
    vb = (b >= 0) & (b <= 30)
    tt = rpb[:, np.clip(a, 0, 14)[:, :, None], np.clip(b, 0, 30)[:, None, :]]
    tt = np.where((va[:, :, None] & vb[:, None, :])[None], tt, 0.0).astype(f)
    tt = np.ascontiguousarray(tt.transpose(1, 0, 2, 3)).reshape(128, 8 * 17 * 64)

    def pk(v):
        return np.ascontiguousarray(np.asarray(v, f).reshape(8, 128).T)

    u = np.asarray(inputs["peer_u"], f)[0]
    uT = np.ascontiguousarray(u.reshape(128, 128, 8, 128).transpose(1, 3, 2, 0)).reshape(128 * 128, 1024)
    v = np.asarray(inputs["peer_v"], f)[0]
    vv = np.ascontiguousarray(v.reshape(128, 128, 1024).transpose(1, 0, 2)).reshape(128 * 128, 1024)
    keys = np.asarray(inputs["peer_keys"], f)[0].reshape(16, 128, 128)
    keysT = np.ascontiguousarray(keys.transpose(2, 0, 1)).reshape(128, 16 * 128)
    shared = {
        "ada_w": np.ascontiguousarray(np.asarray(inputs["ada_w"], f)[0]),
        "ada_b": np.ascontiguousarray(np.asarray(inputs["ada_b"], f)[0].reshape(48, 128).T),
        "ada_brow": np.ascontiguousarray(np.asarray(inputs["ada_b"], f)[0].reshape(1, 6 * D)),
        "g1n": pk(inputs["norm1_g"][0]), "g2n": pk(inputs["norm2_g"][0]),
        "fg": np.asarray(inputs["final_g"], f).reshape(1, D),
        "w_in": np.ascontiguousarray(np.asarray(inputs["w_in"], f)[0]),
        "pool_w": np.ascontiguousarray(np.asarray(inputs["pool_w"], f)[0]),
        "pscale": np.ascontiguousarray(np.asarray(inputs["pool_scale"], f)[0].reshape(4, 128).T),
        "tt": tt,
        "w_out": np.ascontiguousarray(np.asarray(inputs["w_out"], f)[0]),
        "peer_wq": np.ascontiguousarray(np.asarray(inputs["peer_wq"], f)[0]),
        "keysT": keysT, "uT": uT, "vv": vv,
        "colmask": cst["colmask"], "rowmask": cst["rowmask"], "ident": cst["ident"], "MT": cst["MT"],
        "iota16": cst["iota16"], "iota128": cst["iota128"],
    }
    maps = []
    for bi in range(8):
        mm = dict(shared)
        mm["x"] = np.ascontiguousarray(x[bi])
        mm["ctx"] = np.ascontiguousarray(ctx[bi])
        cc = np.stack([pk(c[bi]), pk(c_ctx)], axis=-1).reshape(128, 16)
        mm["cc"] = np.ascontiguousarray(cc)
        maps.append(mm)
    return maps


def kernel(**inputs):
    maps = _host_inputs(inputs)
    nc = build_nc()
    res = run_bass_kernel_spmd(nc, maps, core_ids=list(range(8)))
    return np.stack([np.asarray(r["out"], np.float32) for r in res.results], axis=0)
```
